# Optimizing a Trainium2 kernel written in Bass

```python
import math
import jax
import jax.numpy as jnp
from jax import lax
import numpy as np

D_MODEL = 1024
BATCH = 8
SEQ = 4096
DEPTH = 2

GRID_W = 64
CTX_LEN = 256
ROPE_THETA = 10000.0
BLOCK_Q = 128
MOE_BLOCK = 128

A_HEADS = 4
A_QK_DIM = 64
A_V_DIM = 128
A_WIDTH = A_HEADS * A_V_DIM
A_QK_COLS = 2 * A_HEADS * A_QK_DIM
B_HEADS = 4
B_HEAD = 64
B_WIDTH = B_HEADS * B_HEAD
B_DECAY_RANK = 64
B_A_RANK = 64
B_GATE_RANK = 128
C_HEADS = 4
C_NOPE = 64
C_ROPE = 32
C_V = 64
C_WIDTH = C_HEADS * C_V
C_Q_RANK = 256
C_KV_RANK = 128
MIX_WIDTH = A_WIDTH + B_WIDTH + C_WIDTH

N_A = 2 * A_QK_COLS + A_WIDTH
N_B = 3 * B_WIDTH + 2 * B_DECAY_RANK + 2 * B_A_RANK + B_GATE_RANK
N_C = C_Q_RANK + C_KV_RANK + C_ROPE
N_IN = N_A + N_B + N_C

D_FF = 3584
N_EXPERTS = 8
TOP_K = 2
N_DENSE = (DEPTH + 1) // 2
N_MOE = DEPTH // 2

ALPHA = (2.0 * DEPTH) ** 0.25
BETA = (8.0 * DEPTH) ** -0.25
LN_EPS = 1e-6
RMS_EPS = 1e-6
GN_EPS = 64e-5
F32 = jnp.float32

kernel_name = 'hybrid_diff_rwkv7_mla_moe_trunk'


def _layernorm(x, g=None, b=None, eps=LN_EPS):
    xf = x.astype(F32)
    xc = xf - jnp.mean(xf, -1, keepdims=True)
    y = xc * lax.rsqrt(jnp.mean(xc * xc, -1, keepdims=True) + eps)
    if g is not None:
        y = y * g.astype(F32) + b.astype(F32)
    return y.astype(x.dtype)


def _rmsnorm(x, g, eps=RMS_EPS):
    xf = x.astype(F32)
    y = xf * lax.rsqrt(jnp.mean(xf * xf, -1, keepdims=True) + eps) * g.astype(F32)
    return y.astype(x.dtype)


def _modulate(x, shift, scale):
    return _layernorm(x) * (1 + scale) + shift


def _lambda_init(layer):
    return 0.8 - 0.6 * math.exp(-0.3 * layer)


def _axial_angles(rows, cols, dim):
    quarter = dim // 4
    inv = ROPE_THETA ** (-jnp.arange(quarter, dtype=F32) / quarter)
    return rows[:, None] * inv, cols[:, None] * inv


def _rotate_pairs(x, ang):
    x1, x2 = jnp.split(x, 2, axis=-1)
    cs, sn = jnp.cos(ang)[:, None, :], jnp.sin(ang)[:, None, :]
    return jnp.concatenate([x1 * cs - x2 * sn, x1 * sn + x2 * cs], -1)


def _rope2d(x, ang_r, ang_c):
    xr, xc = jnp.split(x.astype(F32), 2, axis=-1)
    return jnp.concatenate([_rotate_pairs(xr, ang_r), _rotate_pairs(xc, ang_c)], -1).astype(x.dtype)


def _probs(q, k, scale):
    s = jnp.einsum('bhqd,bhkd->bhqk', q.astype(F32), k.astype(F32)) * scale
    return jax.nn.softmax(s, axis=-1)


def _attn_core(q, k, v, scale):
    return jnp.einsum('bhqk,bhkd->bhqd', _probs(q, k, scale), v.astype(F32))


def _diff_core(q1, q2, k1, k2, v, lam, scale):
    p = _probs(q1, k1, scale) - lam * _probs(q2, k2, scale)
    return jnp.einsum('bhqk,bhkd->bhqd', p, v.astype(F32))


def _sweep_query_blocks(fn, queries):
    Bn, H, L, _ = queries[0].shape
    nb = L // BLOCK_Q
    blocks = tuple(q.reshape(Bn, H, nb, BLOCK_Q, q.shape[-1]).transpose(2, 0, 1, 3, 4) for q in queries)
    out = lax.map(lambda qs: fn(*qs), blocks)
    return out.transpose(1, 2, 0, 3, 4).reshape(Bn, H, L, out.shape[-1])


def _merge_heads(o, dtype):
    Bn, H, L, d = o.shape
    return o.transpose(0, 2, 1, 3).reshape(Bn, L, H * d).astype(dtype)


def _diff_attention(pa_l, pa_c, lq1, lk1, lq2, lk2, norm_g, lam_init, rope, with_ctx):
    lam = (jnp.exp(jnp.sum(lq1.astype(F32) * lk1.astype(F32)))
           - jnp.exp(jnp.sum(lq2.astype(F32) * lk2.astype(F32))) + lam_init)
    scale = A_QK_DIM ** -0.5

    def heads(pa, rotary):
        Bn, L, _ = pa.shape
        q = pa[..., :A_QK_COLS].reshape(Bn, L, 2 * A_HEADS, A_QK_DIM)
        k = pa[..., A_QK_COLS:2 * A_QK_COLS].reshape(Bn, L, 2 * A_HEADS, A_QK_DIM)
        v = pa[..., 2 * A_QK_COLS:].reshape(Bn, L, A_HEADS, A_V_DIM)
        if rotary:
            q, k = _rope2d(q, *rope), _rope2d(k, *rope)
        return tuple(t.transpose(0, 2, 1, 3).astype(F32) for t in (q, k, v))

    def finish(o, dtype):
        return _merge_heads(_rmsnorm(o, norm_g) * (1.0 - lam_init), dtype)

    ql, kl, vl = heads(pa_l, True)
    qc, kc, vc = heads(pa_c, False)
    k_all = jnp.concatenate([kc, kl], axis=2)
    v_all = jnp.concatenate([vc, vl], axis=2)
    k1, k2 = k_all[:, 0::2], k_all[:, 1::2]
    o_l = _sweep_query_blocks(lambda q1, q2: _diff_core(q1, q2, k1, k2, v_all, lam, scale),
                              (ql[:, 0::2], ql[:, 1::2]))
    out_l = finish(o_l, pa_l.dtype)
    if not with_ctx:
        return out_l, None
    o_c = _diff_core(qc[:, 0::2], qc[:, 1::2], kc[:, 0::2], kc[:, 1::2], vc, lam, scale)
    return out_l, finish(o_c, pa_c.dtype)


def _token_shift(z, mu):
    zp = jnp.pad(z, ((0, 0), (1, 1), (0, 0)))
    return z + mu * (0.5 * (zp[:, :-2] + zp[:, 2:]) - z)


def _rwkv_prep(pb, shift_mu, w0, w2, a0, a2, g2, k_k, k_a):
    Bn, L, _ = pb.shape
    z = _token_shift(pb.astype(F32), shift_mu.astype(F32))
    heads = lambda t: t.reshape(t.shape[:-1] + (B_HEADS, B_HEAD))
    r, k, v = z[..., :B_WIDTH], z[..., B_WIDTH:2 * B_WIDTH], z[..., 2 * B_WIDTH:3 * B_WIDTH]
    o = 3 * B_WIDTH
    wd = z[..., o:o + 2 * B_DECAY_RANK].reshape(Bn, L, 2, B_DECAY_RANK)
    o += 2 * B_DECAY_RANK
    ad = z[..., o:o + 2 * B_A_RANK].reshape(Bn, L, 2, B_A_RANK)
    o += 2 * B_A_RANK
    gd = z[..., o:]
    w = -jax.nn.softplus(-(w0.astype(F32) + jnp.einsum('bldr,drc->bldc', jnp.tanh(wd), w2.astype(F32)))) - 0.5
    decay = jnp.exp(-jnp.exp(w))
    a = jax.nn.sigmoid(a0.astype(F32) + jnp.einsum('bldr,drc->bldc', ad, a2.astype(F32)))
    g = jnp.matmul(jax.nn.sigmoid(gd), g2.astype(F32))
    kk = heads(k * k_k.astype(F32))
    kk = kk / jnp.maximum(jnp.linalg.norm(kk, axis=-1, keepdims=True), 1e-12)
    k_dir = k[:, :, None, :] * (1.0 + (a - 1.0) * k_a.astype(F32))
    return heads(r), heads(v), kk, heads(decay), heads(k_dir), heads(a), g


def _wkv_scan(S0, r, w, k, v, a, b, reverse, emit):
    xs = tuple(jnp.swapaxes(t, 0, 1) for t in (r, w, k, v, a, b))

    def step(S, inp):
        r_t, w_t, k_t, v_t, a_t, b_t = inp
        sa = jnp.einsum('bhij,bhj->bhi', S, a_t)
        S = S * w_t[:, :, None, :] + sa[..., None] * b_t[:, :, None, :] + v_t[..., None] * k_t[:, :, None, :]
        return S, (jnp.einsum('bhij,bhj->bhi', S, r_t) if emit else None)

    S, ys = lax.scan(step, S0, xs, reverse=reverse)
    return S, (jnp.swapaxes(ys, 0, 1) if emit else None)


def _rwkv_bonus(r, k_d, v, r_k):
    return jnp.sum(r * k_d * r_k, axis=-1, keepdims=True) * v


def _rwkv_out(y, g, lnx_g, lnx_b, dtype):
    Bn, L = y.shape[:2]
    yn = _layernorm(y, lnx_g.reshape(B_HEADS, B_HEAD), lnx_b.reshape(B_HEADS, B_HEAD), GN_EPS)
    return (yn.reshape(Bn, L, B_WIDTH) * g).astype(dtype)


def _rwkv7(pb_l, pb_c, shift_mu, w0, w2, a0, a2, g2, k_k, k_a, r_k, lnx_g, lnx_b, with_ctx):
    prm = (shift_mu, w0, w2, a0, a2, g2, k_k, k_a)
    r_l, v_l, kk_l, dec_l, kd_l, a_l, g_l = _rwkv_prep(pb_l, *prm)
    r_c, v_c, kk_c, dec_c, kd_c, a_c, g_c = _rwkv_prep(pb_c, *prm)
    r_k = r_k.astype(F32)
    S0 = jnp.zeros((pb_l.shape[0], B_HEADS, B_HEAD, B_HEAD), F32)
    y_l, y_c = 0.0, 0.0
    for d, rev in ((0, False), (1, True)):
        S_ctx, yc = _wkv_scan(S0, r_c, dec_c[:, :, d], kd_c[:, :, d], v_c, -kk_c, kk_c * a_c[:, :, d], rev, with_ctx)
        _, yl = _wkv_scan(S_ctx, r_l, dec_l[:, :, d], kd_l[:, :, d], v_l, -kk_l, kk_l * a_l[:, :, d], rev, True)
        y_l = y_l + yl + _rwkv_bonus(r_l, kd_l[:, :, d], v_l, r_k)
        if with_ctx:
            y_c = y_c + yc + _rwkv_bonus(r_c, kd_c[:, :, d], v_c, r_k)
    out_l = _rwkv_out(y_l, g_l, lnx_g, lnx_b, pb_l.dtype)
    if not with_ctx:
        return out_l, None
    return out_l, _rwkv_out(y_c, g_c, lnx_g, lnx_b, pb_c.dtype)


def _mla(pc_l, pc_c, q_norm_g, w_uq, kv_norm_g, w_ukv, rope, with_ctx):
    scale = (C_NOPE + C_ROPE) ** -0.5

    def heads(pc, rotary):
        Bn, L, _ = pc.shape
        cq = _rmsnorm(pc[..., :C_Q_RANK], q_norm_g)
        ckv = _rmsnorm(pc[..., C_Q_RANK:C_Q_RANK + C_KV_RANK], kv_norm_g)
        k_pe = pc[..., C_Q_RANK + C_KV_RANK:][:, :, None, :]
        q = (cq @ w_uq).reshape(Bn, L, C_HEADS, C_NOPE + C_ROPE)
        kv = (ckv @ w_ukv).reshape(Bn, L, C_HEADS, C_NOPE + C_V)
        q_nope, q_pe = q[..., :C_NOPE], q[..., C_NOPE:]
        k_nope, v = kv[..., :C_NOPE], kv[..., C_NOPE:]
        if rotary:
            q_pe, k_pe = _rope2d(q_pe, *rope), _rope2d(k_pe, *rope)
        q = jnp.concatenate([q_nope, q_pe], -1)
        k = jnp.concatenate([k_nope, jnp.broadcast_to(k_pe, (Bn, L, C_HEADS, C_ROPE))], -1)
        return tuple(t.transpose(0, 2, 1, 3).astype(F32) for t in (q, k, v))

    ql, kl, vl = heads(pc_l, True)
    qc, kc, vc = heads(pc_c, False)
    k_all = jnp.concatenate([kc, kl], axis=2)
    v_all = jnp.concatenate([vc, vl], axis=2)
    o_l = _sweep_query_blocks(lambda q: _attn_core(q, k_all, v_all, scale), (ql,))
    out_l = _merge_heads(o_l, pc_l.dtype)
    if not with_ctx:
        return out_l, None
    return out_l, _merge_heads(_attn_core(qc, kc, vc, scale), pc_c.dtype)


def _swiglu(h, w1, w3, w2):
    return jnp.matmul(jax.nn.silu(h @ w1) * (h @ w3), w2)


def _moe(h, router, w1, w3, w2):
    Bn, L, D = h.shape
    t = h.reshape(-1, D)
    T = t.shape[0]
    logits = jnp.matmul(t, router).astype(F32)
    top_v, top_i = lax.top_k(logits, TOP_K)
    gates = jax.nn.softmax(top_v, axis=-1).astype(t.dtype)
    e_flat = top_i.reshape(-1)
    order = jnp.argsort(e_flat)
    e_sorted = e_flat[order]
    tok_sorted = order // TOP_K
    gate_sorted = gates.reshape(-1)[order]
    counts = jnp.bincount(e_flat, length=N_EXPERTS)
    padded = (counts + MOE_BLOCK - 1) // MOE_BLOCK * MOE_BLOCK
    start = jnp.cumsum(counts) - counts
    pend = jnp.cumsum(padded)
    pstart = pend - padded
    dest = pstart[e_sorted] + jnp.arange(T * TOP_K) - start[e_sorted]
    n_blocks = T * TOP_K // MOE_BLOCK + N_EXPERTS
    buf = jnp.zeros((n_blocks * MOE_BLOCK, D), t.dtype).at[dest].set(t[tok_sorted])
    block_e = jnp.minimum(jnp.searchsorted(pend, jnp.arange(n_blocks) * MOE_BLOCK, side='right'), N_EXPERTS - 1)

    def expert_block(args):
        xb, e = args
        return _swiglu(xb, w1[e], w3[e], w2[e])

    y_buf = lax.map(expert_block, (buf.reshape(n_blocks, MOE_BLOCK, D), block_e)).reshape(-1, D)
    out = jnp.zeros_like(t).at[tok_sorted].add(y_buf[dest] * gate_sorted[:, None])
    return out.reshape(Bn, L, D)


def setup_inputs(seed: int = 0) -> dict:
    key = jax.random.key(seed)
    keys = list(jax.random.split(key, 48))

    def nrm(shape, s):
        return jax.random.normal(keys.pop(), shape, jnp.float32) * s

    def unif(shape, lo, hi):
        return jax.random.uniform(keys.pop(), shape, jnp.float32, lo, hi)

    L = DEPTH
    return {
        'x': nrm((BATCH, SEQ, D_MODEL), 1.0),
        'c': nrm((BATCH, D_MODEL), 1.0),
        'ctx': nrm((BATCH, CTX_LEN, D_MODEL), 1.0),
        'c_ctx': nrm((D_MODEL,), 1.0),
        'ada_w': nrm((L, D_MODEL, 6 * D_MODEL), 0.5 * D_MODEL ** -0.5),
        'ada_b': nrm((L, 6 * D_MODEL), 0.02),
        'w_in': nrm((L, D_MODEL, N_IN), D_MODEL ** -0.5),
        'w_out': nrm((L, MIX_WIDTH, D_MODEL), BETA * MIX_WIDTH ** -0.5),
        'ln1_g': 1.0 + nrm((L, D_MODEL), 0.02),
        'ln1_b': nrm((L, D_MODEL), 0.02),
        'ln2_g': 1.0 + nrm((L, D_MODEL), 0.02),
        'ln2_b': nrm((L, D_MODEL), 0.02),
        'lam_q1': nrm((L, A_QK_DIM), 0.1),
        'lam_k1': nrm((L, A_QK_DIM), 0.1),
        'lam_q2': nrm((L, A_QK_DIM), 0.1),
        'lam_k2': nrm((L, A_QK_DIM), 0.1),
        'diff_norm_g': 1.0 + nrm((L, A_V_DIM), 0.02),
        'shift_mu': unif((L, N_B), 0.0, 1.0),
        'w0': unif((L, 2, B_WIDTH), -6.5, -0.5),
        'w2': nrm((L, 2, B_DECAY_RANK, B_WIDTH), 0.1 * B_DECAY_RANK ** -0.5),
        'a0': nrm((L, 2, B_WIDTH), 0.1),
        'a2': nrm((L, 2, B_A_RANK, B_WIDTH), 0.1 * B_A_RANK ** -0.5),
        'g2': nrm((L, B_GATE_RANK, B_WIDTH), B_GATE_RANK ** -0.5),
        'k_k': 0.85 + nrm((L, B_WIDTH), 0.02),
        'k_a': 1.0 + nrm((L, B_WIDTH), 0.02),
        'r_k': nrm((L, B_HEADS, B_HEAD), 0.1),
        'lnx_g': 1.0 + nrm((L, B_WIDTH), 0.02),
        'lnx_b': nrm((L, B_WIDTH), 0.02),
        'q_norm_g': 1.0 + nrm((L, C_Q_RANK), 0.02),
        'w_uq': nrm((L, C_Q_RANK, C_HEADS * (C_NOPE + C_ROPE)), C_Q_RANK ** -0.5),
        'kv_norm_g': 1.0 + nrm((L, C_KV_RANK), 0.02),
        'w_ukv': nrm((L, C_KV_RANK, C_HEADS * (C_NOPE + C_V)), C_KV_RANK ** -0.5),
        'ff_w1': nrm((N_DENSE, D_MODEL, D_FF), D_MODEL ** -0.5),
        'ff_w3': nrm((N_DENSE, D_MODEL, D_FF), D_MODEL ** -0.5),
        'ff_w2': nrm((N_DENSE, D_FF, D_MODEL), BETA * D_FF ** -0.5),
        'router': nrm((N_MOE, D_MODEL, N_EXPERTS), D_MODEL ** -0.5),
        'moe_w1': nrm((N_MOE, N_EXPERTS, D_MODEL, D_FF), D_MODEL ** -0.5),
        'moe_w3': nrm((N_MOE, N_EXPERTS, D_MODEL, D_FF), D_MODEL ** -0.5),
        'moe_w2': nrm((N_MOE, N_EXPERTS, D_FF, D_MODEL), BETA * D_FF ** -0.5),
    }


def reference(x, c, ctx, c_ctx, ada_w, ada_b, w_in, w_out, ln1_g, ln1_b, ln2_g, ln2_b,
              lam_q1, lam_k1, lam_q2, lam_k2, diff_norm_g, shift_mu, w0, w2, a0, a2, g2,
              k_k, k_a, r_k, lnx_g, lnx_b, q_norm_g, w_uq, kv_norm_g, w_ukv,
              ff_w1, ff_w3, ff_w2, router, moe_w1, moe_w3, moe_w2):
    n_rows = x.shape[1] // GRID_W
    rows = jnp.repeat(jnp.arange(n_rows, dtype=F32), GRID_W)
    cols = jnp.tile(jnp.arange(GRID_W, dtype=F32), n_rows)
    rope_a = _axial_angles(rows, cols, A_QK_DIM)
    rope_c = _axial_angles(rows, cols, C_ROPE)
    cond_l = jax.nn.silu(c)
    cond_c = jax.nn.silu(c_ctx)
    sa, sb, sc = slice(0, N_A), slice(N_A, N_A + N_B), slice(N_A + N_B, N_IN)
    xl, xc = x, ctx
    for i in range(DEPTH):
        with_ctx = i < DEPTH - 1
        ml = jnp.split((cond_l @ ada_w[i] + ada_b[i])[:, None, :], 6, axis=-1)
        mc = jnp.split((cond_c @ ada_w[i] + ada_b[i])[None, None, :], 6, axis=-1)
        p_l = _modulate(xl, ml[0], ml[1]) @ w_in[i]
        p_c = _modulate(xc, mc[0], mc[1]) @ w_in[i]
        a_l, a_c = _diff_attention(p_l[..., sa], p_c[..., sa], lam_q1[i], lam_k1[i], lam_q2[i], lam_k2[i],
                                   diff_norm_g[i], _lambda_init(i), rope_a, with_ctx)
        b_l, b_c = _rwkv7(p_l[..., sb], p_c[..., sb], shift_mu[i], w0[i], w2[i], a0[i], a2[i], g2[i],
                          k_k[i], k_a[i], r_k[i], lnx_g[i], lnx_b[i], with_ctx)
        c_l, c_c = _mla(p_l[..., sc], p_c[..., sc], q_norm_g[i], w_uq[i], kv_norm_g[i], w_ukv[i], rope_c, with_ctx)

        def ffn(h):
            j = i // 2
            if i % 2 == 0:
                return _swiglu(h, ff_w1[j], ff_w3[j], ff_w2[j])
            return _moe(h, router[j], moe_w1[j], moe_w3[j], moe_w2[j])

        o_l = jnp.concatenate([a_l, b_l, c_l], axis=-1) @ w_out[i]
        xl = _layernorm(ALPHA * xl + ml[2] * o_l, ln1_g[i], ln1_b[i])
        xl = _layernorm(ALPHA * xl + ml[5] * ffn(_modulate(xl, ml[3], ml[4])), ln2_g[i], ln2_b[i])
        if with_ctx:
            o_c = jnp.concatenate([a_c, b_c, c_c], axis=-1) @ w_out[i]
            xc = _layernorm(ALPHA * xc + mc[2] * o_c, ln1_g[i], ln1_b[i])
            xc = _layernorm(ALPHA * xc + mc[5] * ffn(_modulate(xc, mc[3], mc[4])), ln2_g[i], ln2_b[i])
    return xl
```

```python
import math
import contextlib
import numpy as np
import ml_dtypes
import concourse.bass as bass
import concourse.mybir as mybir
from concourse.bass_utils import run_bass_kernel_spmd

F32 = mybir.dt.float32
BF16 = mybir.dt.bfloat16
AF = mybir.ActivationFunctionType
ALU = mybir.AluOpType
AX = mybir.AxisListType
NDS = 8
SEM_ROT = 20000

D = 1024
SEQ = 4096
CTX = 256
NTOK = SEQ + CTX
NKC = NTOK // 128
DEPTH = 2
DFF = 3584
NFC = DFF // 128
NEXP = 8
ALPHA = (2.0 * DEPTH) ** 0.25
LN_EPS = 1e-6
RMS_EPS = 1e-6
GN_EPS = 64e-5
OQA, OQAP, OKA, OKAP, OVA, OB, OCQ, OCKV, OKPE, OKPEP, NCOL = 0, 512, 1024, 1536, 2048, 2560, 3712, 3968, 4096, 4192, 4288


def lam_init(layer):
    return 0.8 - 0.6 * math.exp(-0.3 * layer)


class T:
    __slots__ = ("w", "r")

    def __init__(self):
        self.w = None
        self.r = {}


class KB:
    def __init__(self, nc, same_eng_wait=True):
        self.nc = nc
        self.same = same_eng_wait
        self.eng = dict(pe=nc.tensor, act=nc.scalar, dve=nc.vector, pool=nc.gpsimd, sp=nc.sync)
        self.sems = []
        self.csem = {}
        self.ccnt = {}
        self.waited = {e: {} for e in self.eng}
        self.pend = {e: [] for e in self.eng}
        for e in ("pe", "act", "dve", "pool"):
            self._new_csem(e)
        self.dq = {}
        for q in ("sp", "act", "pool"):
            ids = []
            for i in range(NDS):
                self.sems.append(nc.alloc_semaphore(f"dq_{q}{i}"))
                ids.append(len(self.sems) - 1)
            self.dq[q] = dict(ids=ids, use=[0] * NDS, rr=0)
        self.nins = 0
        self._uid = 0
        self.tot = {}
        self.plog = []

    def uid(self, p="t"):
        self._uid += 1
        return f"{p}{self._uid}"

    def _new_csem(self, e):
        self.sems.append(self.nc.alloc_semaphore(f"cs_{e}{len(self.sems)}"))
        self.csem[e] = len(self.sems) - 1
        self.ccnt[e] = 0

    def wait(self, e, ev):
        s, v = ev
        if self.waited[e].get(s, 0) >= v:
            return
        self.eng[e].wait_ge(self.sems[s], v)
        self.nins += 1
        self.waited[e][s] = v

    def _deps(self, reads, writes):
        deps = []
        for t in reads:
            if t.w is not None:
                deps.append(t.w)
        for t in writes:
            if t.w is not None:
                deps.append(t.w)
            deps.extend((s_, v_, e_) for s_, (v_, e_) in t.r.items())
        return deps

    def op(self, e, fn, reads=(), writes=(), inc=True):
        for ev in self._deps(reads, writes):
            if ev[2] == e and (e == "pe" or not self.same):
                continue
            self.wait(e, ev[:2])
        ins = fn()
        self.nins += 1
        self.pend[e].append((reads, writes))
        if inc:
            if self.ccnt[e] >= SEM_ROT:
                self._new_csem(e)
            self.ccnt[e] += 1
            self.tot[e] = self.tot.get(e, 0) + 1
            s = self.csem[e]
            ins.then_inc(self.sems[s], 1)
            ev = (s, self.ccnt[e], e)
            for (rs, ws) in self.pend[e]:
                for t in rs:
                    t.r[s] = (ev[1], e)
                for t in ws:
                    t.w = ev
                    t.r = {}
            self.pend[e] = []
        return ins

    def dma(self, q, out, in_, reads=(), writes=(), **kw):
        d = self.dq[q]
        k = d["rr"]
        d["rr"] = (k + 1) % NDS
        s = d["ids"][k]
        j = d["use"][k]
        if j > 0:
            self.wait(q, (s, 16 * j))
        for ev in self._deps(reads, writes):
            self.wait(q, ev[:2])
        ins = self.eng[q].dma_start(out=out, in_=in_, **kw)
        self.nins += 1
        ins.then_inc(self.sems[s], 16)
        d["use"][k] = j + 1
        ev = (s, 16 * (j + 1), "dma_" + q)
        for t in reads:
            t.r[s] = (ev[1], ev[2])
        for t in writes:
            t.w = ev
            t.r = {}
        return ins

    def barrier(self, engines=("pe", "act", "dve", "pool", "sp")):
        evs = []
        for e in ("pe", "act", "dve", "pool"):
            assert not self.pend[e], f"pending non-inc ops on {e}"
            if self.ccnt[e] > 0:
                evs.append((self.csem[e], self.ccnt[e]))
        for q, d in self.dq.items():
            for s, u in zip(d["ids"], d["use"]):
                if u > 0:
                    evs.append((s, 16 * u))
        for e in engines:
            for ev in evs:
                self.wait(e, ev)


class Ring:
    def __init__(self, items):
        self.items = [(it, T()) for it in items]
        self.i = 0

    def next(self):
        it = self.items[self.i]
        self.i = (self.i + 1) % len(self.items)
        return it


class Prog:
    def __init__(self, debug=()):
        self.debug = set(debug)
        nc = self.nc = bass.Bass("TRN2", target_bir_lowering=False)
        self.kb = KB(nc)
        self.inp = {}
        self.scr = {}
        self.tk = {}

    def din(self, name, shape, dt=F32):
        self.inp[name] = self.nc.dram_tensor(name, list(shape), dt, kind="ExternalInput").ap()
        self.tk[name] = T()
        return self.inp[name]

    def dscr(self, name, shape, dt=F32, out=False):
        kind = "ExternalOutput" if (out or name in self.debug) else "Internal"
        self.scr[name] = self.nc.dram_tensor(name, list(shape), dt, kind=kind).ap()
        self.tk[name] = T()
        return self.scr[name]

    @contextlib.contextmanager
    def phase(self, name=""):
        st = contextlib.ExitStack()
        self._st = st
        try:
            yield st
            self.kb.barrier()
            import sys as _sys
            self.kb.plog.append((_sys._getframe(2).f_code.co_name, dict(self.kb.tot)))
        finally:
            st.close()

    def sb(self, shape, dt=F32, name=None):
        return self._st.enter_context(self.nc.sbuf_tensor(self.kb.uid(name or "sb"), list(shape), dt))

    def ps(self, shape, dt=F32, name=None):
        return self._st.enter_context(self.nc.psum_tensor(self.kb.uid(name or "ps"), list(shape), dt))

    def sbring(self, n, shape, dt=F32, name=None):
        return Ring([self.sb(shape, dt, name) for _ in range(n)])

    def psring(self, n, shape=(128, 512), dt=F32, name=None):
        return Ring([self.ps(shape, dt, name) for _ in range(n)])

    def declare(self, big=True):
        din, dscr = self.din, self.dscr
        din("x", [SEQ, D]); din("ctx", [CTX, D]); din("cc", [128, 8, 2])
        din("ada_w", [DEPTH, D, 6 * D]); din("ada_b2", [DEPTH, 2, 6 * D])
        din("w_in_ext", [DEPTH, D, NCOL])
        din("ident_bf", [128, 128], BF16); din("ident_f", [128, 128])
        din("cosA", [128, SEQ]); din("sinA", [128, SEQ]); din("cosC", [96, SEQ]); din("sinC", [96, SEQ])
        din("wuq", [DEPTH, 256, 384]); din("wuqp", [DEPTH, 256, 384]); din("wukv", [DEPTH, 128, 512]); din("wukv_v", [DEPTH, 128, 256])
        din("qng", [DEPTH, 128, 2]); din("kvng", [DEPTH, 128, 1])
        din("lamv", [DEPTH, 128, 4, 64]); din("dngc", [DEPTH, 128, 1])
        din("w_out", [DEPTH, D, D])
        din("lnp", [DEPTH, 4, 128, D])
        if big:
            din("ff_w1", [D, DFF]); din("ff_w3", [D, DFF]); din("ff_w2", [DFF, D])
            din("router", [D, NEXP]); din("moe_w1", [NEXP, D, DFF]); din("moe_w3", [NEXP, D, DFF]); din("moe_w2", [NEXP, DFF, D])
        dscr("mod_d", [DEPTH, 2, 6 * D])
        dscr("QAT", [512, NTOK], BF16); dscr("KAT", [512, NTOK], BF16); dscr("VA", [NTOK, 512], BF16)
        dscr("PBT", [1152, NTOK])
        dscr("QCT", [4, 96, NTOK], BF16); dscr("KCT", [4, 96, NTOK], BF16); dscr("VC", [NTOK, 256], BF16)
        dscr("CATT", [D, NTOK], BF16)
        dscr("X1", [NTOK, D]); dscr("X2", [NTOK, D])
        dscr("UT", [DFF, NTOK], BF16)
        dscr("FACC", [NTOK, D])
        dscr("HT", [D, NTOK], BF16); dscr("GT", [NEXP, NTOK]); dscr("YB", [2, NTOK, 256])
        din("rk_mu", [DEPTH, 128, 9]); din("rk_w0", [DEPTH, 2, 128, 2]); din("rk_a0", [DEPTH, 2, 128, 2])
        din("rk_w2", [DEPTH, 128, 256]); din("rk_a2", [DEPTH, 128, 256]); din("rk_g2", [DEPTH, 128, 256])
        din("rk_kk", [DEPTH, 128, 2]); din("rk_ka", [DEPTH, 128, 2]); din("rk_rk", [DEPTH, 128, 2])
        din("rk_lnx128", [DEPTH, 128, 2, 256]); din("rk_bones", [128, 128])
        din("rk_msk", [2, 64, 4, 128]); din("rk_mskT", [2, 64, 4, 64]); din("rk_id4", [64, 4, 64])
        din("sel8", [NEXP, NEXP, 128])
        dscr("y", [SEQ, D], out=True)

    def src_rows(self, L, stage, g0, n):
        if stage == 1:
            return self.scr["X1"][g0:g0 + n, :]
        if L == 0:
            if g0 < CTX:
                return self.inp["ctx"][g0:g0 + n, :]
            return self.inp["x"][g0 - CTX:g0 - CTX + n, :]
        return self.scr["X2"][g0:g0 + n, :]

    def p_adaln(self):
        nc, kb = self.nc, self.kb
        with self.phase():
            cc = self.sb([128, 8, 2]); tcc = T()
            cond = self.sb([128, 8, 2]); tcond = T()
            kb.dma("sp", cc[:], self.inp["cc"][:, :, :], writes=[tcc])
            kb.op("act", lambda: nc.scalar.activation(out=cond[:], in_=cc[:], func=AF.Silu), reads=[tcc], writes=[tcond])
            wring = self.sbring(2, [128, 8, 512])
            pring = self.psring(2, [2, 512])
            for L in range(DEPTH):
                brow = self.sb([2, 6 * D]); tb = T()
                mrow = self.sb([2, 6 * D]); tm = T()
                kb.dma("act", brow[:], self.inp["ada_b2"][L, :, :], writes=[tb])
                wv = self.inp["ada_w"][L].rearrange("(k p) n -> p k n", p=128)
                for n in range(12):
                    wt, tw = wring.next()
                    kb.dma("sp", wt[:], wv[:, :, n * 512:(n + 1) * 512], writes=[tw])
                    pt, tp = pring.next()
                    for k in range(8):
                        kb.op("pe", lambda k=k: nc.tensor.matmul(pt[:, :], lhsT=cond[:, k, :], rhs=wt[:, k, :], start=(k == 0), stop=(k == 7)),
                              reads=[tcond, tw], writes=[tp], inc=(k == 7))
                    kb.op("dve", lambda: nc.vector.tensor_tensor(out=mrow[:, n * 512:(n + 1) * 512], in0=pt[:, :], in1=brow[:, n * 512:(n + 1) * 512], op=ALU.add),
                          reads=[tp, tb], writes=[tm])
                kb.dma("sp", self.scr["mod_d"][L, :, :], mrow[:], reads=[tm])

    def load_mod(self, L, stage):
        nc, kb = self.nc, self.kb
        md = self.scr["mod_d"]
        res = {}
        for s in range(2):
            fm = self.sb([128, 16]); tfm = T()
            gb = self.sb([128, D]); tgb = T()
            base = stage * 3 * D
            for j in range(2):
                v = md[L, s, base + j * D: base + (j + 1) * D].rearrange("(k p) -> p k", p=128)
                kb.dma("sp", fm[:, j * 8:(j + 1) * 8], v, writes=[tfm], allow_slow_non_contiguous=True)
            kb.op("dve", lambda fm=fm: nc.vector.tensor_scalar_add(out=fm[:, 8:16], in0=fm[:, 8:16], scalar1=1.0), reads=[tfm], writes=[tfm])
            kb.dma("act", gb[:], md[L, s, base + 2 * D: base + 3 * D].partition_broadcast(128), writes=[tgb])
            res[s] = dict(fm=fm, tfm=tfm, gb=gb, tgb=tgb)
        return res

    def ln_mod_tile(self, L, stage, g0, ntok, mod, R):
        xm, txm = R["xm"].next()
        for _ in self.ln_mod_gen(L, stage, g0, ntok, mod, R, xm, txm):
            pass
        return xm, txm

    def ln_mod_gen(self, L, stage, g0, ntok, mod, R, xm, txm):
        nc, kb = self.nc, self.kb
        fm, tfm = mod["fm"], mod["tfm"]
        for j in range(ntok // 128):
            xt, tx = R["xt"].next()
            kb.dma("sp", xt[:], self.src_rows(L, stage, g0 + j * 128, 128), writes=[tx])
            st, tst = R["st"].next()
            kb.op("dve", lambda: nc.vector.bn_stats(out=st[:, 0:6], in_=xt[:, 0:512]), reads=[tx], writes=[tst])
            kb.op("dve", lambda: nc.vector.bn_stats(out=st[:, 6:12], in_=xt[:, 512:1024]), reads=[tx], writes=[tst])
            kb.op("dve", lambda: nc.vector.bn_aggr(out=st[:, 12:14], in_=st[:, 0:12]), reads=[tst], writes=[tst])
            kb.op("act", lambda: nc.scalar.activation(out=st[:, 14:15], in_=st[:, 13:14], func=AF.Ln, bias=LN_EPS), reads=[tst], writes=[tst])
            kb.op("act", lambda: nc.scalar.activation(out=st[:, 15:16], in_=st[:, 14:15], func=AF.Exp, scale=-0.5), reads=[tst], writes=[tst])
            xn, txn = R["xn"].next()
            kb.op("dve", lambda: nc.vector.tensor_scalar(out=xn[:], in0=xt[:], scalar1=st[:, 12:13], scalar2=st[:, 15:16], op0=ALU.subtract, op1=ALU.mult),
                  reads=[tx, tst], writes=[txn])
            pT, tpT = R["pT"].next()
            for k in range(8):
                kb.op("pe", lambda: nc.tensor.transpose(out=pT[:, k * 128:(k + 1) * 128], in_=xn[:, k * 128:(k + 1) * 128], identity=R["identb"][:]),
                      reads=[txn, R["tconst"]], writes=[tpT], inc=(k == 7))
            for k in range(8):
                if k % 2 == 0:
                    kb.op("act", lambda: nc.scalar.activation(out=xm[:, k, j * 128:(j + 1) * 128], in_=pT[:, k * 128:(k + 1) * 128], func=AF.Identity,
                                                              scale=fm[:, 8 + k:9 + k], bias=fm[:, k:k + 1]), reads=[tpT, tfm], writes=[txm])
                else:
                    kb.op("dve", lambda: nc.vector.tensor_scalar(out=xm[:, k, j * 128:(j + 1) * 128], in0=pT[:, k * 128:(k + 1) * 128],
                                                                 scalar1=fm[:, 8 + k:9 + k], scalar2=fm[:, k:k + 1], op0=ALU.mult, op1=ALU.add),
                          reads=[tpT, tfm], writes=[txm])
            yield

    def ln_rings(self):
        nc, kb = self.nc, self.kb
        R = dict(xt=self.sbring(2, [128, D]), st=self.sbring(3, [128, 16]), xn=self.sbring(2, [128, D], BF16),
                 pT=self.psring(2, [128, D], BF16), xm=self.sbring(2, [128, 8, 512], BF16))
        R["identb"] = self.sb([128, 128], BF16)
        R["tconst"] = T()
        kb.dma("act", R["identb"][:], self.inp["ident_bf"][:, :], writes=[R["tconst"]])
        return R

    def rms_bc(self, pss, n, width, out_t, tout):
        nc, kb = self.nc, self.kb
        ps_t, tps = pss
        kb.op("act", lambda: nc.scalar.activation(out=out_t[:, 0:n], in_=ps_t[:, 0:n], func=AF.Ln, scale=1.0 / width, bias=RMS_EPS), reads=[tps], writes=[tout])
        kb.op("act", lambda: nc.scalar.activation(out=out_t[:, 0:n], in_=out_t[:, 0:n], func=AF.Exp, scale=-0.5), reads=[tout], writes=[tout])

    def p_proj(self, L, with_ctx_q):
        nc, kb = self.nc, self.kb
        scr = self.scr
        with self.phase():
            R = self.ln_rings()
            tc = R["tconst"]
            wext = self.sb([128, 8, NCOL], BF16); twext = T()
            wv = self.inp["w_in_ext"][L].rearrange("(k p) n -> p k n", p=128)
            for k in range(8):
                kb.dma("pool", wext[:, k, :], wv[:, k, :], writes=[twext])
            wuq = self.sb([128, 2, 384], BF16); wuqp = self.sb([128, 2, 384], BF16)
            wukv = self.sb([128, 512], BF16); wukvv = self.sb([128, 256], BF16)
            kb.dma("pool", wuq[:], self.inp["wuq"][L].rearrange("(k p) n -> p k n", p=128), writes=[tc])
            kb.dma("pool", wuqp[:], self.inp["wuqp"][L].rearrange("(k p) n -> p k n", p=128), writes=[tc])
            kb.dma("pool", wukv[:], self.inp["wukv"][L], writes=[tc])
            kb.dma("pool", wukvv[:], self.inp["wukv_v"][L], writes=[tc])
            qng = self.sb([128, 2]); kvng = self.sb([128, 1])
            kb.dma("act", qng[:], self.inp["qng"][L], writes=[tc])
            kb.dma("act", kvng[:], self.inp["kvng"][L], writes=[tc])
            onesb = self.sb([128, 128], BF16)
            kb.op("pool", lambda: nc.gpsimd.memset(onesb[:], 1.0), writes=[tc])
            mods = self.load_mod(L, 0)
            pp = self.psring(4)
            sgb = self.sbring(6, [128, 512], BF16)
            sgf = self.sbring(6, [128, 512], F32)
            rcA = self.sbring(2, [128, 2, 512], F32)
            rcC = self.sbring(2, [96, 2, 512], F32)
            ded = [(self.sb([128, 512], BF16), T()) for _ in range(4)]
            tiles = [(0, CTX, 1)] + [(CTX + i * 512, 512, 0) for i in range(SEQ // 512)]
            evi = [0]

            def evac_copy(dst_ap, src_ap, reads, writes):
                evi[0] += 1
                if evi[0] % 2:
                    kb.op("act", lambda: nc.scalar.copy(out=dst_ap, in_=src_ap), reads=reads, writes=writes)
                else:
                    kb.op("dve", lambda: nc.vector.tensor_copy(out=dst_ap, in_=src_ap), reads=reads, writes=writes)

            lnq = {}

            def ln_start(ti_):
                g0_, n_, s_ = tiles[ti_]
                xm_, txm_ = R["xm"].next()
                lnq[ti_] = (xm_, txm_, self.ln_mod_gen(L, 0, g0_, n_, mods[s_], R, xm_, txm_))

            def ln_step(ti_):
                if ti_ in lnq:
                    try:
                        next(lnq[ti_][2])
                    except StopIteration:
                        pass
            ln_start(0)
            for ti, (g0, n, s) in enumerate(tiles):
                lat = (s == 0)
                t0 = g0 - CTX
                xm, txm, gcur = lnq.pop(ti)
                for _ in gcur:
                    pass
                if ti + 1 < len(tiles):
                    ln_start(ti + 1)

                def proj_fm(off, m):
                    pt, tp = pp.next()
                    for k in range(8):
                        kb.op("pe", lambda: nc.tensor.matmul(pt[0:m, 0:n], lhsT=wext[:, k, off:off + m], rhs=xm[:, k, 0:n], start=(k == 0), stop=(k == 7)),
                              reads=[twext, txm], writes=[tp], inc=(k == 7))
                    return pt, tp

                def rope_comb(dst, tdst, p1, tp1, p2, tp2, tab, ttab, lo, hi):
                    f1, tf1 = sgf.next()
                    f2, tf2 = sgf.next()
                    kb.op("dve", lambda: nc.vector.tensor_tensor(out=f1[lo:hi, 0:n], in0=p1[lo:hi, 0:n], in1=tab[lo:hi, 0, 0:n], op=ALU.mult), reads=[tp1, ttab], writes=[tf1])
                    kb.op("dve", lambda: nc.vector.tensor_tensor(out=f2[lo:hi, 0:n], in0=p2[lo:hi, 0:n], in1=tab[lo:hi, 1, 0:n], op=ALU.mult), reads=[tp2, ttab], writes=[tf2])
                    kb.op("pool", lambda: nc.gpsimd.tensor_tensor(out=dst[lo:hi, 0:n], in0=f1[lo:hi, 0:n], in1=f2[lo:hi, 0:n], op=ALU.add), reads=[tf1, tf2], writes=[tdst])

                if lat:
                    tabA, ttA = rcA.next()
                    kb.dma("act", tabA[:, 0, 0:n], self.inp["cosA"][:, t0:t0 + n], writes=[ttA])
                    kb.dma("act", tabA[:, 1, 0:n], self.inp["sinA"][:, t0:t0 + n], writes=[ttA])
                    tabC, ttC = rcC.next()
                    kb.dma("act", tabC[:, 0, 0:n], self.inp["cosC"][:, t0:t0 + n], writes=[ttC])
                    kb.dma("act", tabC[:, 1, 0:n], self.inp["sinC"][:, t0:t0 + n], writes=[ttC])
                for (off, offp, dst) in ((OQA, OQAP, "QAT"), (OKA, OKAP, "KAT")):
                    for c in range(4):
                        p1, tp1 = proj_fm(off + c * 128, 128)
                        sg, tsg = sgb.next()
                        if lat:
                            p2, tp2 = proj_fm(offp + c * 128, 128)
                            rope_comb(sg, tsg, p1, tp1, p2, tp2, tabA, ttA, 0, 128)
                        else:
                            evac_copy(sg[:, 0:n], p1[:, 0:n], [tp1], [tsg])
                        kb.dma("pool", scr[dst][c * 128:(c + 1) * 128, g0:g0 + n], sg[:, 0:n], reads=[tsg])
                ln_step(ti + 1)
                for j in range(n // 128):
                    pt, tp = pp.next()
                    for k in range(8):
                        kb.op("pe", lambda: nc.tensor.matmul(pt[:, :], lhsT=xm[:, k, j * 128:(j + 1) * 128], rhs=wext[:, k, OVA:OVA + 512], start=(k == 0), stop=(k == 7)),
                              reads=[twext, txm], writes=[tp], inc=(k == 7))
                    sg, tsg = sgb.next()
                    evac_copy(sg[:, :], pt[:, :], [tp], [tsg])
                    kb.dma("pool", scr["VA"][g0 + j * 128:g0 + (j + 1) * 128, :], sg[:, :], reads=[tsg])
                ln_step(ti + 1)
                for c in range(9):
                    p1, tp1 = proj_fm(OB + c * 128, 128)
                    sg, tsg = sgf.next()
                    evac_copy(sg[:, 0:n], p1[:, 0:n], [tp1], [tsg])
                    kb.dma("pool", scr["PBT"][c * 128:(c + 1) * 128, g0:g0 + n], sg[:, 0:n], reads=[tsg])
                ln_step(ti + 1)
                cqn = []
                cqr = []
                ssp = pp.next()
                for c in range(2):
                    p1, tp1 = proj_fm(OCQ + c * 128, 128)
                    f, tf = sgf.next()
                    kb.op("act", lambda: nc.scalar.copy(out=f[:, 0:n], in_=p1[:, 0:n]), reads=[tp1], writes=[tf])
                    sq, tsq = sgb.next()
                    kb.op("act", lambda: nc.scalar.activation(out=sq[:, 0:n], in_=p1[:, 0:n], func=AF.Square), reads=[tp1], writes=[tsq])
                    kb.op("pe", lambda: nc.tensor.matmul(ssp[0][:, 0:n], lhsT=onesb[:, :], rhs=sq[:, 0:n], start=(c == 0), stop=(c == 1)),
                          reads=[tc, tsq], writes=[ssp[1]], inc=(c == 1))
                    cqr.append((f, tf))
                rsb, trsb = sgf.next()
                self.rms_bc(ssp, n, 256.0, rsb, trsb)
                for c in range(2):
                    f, tf = cqr[c]
                    o, to = ded[c]
                    kb.op("dve", lambda: nc.vector.scalar_tensor_tensor(out=o[:, 0:n], in0=f[:, 0:n], scalar=qng[:, c:c + 1], in1=rsb[:, 0:n], op0=ALU.mult, op1=ALU.mult),
                          reads=[tf, trsb, tc], writes=[to])
                    cqn.append((o, to))
                if lat or with_ctx_q:
                    for h in range(4):
                        pq, tpq = pp.next()
                        for rc in range(2):
                            kb.op("pe", lambda: nc.tensor.matmul(pq[0:96, 0:n], lhsT=wuq[:, rc, h * 96:(h + 1) * 96], rhs=cqn[rc][0][:, 0:n], start=(rc == 0), stop=(rc == 1)),
                                  reads=[tc, cqn[rc][1]], writes=[tpq], inc=(rc == 1))
                        sg, tsg = sgb.next()
                        if lat:
                            pq2, tpq2 = pp.next()
                            for rc in range(2):
                                kb.op("pe", lambda: nc.tensor.matmul(pq2[0:96, 0:n], lhsT=wuqp[:, rc, h * 96:(h + 1) * 96], rhs=cqn[rc][0][:, 0:n], start=(rc == 0), stop=(rc == 1)),
                                      reads=[tc, cqn[rc][1]], writes=[tpq2], inc=(rc == 1))
                            evac_copy(sg[0:64, 0:n], pq[0:64, 0:n], [tpq], [tsg])
                            rope_comb(sg, tsg, pq, tpq, pq2, tpq2, tabC, ttC, 64, 96)
                        else:
                            evac_copy(sg[0:96, 0:n], pq[0:96, 0:n], [tpq], [tsg])
                        kb.dma("pool", scr["QCT"][h, :, g0:g0 + n], sg[0:96, 0:n], reads=[tsg])
                ln_step(ti + 1)
                p1, tp1 = proj_fm(OCKV, 128)
                f, tf = sgf.next()
                kb.op("act", lambda: nc.scalar.copy(out=f[:, 0:n], in_=p1[:, 0:n]), reads=[tp1], writes=[tf])
                sq, tsq = sgb.next()
                kb.op("act", lambda: nc.scalar.activation(out=sq[:, 0:n], in_=p1[:, 0:n], func=AF.Square), reads=[tp1], writes=[tsq])
                ssp = pp.next()
                kb.op("pe", lambda: nc.tensor.matmul(ssp[0][:, 0:n], lhsT=onesb[:, :], rhs=sq[:, 0:n], start=True, stop=True), reads=[tc, tsq], writes=[ssp[1]])
                rsb, trsb = sgf.next()
                self.rms_bc(ssp, n, 128.0, rsb, trsb)
                ckvn, tckvn = ded[2]
                kb.op("dve", lambda: nc.vector.scalar_tensor_tensor(out=ckvn[:, 0:n], in0=f[:, 0:n], scalar=kvng[:, 0:1], in1=rsb[:, 0:n], op0=ALU.mult, op1=ALU.mult),
                      reads=[tf, trsb, tc], writes=[tckvn])
                pk, tpk = proj_fm(OKPE, 96)
                kpe, tkpe = ded[3]
                if lat:
                    pk2, tpk2 = proj_fm(OKPEP, 96)
                    rope_comb(kpe, tkpe, pk, tpk, pk2, tpk2, tabC, ttC, 64, 96)
                else:
                    evac_copy(kpe[64:96, 0:n], pk[64:96, 0:n], [tpk], [tkpe])
                for h in range(4):
                    pkn, tpkn = pp.next()
                    kb.op("pe", lambda: nc.tensor.matmul(pkn[0:64, 0:n], lhsT=wukv[:, h * 128:h * 128 + 64], rhs=ckvn[:, 0:n], start=True, stop=True),
                          reads=[tc, tckvn], writes=[tpkn])
                    sg, tsg = sgb.next()
                    evac_copy(sg[0:64, 0:n], pkn[0:64, 0:n], [tpkn], [tsg])
                    kb.dma("pool", scr["KCT"][h, 0:64, g0:g0 + n], sg[0:64, 0:n], reads=[tsg])
                    kb.dma("pool", scr["KCT"][h, 64:96, g0:g0 + n], kpe[64:96, 0:n], reads=[tkpe])
                for j in range(n // 128):
                    pv, tpv = pp.next()
                    kb.op("pe", lambda: nc.tensor.matmul(pv[:, 0:256], lhsT=ckvn[:, j * 128:(j + 1) * 128], rhs=wukvv[:, :], start=True, stop=True),
                          reads=[tc, tckvn], writes=[tpv])
                    sg, tsg = sgb.next()
                    evac_copy(sg[:, 0:256], pv[:, 0:256], [tpv], [tsg])
                    kb.dma("pool", scr["VC"][g0 + j * 128:g0 + (j + 1) * 128, :], sg[:, 0:256], reads=[tsg])


    def p_attn_a(self, L, with_ctx):
        nc, kb = self.nc, self.kb
        scr = self.scr
        sc = 64 ** -0.5
        li = lam_init(L)
        with self.phase():
            tc = T()
            KT = self.sb([128, 4, NTOK], BF16)
            V = self.sb([128, NKC, 512], BF16)
            for c in range(4):
                kb.dma("sp", KT[:, c, :], scr["KAT"][c * 128:(c + 1) * 128, :], writes=[tc])
            vv = scr["VA"].rearrange("(k p) n -> p k n", p=128)
            for k0 in range(0, NKC, 6):
                k1 = min(NKC, k0 + 6)
                kb.dma("act", V[:, k0:k1, :], vv[:, k0:k1, :], writes=[tc])
            onesb = self.sb([128, 128], BF16); onesf = self.sb([128, 128])
            kb.op("pool", lambda: nc.gpsimd.memset(onesb[:], 1.0), writes=[tc])
            kb.op("pool", lambda: nc.gpsimd.memset(onesf[:], 1.0), writes=[tc])
            dng = self.sb([128, 1])
            kb.dma("act", dng[:], self.inp["dngc"][L], writes=[tc])
            kb.op("dve", lambda: nc.vector.tensor_scalar(out=dng[:], in0=dng[:], scalar1=(1.0 - li), scalar2=None, op0=ALU.mult), reads=[tc], writes=[tc])
            lv = self.sb([128, 4, 64]); lt = self.sb([128, 2, 64]); ls = self.sb([128, 4]); tl = T()
            kb.dma("act", lv[:], self.inp["lamv"][L], writes=[tl])
            kb.op("dve", lambda: nc.vector.tensor_tensor(out=lt[:, 0, :], in0=lv[:, 0, :], in1=lv[:, 1, :], op=ALU.mult), reads=[tl], writes=[tl])
            kb.op("dve", lambda: nc.vector.tensor_tensor(out=lt[:, 1, :], in0=lv[:, 2, :], in1=lv[:, 3, :], op=ALU.mult), reads=[tl], writes=[tl])
            kb.op("dve", lambda: nc.vector.tensor_reduce(out=ls[:, 0:2], in_=lt[:, :, :], axis=AX.X, op=ALU.add), reads=[tl], writes=[tl])
            kb.op("act", lambda: nc.scalar.activation(out=ls[:, 0:2], in_=ls[:, 0:2], func=AF.Exp), reads=[tl], writes=[tl])
            kb.op("dve", lambda: nc.vector.tensor_tensor(out=ls[:, 2:3], in0=ls[:, 1:2], in1=ls[:, 0:1], op=ALU.subtract), reads=[tl], writes=[tl])
            kb.op("dve", lambda: nc.vector.tensor_scalar_add(out=ls[:, 2:3], in0=ls[:, 2:3], scalar1=-li), reads=[tl], writes=[tl])
            qz = [[(self.sb([128, 512], BF16), T()) for _m in range(2)] for _ in range(3)]
            for sl_ in qz:
                for (qt_, tq_) in sl_:
                    kb.op("pool", lambda: nc.gpsimd.memset(qt_[:], 0.0), writes=[tq_])
            spr = self.psring(3)
            pr = self.sbring(5, [128, 512], BF16)
            O = [(self.ps([128, 512]), T()) for _ in range(2)]
            S = [(self.ps([128, 512]), T()) for _ in range(2)]
            SS = (self.ps([128, 512]), T())
            pacc = [[(self.sb([128, 512]), T()) for _ in range(2)] for _ in range(2)]
            rr = self.sbring(2, [128, 2, 512]); orr = self.sbring(2, [128, 512]); tr2 = self.sbring(2, [128, 512])
            sqr = self.sbring(2, [128, 512], BF16); rsr = self.sbring(2, [128, 512]); obr = self.sbring(3, [128, 512], BF16)
            tiles = [(CTX + i * 512, 512, 0, NKC) for i in range(SEQ // 512)]
            if with_ctx:
                tiles = [(0, CTX, 0, CTX // 128)] + tiles
            hi = 0
            for (g0, n, kc0, kc1) in tiles:
                for h in range(4):
                    par = hi % 2
                    hi += 1
                    qsl = qz[(hi - 1) % 3]
                    for m_ in range(2):
                        kb.dma("sp", qsl[m_][0][64 * m_:64 * m_ + 64, 0:n], scr["QAT"][h * 128 + 64 * m_:h * 128 + 64 * m_ + 64, g0:g0 + n], writes=[qsl[m_][1]])
                    units = [(kc, m) for kc in range(kc0, kc1) for m in range(2)]

                    def emit_score(kc, m):
                        st_, tst = spr.next()
                        kb.op("pe", lambda: nc.tensor.matmul(st_[:, 0:n], lhsT=KT[:, h, kc * 128:(kc + 1) * 128], rhs=qsl[m][0][:, 0:n], start=True, stop=True),
                              reads=[tc, qsl[m][1]], writes=[tst])
                        pt, tp = pr.next()
                        kb.op("act", lambda: nc.scalar.activation(out=pt[:, 0:n], in_=st_[:, 0:n], func=AF.Exp, scale=sc), reads=[tst], writes=[tp])
                        return pt, tp

                    def emit_pv(kc, m, pt, tp):
                        kb.op("pe", lambda: nc.tensor.matmul(O[m][0][:, 0:n], lhsT=V[:, kc, h * 128:(h + 1) * 128], rhs=pt[:, 0:n], start=(kc == kc0), stop=(kc == kc1 - 1)),
                              reads=[tp, tc], writes=[O[m][1]])
                        if m == 0:
                            kb.op("pe", lambda: nc.tensor.matmul(S[0][0][:, 0:n], lhsT=onesb[:, :], rhs=pt[:, 0:n], start=(kc == kc0), stop=(kc == kc1 - 1)),
                                  reads=[tp, tc], writes=[S[0][1]])
                        else:
                            pa, tpa = pacc[par][1]
                            if kc == kc0:
                                kb.op("dve", lambda: nc.vector.tensor_copy(out=pa[:, 0:n], in_=pt[:, 0:n]), reads=[tp], writes=[tpa])
                            else:
                                kb.op("dve", lambda: nc.vector.tensor_tensor(out=pa[:, 0:n], in0=pa[:, 0:n], in1=pt[:, 0:n], op=ALU.add), reads=[tp, tpa], writes=[tpa])
                    pend = []
                    for (kc, m) in units:
                        pend.append((kc, m) + emit_score(kc, m))
                        if len(pend) > 2:
                            emit_pv(*pend.pop(0))
                    while pend:
                        emit_pv(*pend.pop(0))
                    for m in range(1, 2):
                        pa, tpa = pacc[par][m]
                        kb.op("pe", lambda: nc.tensor.matmul(S[m][0][:, 0:n], lhsT=onesf[:, :], rhs=pa[:, 0:n], start=True, stop=True), reads=[tpa, tc], writes=[S[m][1]])
                    r_, tr_ = rr.next()
                    kb.op("dve", lambda: nc.vector.reciprocal(out=r_[:, 0, 0:n], in_=S[0][0][:, 0:n]), reads=[S[0][1]], writes=[tr_])
                    kb.op("dve", lambda: nc.vector.reciprocal(out=r_[:, 1, 0:n], in_=S[1][0][:, 0:n]), reads=[S[1][1]], writes=[tr_])
                    o, to = orr.next(); t2, tt2 = tr2.next()
                    kb.op("dve", lambda: nc.vector.tensor_tensor(out=o[:, 0:n], in0=O[0][0][:, 0:n], in1=r_[:, 0, 0:n], op=ALU.mult), reads=[O[0][1], tr_], writes=[to])
                    kb.op("dve", lambda: nc.vector.tensor_tensor(out=t2[:, 0:n], in0=O[1][0][:, 0:n], in1=r_[:, 1, 0:n], op=ALU.mult), reads=[O[1][1], tr_], writes=[tt2])
                    kb.op("dve", lambda: nc.vector.scalar_tensor_tensor(out=o[:, 0:n], in0=t2[:, 0:n], scalar=ls[:, 2:3], in1=o[:, 0:n], op0=ALU.mult, op1=ALU.add), reads=[tt2, to, tl], writes=[to])
                    sq, tsq = sqr.next()
                    kb.op("act", lambda: nc.scalar.activation(out=sq[:, 0:n], in_=o[:, 0:n], func=AF.Square), reads=[to], writes=[tsq])
                    kb.op("pe", lambda: nc.tensor.matmul(SS[0][:, 0:n], lhsT=onesb[:, :], rhs=sq[:, 0:n], start=True, stop=True), reads=[tsq, tc], writes=[SS[1]])
                    rs, trs = rsr.next()
                    kb.op("act", lambda: nc.scalar.activation(out=rs[:, 0:n], in_=SS[0][:, 0:n], func=AF.Ln, scale=1.0 / 128, bias=RMS_EPS), reads=[SS[1]], writes=[trs])
                    kb.op("act", lambda: nc.scalar.activation(out=rs[:, 0:n], in_=rs[:, 0:n], func=AF.Exp, scale=-0.5), reads=[trs], writes=[trs])
                    ob, tob = obr.next()
                    kb.op("dve", lambda: nc.vector.scalar_tensor_tensor(out=ob[:, 0:n], in0=o[:, 0:n], scalar=dng[:, 0:1], in1=rs[:, 0:n], op0=ALU.mult, op1=ALU.mult), reads=[to, trs, tc], writes=[tob])
                    kb.dma("pool", scr["CATT"][h * 128:(h + 1) * 128, g0:g0 + n], ob[:, 0:n], reads=[tob])

    def p_attn_c(self, L, with_ctx):
        nc, kb = self.nc, self.kb
        scr = self.scr
        sc = 96 ** -0.5
        with self.phase():
            tc = T()
            KT = self.sb([96, 4, NTOK], BF16)
            V = self.sb([128, NKC, 4, 65], BF16)
            for h in range(4):
                kb.dma("sp", KT[:, h, :], scr["KCT"][h, :, :], writes=[tc])
            kb.op("pool", lambda: nc.gpsimd.memset(V[:], 1.0), writes=[tc])
            vv = scr["VC"].rearrange("(k p) (h d) -> p k h d", p=128, h=4)
            for k0 in range(0, NKC, 6):
                k1 = min(NKC, k0 + 6)
                for h in range(4):
                    kb.dma("act", V[:, k0:k1, h, 0:64], vv[:, k0:k1, h, :], writes=[tc])
            identb = self.sb([128, 128], BF16)
            kb.dma("act", identb[:], self.inp["ident_bf"][:, :], writes=[tc])
            qr = self.sbring(3, [96, 512], BF16)
            spr = self.psring(3)
            pr = self.sbring(4, [128, 512], BF16)
            O = (self.ps([128, 4, 65]), T())
            tpr = self.psring(2, [128, 128], BF16)
            stage = self.sbring(2, [128, 4, 256], BF16)
            eo = self.sbring(3, [128, 128], BF16)
            sm = self.sbring(2, [128, 4], F32)
            tiles = [(CTX + i * 512, 512, 0, NKC) for i in range(SEQ // 512)]
            if with_ctx:
                tiles = [(0, CTX, 0, CTX // 128)] + tiles
            for (g0, n, kc0, kc1) in tiles:
                nj = n // 128
                sg, tsg = stage.next()
                for h in range(4):
                    qt, tq = qr.next()
                    kb.dma("sp", qt[:, 0:n], scr["QCT"][h, :, g0:g0 + n], writes=[tq])
                    first = [True]

                    def emit_score(kc):
                        st_, tst = spr.next()
                        kb.op("pe", lambda: nc.tensor.matmul(st_[:, 0:n], lhsT=KT[:, h, kc * 128:(kc + 1) * 128], rhs=qt[:, 0:n], start=True, stop=True),
                              reads=[tc, tq], writes=[tst])
                        pt, tp = pr.next()
                        kb.op("act", lambda: nc.scalar.activation(out=pt[:, 0:n], in_=st_[:, 0:n], func=AF.Exp, scale=sc), reads=[tst], writes=[tp])
                        return pt, tp

                    def emit_pv(kc, pt, tp):
                        last = (kc == kc1 - 1)
                        for j in range(nj):
                            kb.op("pe", lambda: nc.tensor.matmul(O[0][:, j, :], lhsT=pt[:, j * 128:(j + 1) * 128], rhs=V[:, kc, h, :],
                                                                 start=first[0], stop=last, skip_group_check=True), reads=[tp, tc], writes=[O[1]], inc=(j == nj - 1))
                            first[0] = False
                    pend = []
                    for kc in range(kc0, kc1):
                        pend.append((kc,) + emit_score(kc))
                        if len(pend) > 2:
                            emit_pv(*pend.pop(0))
                    while pend:
                        emit_pv(*pend.pop(0))
                    s_, ts = sm.next()
                    kb.op("dve", lambda: nc.vector.reciprocal(out=s_[:, 0:nj], in_=O[0][:, 0:nj, 64]), reads=[O[1]], writes=[ts])
                    for j in range(nj):
                        kb.op("dve", lambda: nc.vector.tensor_scalar(out=sg[:, j, h * 64:(h + 1) * 64], in0=O[0][:, j, 0:64], scalar1=s_[:, j:j + 1], scalar2=None, op0=ALU.mult),
                              reads=[O[1], ts], writes=[tsg])
                for j in range(nj):
                    for c in range(2):
                        tp_, ttp = tpr.next()
                        kb.op("pe", lambda: nc.tensor.transpose(out=tp_[:, :], in_=sg[:, j, c * 128:(c + 1) * 128], identity=identb[:]), reads=[tsg, tc], writes=[ttp])
                        oo, too = eo.next()
                        kb.op("act", lambda: nc.scalar.copy(out=oo[:], in_=tp_[:, :]), reads=[ttp], writes=[too])
                        kb.dma("pool", scr["CATT"][768 + c * 128:768 + (c + 1) * 128, g0 + j * 128:g0 + (j + 1) * 128], oo[:], reads=[too])


    def resid_ln(self, xt, tx, o_parts, gate, tgate, lng, lnb, tln, RR, dst_ap):
        nc, kb = self.nc, self.kb
        y, ty = RR["y"].next()
        for hh in range(2):
            sl = slice(hh * 512, (hh + 1) * 512)
            kb.op("dve", lambda: nc.vector.tensor_tensor(out=y[:, sl], in0=o_parts[hh][0], in1=gate[:, sl], op=ALU.mult), reads=[o_parts[hh][1], tgate], writes=[ty])
        kb.op("dve", lambda: nc.vector.scalar_tensor_tensor(out=y[:], in0=xt[:], scalar=ALPHA, in1=y[:], op0=ALU.mult, op1=ALU.add), reads=[tx, ty], writes=[ty])
        st, tst = RR["st"].next()
        kb.op("dve", lambda: nc.vector.bn_stats(out=st[:, 0:6], in_=y[:, 0:512]), reads=[ty], writes=[tst])
        kb.op("dve", lambda: nc.vector.bn_stats(out=st[:, 6:12], in_=y[:, 512:1024]), reads=[ty], writes=[tst])
        kb.op("dve", lambda: nc.vector.bn_aggr(out=st[:, 12:14], in_=st[:, 0:12]), reads=[tst], writes=[tst])
        kb.op("act", lambda: nc.scalar.activation(out=st[:, 14:15], in_=st[:, 13:14], func=AF.Ln, bias=LN_EPS), reads=[tst], writes=[tst])
        kb.op("act", lambda: nc.scalar.activation(out=st[:, 15:16], in_=st[:, 14:15], func=AF.Exp, scale=-0.5), reads=[tst], writes=[tst])
        z, tz = RR["z"].next()
        kb.op("dve", lambda: nc.vector.tensor_scalar(out=z[:], in0=y[:], scalar1=st[:, 12:13], scalar2=st[:, 15:16], op0=ALU.subtract, op1=ALU.mult), reads=[ty, tst], writes=[tz])
        kb.op("pool", lambda: nc.gpsimd.tensor_tensor(out=z[:], in0=z[:], in1=lng[:], op=ALU.mult), reads=[tz, tln], writes=[tz])
        kb.op("pool", lambda: nc.gpsimd.tensor_tensor(out=z[:], in0=z[:], in1=lnb[:], op=ALU.add), reads=[tz, tln], writes=[tz])
        kb.dma("pool", dst_ap, z[:], reads=[tz])

    def resid_rings(self, L, which):
        kb = self.kb
        RR = dict(y=self.sbring(2, [128, D]), z=self.sbring(2, [128, D]), st=self.sbring(3, [128, 16]), xt=self.sbring(2, [128, D]))
        lng = self.sb([128, D]); lnb = self.sb([128, D]); tln = T()
        kb.dma("act", lng[:], self.inp["lnp"][L, 2 * which], writes=[tln])
        kb.dma("act", lnb[:], self.inp["lnp"][L, 2 * which + 1], writes=[tln])
        RR.update(lng=lng, lnb=lnb, tln=tln)
        return RR

    def p_out_ln1(self, L, with_ctx):
        nc, kb = self.nc, self.kb
        scr = self.scr
        with self.phase():
            tc = T()
            wo = self.sb([128, 8, D], BF16)
            wv = self.inp["w_out"][L].rearrange("(k p) n -> p k n", p=128)
            for k in range(8):
                kb.dma("pool", wo[:, k, :], wv[:, k, :], writes=[tc])
            mods = self.load_mod(L, 0)
            RR = self.resid_rings(L, 0)
            ctr = self.sbring(3, [128, 8, 128], BF16)
            pp = self.psring(4)
            cv = scr["CATT"].rearrange("(k p) t -> p k t", p=128)
            for g0 in range(0 if with_ctx else CTX, NTOK, 128):
                s = 1 if g0 < CTX else 0
                ct, tct = ctr.next()
                kb.dma("sp", ct[:], cv[:, :, g0:g0 + 128], writes=[tct])
                xt, tx = RR["xt"].next()
                kb.dma("sp", xt[:], self.src_rows(L, 0, g0, 128), writes=[tx])
                parts = []
                for hh in range(2):
                    pt, tp = pp.next()
                    for k in range(8):
                        kb.op("pe", lambda: nc.tensor.matmul(pt[:, :], lhsT=ct[:, k, :], rhs=wo[:, k, hh * 512:(hh + 1) * 512], start=(k == 0), stop=(k == 7)),
                              reads=[tct, tc], writes=[tp], inc=(k == 7))
                    parts.append((pt[:, :], tp))
                self.resid_ln(xt, tx, parts, mods[s]["gb"], mods[s]["tgb"], RR["lng"], RR["lnb"], RR["tln"], RR, scr["X1"][g0:g0 + 128, :])

    def p_ffn_prep(self, L, with_ctx, moe):
        nc, kb = self.nc, self.kb
        scr = self.scr
        with self.phase():
            R = self.ln_rings()
            mods = self.load_mod(L, 1)
            tiles = ([(0, CTX, 1)] if with_ctx else []) + [(CTX + i * 512, 512, 0) for i in range(SEQ // 512)]
            if moe:
                identf = self.sb([128, 128]); tc = T()
                kb.dma("act", identf[:], self.inp["ident_f"][:, :], writes=[tc])
                rw = self.sb([128, 8, NEXP])
                kb.dma("act", rw[:], self.inp["router"].rearrange("(k p) e -> p k e", p=128), writes=[tc])
                xfr = self.sbring(2, [128, D]); hfr = self.sbring(2, [128, 8, 128])
                ptr = self.psring(1, [128, D]); plr = self.psring(2, [128, NEXP])
                gr = self.sbring(2, [128, 40]); gtr = self.sbring(2, [NEXP, 128])
                ptg = self.psring(1, [NEXP, 128])
                fmb = self.sb([128, 2, D]); tfmb = T()
                md = scr["mod_d"]
                kb.dma("act", fmb[:, 0, :], md[L, 0, 3 * D:4 * D].partition_broadcast(128), writes=[tfmb])
                kb.dma("act", fmb[:, 1, :], md[L, 0, 4 * D:5 * D].partition_broadcast(128), writes=[tfmb])
                kb.op("pool", lambda: nc.gpsimd.tensor_scalar(out=fmb[:, 1, :], in0=fmb[:, 1, :], scalar1=1.0, scalar2=None, op0=ALU.add), reads=[tfmb], writes=[tfmb])
            for (g0, n, s) in tiles:
                xm, txm = self.ln_mod_tile(L, 1, g0, n, mods[s], R)
                for k in range(8):
                    kb.dma("pool", scr["HT"][k * 128:(k + 1) * 128, g0:g0 + n], xm[:, k, 0:n], reads=[txm])
                if not moe:
                    continue
                for j in range(n // 128):
                    gg = g0 + j * 128
                    xt, tx = xfr.next()
                    kb.dma("sp", xt[:], scr["X1"][gg:gg + 128, :], writes=[tx])
                    st, tst = R["st"].next()
                    kb.op("dve", lambda: nc.vector.bn_stats(out=st[:, 0:6], in_=xt[:, 0:512]), reads=[tx], writes=[tst])
                    kb.op("dve", lambda: nc.vector.bn_stats(out=st[:, 6:12], in_=xt[:, 512:1024]), reads=[tx], writes=[tst])
                    kb.op("dve", lambda: nc.vector.bn_aggr(out=st[:, 12:14], in_=st[:, 0:12]), reads=[tst], writes=[tst])
                    kb.op("act", lambda: nc.scalar.activation(out=st[:, 14:15], in_=st[:, 13:14], func=AF.Ln, bias=LN_EPS), reads=[tst], writes=[tst])
                    kb.op("act", lambda: nc.scalar.activation(out=st[:, 15:16], in_=st[:, 14:15], func=AF.Exp, scale=-0.5), reads=[tst], writes=[tst])
                    kb.op("dve", lambda: nc.vector.tensor_scalar(out=xt[:], in0=xt[:], scalar1=st[:, 12:13], scalar2=st[:, 15:16], op0=ALU.subtract, op1=ALU.mult), reads=[tx, tst], writes=[tx])
                    kb.op("pool", lambda: nc.gpsimd.tensor_tensor(out=xt[:], in0=xt[:], in1=fmb[:, 1, :], op=ALU.mult), reads=[tx, tfmb], writes=[tx])
                    kb.op("pool", lambda: nc.gpsimd.tensor_tensor(out=xt[:], in0=xt[:], in1=fmb[:, 0, :], op=ALU.add), reads=[tx, tfmb], writes=[tx])
                    pt, tp = ptr.next()
                    for k in range(8):
                        kb.op("pe", lambda: nc.tensor.matmul(pt[:, k * 128:(k + 1) * 128], lhsT=xt[:, k * 128:(k + 1) * 128], rhs=identf[:], start=True, stop=True), reads=[tx, tc], writes=[tp], inc=(k == 7))
                    hf, thf = hfr.next()
                    kb.op("act", lambda: nc.scalar.copy(out=hf[:, 0:4, :], in_=pt[:, 0:512]), reads=[tp], writes=[thf])
                    kb.op("dve", lambda: nc.vector.tensor_copy(out=hf[:, 4:8, :], in_=pt[:, 512:1024]), reads=[tp], writes=[thf])
                    pl, tpl = plr.next()
                    for k in range(8):
                        kb.op("pe", lambda: nc.tensor.matmul(pl[:, :], lhsT=hf[:, k, :], rhs=rw[:, k, :], start=(k == 0), stop=(k == 7)), reads=[thf, tc], writes=[tpl], inc=(k == 7))
                    g, tg = gr.next()
                    kb.op("dve", lambda: nc.vector.tensor_copy(out=g[:, 0:8], in_=pl[:, :]), reads=[tpl], writes=[tg])
                    kb.op("dve", lambda: nc.vector.tensor_reduce(out=g[:, 8:9], in_=g[:, 0:8], axis=AX.X, op=ALU.max), reads=[tg], writes=[tg])
                    kb.op("dve", lambda: nc.vector.tensor_scalar(out=g[:, 10:18], in0=g[:, 0:8], scalar1=g[:, 8:9], scalar2=None, op0=ALU.is_equal), reads=[tg], writes=[tg])
                    kb.op("dve", lambda: nc.vector.scalar_tensor_tensor(out=g[:, 18:26], in0=g[:, 10:18], scalar=-1e30, in1=g[:, 0:8], op0=ALU.mult, op1=ALU.add), reads=[tg], writes=[tg])
                    kb.op("dve", lambda: nc.vector.tensor_reduce(out=g[:, 9:10], in_=g[:, 18:26], axis=AX.X, op=ALU.max), reads=[tg], writes=[tg])
                    kb.op("dve", lambda: nc.vector.tensor_scalar(out=g[:, 18:26], in0=g[:, 18:26], scalar1=g[:, 9:10], scalar2=None, op0=ALU.is_equal), reads=[tg], writes=[tg])
                    kb.op("dve", lambda: nc.vector.tensor_tensor(out=g[:, 26:27], in0=g[:, 9:10], in1=g[:, 8:9], op=ALU.subtract), reads=[tg], writes=[tg])
                    kb.op("act", lambda: nc.scalar.activation(out=g[:, 26:27], in_=g[:, 26:27], func=AF.Exp), reads=[tg], writes=[tg])
                    kb.op("dve", lambda: nc.vector.tensor_scalar_add(out=g[:, 26:27], in0=g[:, 26:27], scalar1=1.0), reads=[tg], writes=[tg])
                    kb.op("dve", lambda: nc.vector.reciprocal(out=g[:, 26:27], in_=g[:, 26:27]), reads=[tg], writes=[tg])
                    kb.op("dve", lambda: nc.vector.tensor_scalar(out=g[:, 27:28], in0=g[:, 26:27], scalar1=-1.0, scalar2=1.0, op0=ALU.mult, op1=ALU.add), reads=[tg], writes=[tg])
                    kb.op("dve", lambda: nc.vector.tensor_scalar(out=g[:, 28:36], in0=g[:, 10:18], scalar1=g[:, 26:27], scalar2=None, op0=ALU.mult), reads=[tg], writes=[tg])
                    kb.op("dve", lambda: nc.vector.scalar_tensor_tensor(out=g[:, 28:36], in0=g[:, 18:26], scalar=g[:, 27:28], in1=g[:, 28:36], op0=ALU.mult, op1=ALU.add), reads=[tg], writes=[tg])
                    pg, tpg = ptg.next()
                    kb.op("pe", lambda: nc.tensor.matmul(pg[:, :], lhsT=g[:, 28:36], rhs=identf[:], start=True, stop=True), reads=[tg, tc], writes=[tpg])
                    gt, tgt = gtr.next()
                    kb.op("act", lambda: nc.scalar.copy(out=gt[:, :], in_=pg[:, :]), reads=[tpg], writes=[tgt])
                    kb.dma("sp", scr["GT"][:, gg:gg + 128], gt[:, :], reads=[tgt])

    def p_ffn_up(self, w1_ap, w3_ap, e, tok0, moe):
        nc, kb = self.nc, self.kb
        scr = self.scr
        ntok = NTOK - tok0
        with self.phase():
            tc = T()
            hT = self.sb([128, 8, ntok], BF16)
            for k in range(8):
                kb.dma("sp" if k % 2 else "act", hT[:, k, :], scr["HT"][k * 128:(k + 1) * 128, tok0:NTOK], writes=[tc])
            if moe:
                sel = self.sb([NEXP, 128]); gT = self.sb([NEXP, ntok]); gbc = self.sb([128, ntok]); tg = T()
                kb.dma("act", gT[:], scr["GT"][:, tok0:NTOK], writes=[tg])
                kb.dma("act", sel[:], self.inp["sel8"][e], writes=[tg])
            w1r = self.sbring(2, [128, 8, 256], BF16); w3r = self.sbring(2, [128, 8, 256], BF16)
            pp = self.psring(6)
            sr = self.sbring(3, [128, 512], F32); tr_ = self.sbring(3, [128, 512], F32); ur = self.sbring(3, [128, 512], BF16)
            tiles = ([(0, CTX)] if tok0 == 0 else []) + [(CTX + i * 512, 512) for i in range(SEQ // 512)]
            if moe:
                for (g0, n) in tiles:
                    pt, tp = pp.next()
                    kb.op("pe", lambda: nc.tensor.matmul(pt[:, 0:n], lhsT=sel[:, :], rhs=gT[:, g0 - tok0:g0 - tok0 + n], start=True, stop=True), reads=[tg], writes=[tp])
                    kb.op("act", lambda: nc.scalar.copy(out=gbc[:, g0 - tok0:g0 - tok0 + n], in_=pt[:, 0:n]), reads=[tp], writes=[tg])
            w1v = w1_ap.rearrange("(k p) n -> p k n", p=128)
            w3v = w3_ap.rearrange("(k p) n -> p k n", p=128)
            wl = {}

            def wload(fp_):
                w1_, tw1_ = w1r.next(); w3_, tw3_ = w3r.next()
                kb.dma("pool", w1_[:], w1v[:, :, fp_ * 256:(fp_ + 1) * 256], writes=[tw1_])
                kb.dma("pool", w3_[:], w3v[:, :, fp_ * 256:(fp_ + 1) * 256], writes=[tw3_])
                wl[fp_] = (w1_, tw1_, w3_, tw3_)
            wload(0)
            for fp in range(NFC // 2):
                if fp + 1 < NFC // 2:
                    wload(fp + 1)
                w1, tw1, w3, tw3 = wl.pop(fp)
                for fi in range(2):
                    f = fp * 2 + fi
                    for (g0, n) in tiles:
                        c0 = g0 - tok0
                        p1, tp1 = pp.next(); p3, tp3 = pp.next()
                        for k in range(8):
                            kb.op("pe", lambda: nc.tensor.matmul(p1[:, 0:n], lhsT=w1[:, k, fi * 128:(fi + 1) * 128], rhs=hT[:, k, c0:c0 + n], start=(k == 0), stop=(k == 7)),
                                  reads=[tw1, tc], writes=[tp1], inc=(k == 7))
                        for k in range(8):
                            kb.op("pe", lambda: nc.tensor.matmul(p3[:, 0:n], lhsT=w3[:, k, fi * 128:(fi + 1) * 128], rhs=hT[:, k, c0:c0 + n], start=(k == 0), stop=(k == 7)),
                                  reads=[tw3, tc], writes=[tp3], inc=(k == 7))
                        s_, ts = sr.next()
                        kb.op("act", lambda: nc.scalar.activation(out=s_[:, 0:n], in_=p1[:, 0:n], func=AF.Silu), reads=[tp1], writes=[ts])
                        u, tu = ur.next()
                        if moe:
                            t_, tt = tr_.next()
                            kb.op("dve", lambda: nc.vector.tensor_tensor(out=t_[:, 0:n], in0=p3[:, 0:n], in1=gbc[:, c0:c0 + n], op=ALU.mult), reads=[tp3, tg], writes=[tt])
                            kb.op("dve", lambda: nc.vector.tensor_tensor(out=u[:, 0:n], in0=s_[:, 0:n], in1=t_[:, 0:n], op=ALU.mult), reads=[ts, tt], writes=[tu])
                        else:
                            kb.op("dve", lambda: nc.vector.tensor_tensor(out=u[:, 0:n], in0=p3[:, 0:n], in1=s_[:, 0:n], op=ALU.mult), reads=[tp3, ts], writes=[tu])
                        kb.dma("sp", scr["UT"][f * 128:(f + 1) * 128, g0:g0 + n], u[:, 0:n], reads=[tu])

    def p_ffn_down(self, L, w2_ap, tok0, first, last, final):
        nc, kb = self.nc, self.kb
        scr = self.scr
        with self.phase():
            tc = T()
            w2 = self.sb([128, NFC, D], BF16)
            w2v = w2_ap.rearrange("(f p) n -> p f n", p=128)
            for f0 in range(0, NFC, 4):
                kb.dma("pool", w2[:, f0:f0 + 4, :], w2v[:, f0:f0 + 4, :], writes=[tc])
            utr = self.sbring(2, [128, NFC, 512], BF16)
            pp = self.psring(4)
            uv = scr["UT"].rearrange("(f p) t -> p f t", p=128)
            if last:
                mods = self.load_mod(L, 1)
                RR = self.resid_rings(L, 1)
            accr = self.sbring(2, [128, D])
            ut = tut = None
            ubase = None
            for g0 in range(tok0, NTOK, 128):
                s = 1 if g0 < CTX else 0
                if ubase is None or g0 >= ubase + uw:
                    ubase = g0
                    uw = CTX - g0 if g0 < CTX else 512
                    ut, tut = utr.next()
                    for qi, f0 in enumerate(range(0, NFC, 7)):
                        kb.dma("act" if qi % 2 == 0 else "sp", ut[:, f0:f0 + 7, 0:uw], uv[:, f0:f0 + 7, ubase:ubase + uw], writes=[tut])
                uo = g0 - ubase
                parts = []
                for hh in range(2):
                    pt, tp = pp.next()
                    for f in range(NFC):
                        kb.op("pe", lambda: nc.tensor.matmul(pt[:, :], lhsT=ut[:, f, uo:uo + 128], rhs=w2[:, f, hh * 512:(hh + 1) * 512], start=(f == 0), stop=(f == NFC - 1)),
                              reads=[tut, tc], writes=[tp], inc=(f == NFC - 1))
                    parts.append((pt[:, :], tp))
                if not first:
                    acc, ta = accr.next()
                    kb.dma("sp", acc[:], scr["FACC"][g0:g0 + 128, :], writes=[ta])
                    for hh in range(2):
                        sl = slice(hh * 512, (hh + 1) * 512)
                        kb.op("dve", lambda: nc.vector.tensor_tensor(out=acc[:, sl], in0=parts[hh][0], in1=acc[:, sl], op=ALU.add), reads=[parts[hh][1], ta], writes=[ta])
                    parts = [(acc[:, 0:512], ta), (acc[:, 512:1024], ta)]
                if last:
                    xt, tx = RR["xt"].next()
                    kb.dma("sp", xt[:], scr["X1"][g0:g0 + 128, :], writes=[tx])
                    dst = scr["y"][g0 - CTX:g0 - CTX + 128, :] if final else scr["X2"][g0:g0 + 128, :]
                    self.resid_ln(xt, tx, parts, mods[s]["gb"], mods[s]["tgb"], RR["lng"], RR["lnb"], RR["tln"], RR, dst)
                else:
                    if first:
                        acc, ta = accr.next()
                        kb.op("act", lambda: nc.scalar.copy(out=acc[:, 0:512], in_=parts[0][0]), reads=[parts[0][1]], writes=[ta])
                        kb.op("dve", lambda: nc.vector.tensor_copy(out=acc[:, 512:1024], in_=parts[1][0]), reads=[parts[1][1]], writes=[ta])
                    kb.dma("pool", scr["FACC"][g0:g0 + 128, :], acc[:], reads=[ta])

    def ffn(self, L, with_ctx, final):
        moe = (L % 2 == 1)
        tok0 = 0 if with_ctx else CTX
        self.p_ffn_prep(L, with_ctx, moe)
        if not moe:
            self.p_ffn_up(self.inp["ff_w1"], self.inp["ff_w3"], 0, tok0, False)
            self.p_ffn_down(L, self.inp["ff_w2"], tok0, True, True, final)
        else:
            for e in range(NEXP):
                self.p_ffn_up(self.inp["moe_w1"][e], self.inp["moe_w3"][e], e, tok0, True)
                self.p_ffn_down(L, self.inp["moe_w2"][e], tok0, e == 0, e == NEXP - 1, final)


    def rwkv_gen(self, L, d):
        nc, kb = self.nc, self.kb
        scr = self.scr
        rev = (d == 1)
        CH = 64
        with contextlib.nullcontext():
            tc = T()
            def cload(name, shape, src, q="act"):
                t_ = self.sb(shape)
                kb.dma(q, t_[:], src, writes=[tc])
                return t_
            mu = cload("mu", [128, 9], self.inp["rk_mu"][L])
            w0 = cload("w0", [128, 2], self.inp["rk_w0"][L, d]); a0 = cload("a0", [128, 2], self.inp["rk_a0"][L, d])
            w2 = cload("w2", [128, 256], self.inp["rk_w2"][L]); a2 = cload("a2", [128, 256], self.inp["rk_a2"][L])
            kkv = cload("kkv", [128, 2], self.inp["rk_kk"][L]); kav = cload("kav", [128, 2], self.inp["rk_ka"][L]); rkv = cload("rkv", [128, 2], self.inp["rk_rk"][L])
            bones = cload("bones", [128, 128], self.inp["rk_bones"])
            msk = cload("msk", [64, 4, 128], self.inp["rk_msk"][d]); mskT = cload("mskT", [64, 4, 64], self.inp["rk_mskT"][d])
            id4 = cload("id4", [64, 4, 64], self.inp["rk_id4"])
            identf = cload("identf", [128, 128], self.inp["ident_f"][:, :])
            identb = self.sb([128, 128], BF16)
            kb.dma("act", identb[:], self.inp["ident_bf"][:, :], writes=[tc])
            TW = 256
            pbr = self.sbring(1, [128, 9, TW + 2])
            tmp = self.sbring(10, [128, TW])
            tmp1 = self.sbring(2, [128, TW])
            zded = [(self.sb([128, TW]), T()) for _ in range(3)]
            pps = self.psring(1)
            NB = 3
            bkr = Ring([0, 1, 2])
            bank = [(self.ps([128, 8, 64]), T(), T()) for _ in range(NB)]
            def half(i):
                b_, t0_, t1_ = bank[i // 2]
                return (b_[0:64, 0:4, :], t0_) if i % 2 == 0 else (b_[0:64, 4:8, :], t1_)
            def halfF(i):
                b_, t0_, t1_ = bank[i // 2]
                return (b_[:, 0:4, :], t0_) if i % 2 == 0 else (b_[:, 4:8, :], t1_)
            def mk(shape):
                return [(self.sb(shape), T()) for _ in range(1)]
            AR = [mk([128, 4, 2, 64]) for _ in range(2)]; BK = [mk([128, 4, 2, 64]) for _ in range(2)]
            VV = [mk([128, 4, 64]) for _ in range(2)]; GC = [mk([128, 4]) for _ in range(2)]
            ARo = [mk([64, 4, 2, 64]) for _ in range(2)]; BKo = [mk([64, 4, 2, 64]) for _ in range(2)]
            VVo = [mk([64, 4, 64]) for _ in range(2)]; GCo = [mk([64, 4]) for _ in range(2)]
            BON = [mk([128, TW]) for _ in range(2)]
            def mkb(shape):
                return [(self.sb(shape, BF16), T()) for _ in range(1)]
            ARb = [mkb([128, 4, 2, 64]) for _ in range(2)]; BKb = [mkb([128, 4, 2, 64]) for _ in range(2)]; VVb = [mkb([128, 4, 64]) for _ in range(2)]
            ARbo = [mkb([64, 4, 2, 64]) for _ in range(2)]; BKbo = [mkb([64, 4, 2, 64]) for _ in range(2)]; VVbo = [mkb([64, 4, 64]) for _ in range(2)]
            S0 = [(self.sb([64, 4, 64]), T()) for _ in range(2)]
            kb.op("pool", lambda: nc.gpsimd.memset(S0[0][0][:], 0.0), writes=[S0[0][1]])
            scur_box = [0]
            pfin = self.sbring(2, [64, 4, 64], BF16)
            tokr = self.sbring(2, [64, 12, 64], BF16); gbr = self.sbring(2, [64, 4, 128], BF16); gkr = self.sbring(2, [64, 4, 128], BF16)
            xxr = self.sbring(3, [64, 8, 64], BF16); prr = self.sbring(3, [64, 4, 64], BF16)
            wr = self.sbring(2, [64, 4, 64], BF16); ur = self.sbring(2, [64, 4, 64], BF16); yr = self.sbring(3, [64, 256])
            tiles = [(0, CTX, 0, CTX)] + [(CTX + i * TW, TW, CTX, NTOK) for i in range(SEQ // TW)]
            if rev:
                tiles = [tiles[0]] + tiles[:0:-1]
            for ti, (g0, n, s_lo, s_hi) in enumerate(tiles):
                par = 0
                nch = n // CH
                pb, tpb = pbr.next()
                lo = max(s_lo, g0 - 1); hi = min(s_hi, g0 + n + 1)
                kb.op("pool", lambda: nc.gpsimd.memset(pb[:, :, 0:1], 0.0), writes=[tpb])
                kb.op("pool", lambda: nc.gpsimd.memset(pb[:, :, n + 1:n + 2], 0.0), writes=[tpb])
                pv = scr["PBT"].rearrange("(c p) t -> p c t", p=128)
                for c0 in range(0, 9, 3):
                    kb.dma("sp", pb[:, c0:c0 + 3, lo - (g0 - 1):hi - (g0 - 1)], pv[:, c0:c0 + 3, lo:hi], writes=[tpb])

                def zshift(c, dst, tdst):
                    t1, tt1 = tmp1.next()
                    kb.op("dve", lambda: nc.vector.tensor_tensor(out=t1[:, 0:n], in0=pb[:, c, 0:n], in1=pb[:, c, 2:n + 2], op=ALU.add), reads=[tpb], writes=[tt1])
                    kb.op("dve", lambda: nc.vector.scalar_tensor_tensor(out=t1[:, 0:n], in0=t1[:, 0:n], scalar=0.5, in1=pb[:, c, 1:n + 1], op0=ALU.mult, op1=ALU.subtract), reads=[tt1, tpb], writes=[tt1])
                    kb.op("dve", lambda: nc.vector.scalar_tensor_tensor(out=dst, in0=t1[:, 0:n], scalar=mu[:, c:c + 1], in1=pb[:, c, 1:n + 1], op0=ALU.mult, op1=ALU.add), reads=[tt1, tpb, tc], writes=[tdst])

                def v3(ap_):
                    return ap_.rearrange("p (c t) -> p c t", t=CH)

                zw, tzw = zded[0]; zshift(6, zw[:, 0:n], tzw)
                kb.op("act", lambda: nc.scalar.activation(out=zw[:, 0:n], in_=zw[:, 0:n], func=AF.Tanh), reads=[tzw], writes=[tzw])
                za, tza = zded[1]; zshift(7, za[:, 0:n], tza)
                yield
                for c in range(2):
                    ar, tar = AR[c][par]; bk, tbk = BK[c][par]; vv_, tvv = VV[c][par]; gc, tgc = GC[c][par]; bon, tbon = BON[c][par]
                    p1, tp1 = pps.next()
                    kb.op("pe", lambda: nc.tensor.matmul(p1[:, 0:n], lhsT=w2[64 * d:64 * d + 64, c * 128:(c + 1) * 128], rhs=zw[64 * d:64 * d + 64, 0:n], start=True, stop=True), reads=[tzw, tc], writes=[tp1])
                    ld, tld = tmp.next()
                    kb.op("act", lambda: nc.scalar.activation(out=ld[:, 0:n], in_=p1[:, 0:n], func=AF.Sigmoid, bias=w0[:, c:c + 1]), reads=[tp1, tc], writes=[tld])
                    kb.op("pool", lambda: nc.gpsimd.tensor_scalar(out=ld[:, 0:n], in0=ld[:, 0:n], scalar1=-math.exp(-0.5), scalar2=None, op0=ALU.mult), reads=[tld], writes=[tld])
                    yield
                    p2, tp2 = pps.next()
                    kb.op("pe", lambda: nc.tensor.matmul(p2[:, 0:n], lhsT=a2[64 * d:64 * d + 64, c * 128:(c + 1) * 128], rhs=za[64 * d:64 * d + 64, 0:n], start=True, stop=True), reads=[tza, tc], writes=[tp2])
                    ac, tac = tmp.next()
                    kb.op("act", lambda: nc.scalar.activation(out=ac[:, 0:n], in_=p2[:, 0:n], func=AF.Sigmoid, bias=a0[:, c:c + 1]), reads=[tp2, tc], writes=[tac])
                    yield
                    bufs = [tmp.next(), tmp.next()]
                    src, tsrc = ld, tld
                    bi = 0
                    sh = 1
                    while sh < CH:
                        dst_, tdst_ = bufs[bi]
                        kb.op("pool", lambda: nc.gpsimd.tensor_copy(out=dst_[:, 0:n], in_=src[:, 0:n]), reads=[tsrc], writes=[tdst_])
                        if not rev:
                            kb.op("dve", lambda: nc.vector.tensor_tensor(out=v3(dst_[:, 0:n])[:, :, sh:CH], in0=v3(src[:, 0:n])[:, :, sh:CH], in1=v3(src[:, 0:n])[:, :, 0:CH - sh], op=ALU.add), reads=[tsrc, tdst_], writes=[tdst_])
                        else:
                            kb.op("dve", lambda: nc.vector.tensor_tensor(out=v3(dst_[:, 0:n])[:, :, 0:CH - sh], in0=v3(src[:, 0:n])[:, :, 0:CH - sh], in1=v3(src[:, 0:n])[:, :, sh:CH], op=ALU.add), reads=[tsrc, tdst_], writes=[tdst_])
                        src, tsrc = dst_, tdst_
                        bi = 1 - bi
                        sh *= 2
                        if sh in (4, 16):
                            yield
                    cs, tcs = src, tsrc
                    egx, tegx = bufs[bi]
                    kb.op("dve", lambda: nc.vector.tensor_tensor(out=egx[:, 0:n], in0=cs[:, 0:n], in1=ld[:, 0:n], op=ALU.subtract), reads=[tcs, tld], writes=[tegx])
                    kb.op("act", lambda: nc.scalar.activation(out=egx[:, 0:n], in_=egx[:, 0:n], func=AF.Exp), reads=[tegx], writes=[tegx])
                    egi, tegi = ld, tld
                    kb.op("act", lambda: nc.scalar.activation(out=egi[:, 0:n], in_=cs[:, 0:n], func=AF.Exp, scale=-1.0), reads=[tcs, tegx], writes=[tegi])
                    kb.op("act", lambda: nc.scalar.activation(out=cs[:, 0:n], in_=cs[:, 0:n], func=AF.Exp), reads=[tcs, tegi], writes=[tcs])
                    eg, teg = cs, tcs
                    yield
                    gsel = (CH - 1) if not rev else 0
                    kb.op("pool", lambda: nc.gpsimd.tensor_copy(out=gc[:, 0:nch], in_=v3(eg[:, 0:n])[:, :, gsel]), reads=[teg], writes=[tgc])
                    yield
                    zr, tzr = tmp.next(); zshift(0 + c, zr[:, 0:n], tzr)
                    zk, tzk = tmp.next(); zshift(2 + c, zk[:, 0:n], tzk)
                    zshift(4 + c, vv_[:, 0:nch, :].rearrange("p c t -> p (c t)"), tvv)
                    yield
                    kx, tkx = tmp.next()
                    kb.op("dve", lambda: nc.vector.tensor_scalar(out=kx[:, 0:n], in0=zk[:, 0:n], scalar1=kkv[:, c:c + 1], scalar2=None, op0=ALU.mult), reads=[tzk, tc], writes=[tkx])
                    sq, tsq = tmp.next()
                    kb.op("pool", lambda: nc.gpsimd.tensor_tensor(out=sq[:, 0:n], in0=kx[:, 0:n], in1=kx[:, 0:n], op=ALU.mult), reads=[tkx], writes=[tsq])
                    p3, tp3 = pps.next()
                    kb.op("pe", lambda: nc.tensor.matmul(p3[:, 0:n], lhsT=bones[:, :], rhs=sq[:, 0:n], start=True, stop=True), reads=[tsq, tc], writes=[tp3])
                    kb.op("act", lambda: nc.scalar.activation(out=sq[:, 0:n], in_=p3[:, 0:n], func=AF.Ln, bias=1e-24), reads=[tp3], writes=[tsq])
                    kb.op("act", lambda: nc.scalar.activation(out=sq[:, 0:n], in_=sq[:, 0:n], func=AF.Exp, scale=-0.5), reads=[tsq], writes=[tsq])
                    kb.op("dve", lambda: nc.vector.tensor_tensor(out=kx[:, 0:n], in0=kx[:, 0:n], in1=sq[:, 0:n], op=ALU.mult), reads=[tkx, tsq], writes=[tkx])
                    yield
                    kb.op("dve", lambda: nc.vector.scalar_tensor_tensor(out=ar[:, 0:nch, 0, :], in0=v3(kx[:, 0:n]), scalar=-1.0, in1=v3(egx[:, 0:n]), op0=ALU.mult, op1=ALU.mult), reads=[tkx, tegx], writes=[tar])
                    kb.op("pool", lambda: nc.gpsimd.tensor_tensor(out=sq[:, 0:n], in0=kx[:, 0:n], in1=ac[:, 0:n], op=ALU.mult), reads=[tkx, tac], writes=[tsq])
                    kb.op("dve", lambda: nc.vector.tensor_tensor(out=bk[:, 0:nch, 0, :], in0=v3(sq[:, 0:n]), in1=v3(egi[:, 0:n]), op=ALU.mult), reads=[tsq, tegi], writes=[tbk])
                    kb.op("dve", lambda: nc.vector.tensor_tensor(out=ar[:, 0:nch, 1, :], in0=v3(zr[:, 0:n]), in1=v3(eg[:, 0:n]), op=ALU.mult), reads=[tzr, teg], writes=[tar])
                    yield
                    kb.op("dve", lambda: nc.vector.tensor_scalar(out=ac[:, 0:n], in0=ac[:, 0:n], scalar1=-1.0, scalar2=kav[:, c:c + 1], op0=ALU.add, op1=ALU.mult), reads=[tac, tsq, tc], writes=[tac])
                    kb.op("dve", lambda: nc.vector.scalar_tensor_tensor(out=zk[:, 0:n], in0=ac[:, 0:n], scalar=1.0, in1=zk[:, 0:n], op0=ALU.add, op1=ALU.mult), reads=[tac, tzk, tkx], writes=[tzk])
                    kb.op("dve", lambda: nc.vector.tensor_tensor(out=bk[:, 0:nch, 1, :], in0=v3(zk[:, 0:n]), in1=v3(egi[:, 0:n]), op=ALU.mult), reads=[tzk, tegi], writes=[tbk])
                    yield
                    kb.op("dve", lambda: nc.vector.scalar_tensor_tensor(out=zr[:, 0:n], in0=zr[:, 0:n], scalar=rkv[:, c:c + 1], in1=zk[:, 0:n], op0=ALU.mult, op1=ALU.mult), reads=[tzr, tzk, tar, tc], writes=[tzr])
                    p4, tp4 = pps.next()
                    kb.op("pe", lambda: nc.tensor.matmul(p4[:, 0:n], lhsT=bones[:, :], rhs=zr[:, 0:n], start=True, stop=True), reads=[tzr, tc], writes=[tp4])
                    kb.op("dve", lambda: nc.vector.tensor_tensor(out=bon[:, 0:n], in0=p4[:, 0:n], in1=vv_[:, 0:nch, :].rearrange("p c t -> p (c t)"), op=ALU.mult), reads=[tp4, tvv], writes=[tbon])
                    yield
                    arb, tarb = ARb[c][par]; bkb, tbkb = BKb[c][par]; vvb, tvvb = VVb[c][par]
                    kb.op("pool", lambda: nc.gpsimd.tensor_copy(out=arb[:, 0:nch], in_=ar[:, 0:nch]), reads=[tar], writes=[tarb])
                    kb.op("pool", lambda: nc.gpsimd.tensor_copy(out=bkb[:, 0:nch], in_=bk[:, 0:nch]), reads=[tbk], writes=[tbkb])
                    kb.op("pool", lambda: nc.gpsimd.tensor_copy(out=vvb[:, 0:nch], in_=vv_[:, 0:nch]), reads=[tvv], writes=[tvvb])
                    kb.dma("sp", ARo[c][par][0][:, 0:nch], ar[64:128, 0:nch], reads=[tar], writes=[ARo[c][par][1]])
                    kb.dma("sp", ARbo[c][par][0][:, 0:nch], arb[64:128, 0:nch], reads=[tarb], writes=[ARbo[c][par][1]])
                    kb.dma("sp", BKbo[c][par][0][:, 0:nch], bkb[64:128, 0:nch], reads=[tbkb], writes=[BKbo[c][par][1]])
                    kb.dma("sp", VVbo[c][par][0][:, 0:nch], vvb[64:128, 0:nch], reads=[tvvb], writes=[VVbo[c][par][1]])
                    kb.dma("sp", GCo[c][par][0][:, 0:nch], gc[64:128, 0:nch], reads=[tgc], writes=[GCo[c][par][1]])

                def hd(h):
                    c = h // 2
                    if h % 2 == 0:
                        return (ARb[c][par][0][0:64], ARb[c][par][1], BKb[c][par][0][0:64], BKb[c][par][1], VVb[c][par][0][0:64], VVb[c][par][1], GC[c][par][0][0:64], GC[c][par][1], AR[c][par][0][0:64], AR[c][par][1])
                    return (ARbo[c][par][0], ARbo[c][par][1], BKbo[c][par][0], BKbo[c][par][1], VVbo[c][par][0], VVbo[c][par][1], GCo[c][par][0], GCo[c][par][1], ARo[c][par][0], ARo[c][par][1])
                H = [hd(h) for h in range(4)]
                chs = list(range(nch))
                if rev:
                    chs = chs[::-1]
                def nb():
                    i_, _t = bkr.next()
                    return bank[i_][0], bank[i_][1]

                def prep_chain(ch, out):
                    gch = g0 + ch * CH
                    tok, ttok = tokr.next()
                    B0, TB0 = nb()
                    for a_ in range(2):
                        for h in range(4):
                            ar, tar, bk, tbk, vh, tvh, gch_, tgch, a32, ta32 = H[h]
                            kb.op("pe", lambda: nc.tensor.matmul(B0[0:64, a_ * 4 + h, :], lhsT=bk[:, ch, a_, :], rhs=identb[0:64, 0:64], start=True, stop=True), reads=[tbk, tc], writes=[TB0], inc=(a_ == 1 and h == 3))
                    kb.op("act", lambda: nc.scalar.copy(out=tok[:, 0:8, :], in_=B0[0:64, :, :]), reads=[TB0], writes=[ttok])
                    B1, TB1 = nb()
                    for h in range(4):
                        ar, tar, bk, tbk, vh, tvh, gch_, tgch, a32, ta32 = H[h]
                        kb.op("pe", lambda: nc.tensor.matmul(B1[0:64, h, :], lhsT=vh[:, ch, :], rhs=identb[0:64, 0:64], start=True, stop=True), reads=[tvh, tc], writes=[TB1], inc=False)
                    for h in range(4):
                        ar, tar, bk, tbk, vh, tvh, gch_, tgch, a32, ta32 = H[h]
                        kb.op("pe", lambda: nc.tensor.matmul(B1[0:64, 4 + h, :], lhsT=ar[:, ch, 0, :], rhs=bk[:, ch, 0, :], start=True, stop=True), reads=[tar, tbk], writes=[TB1], inc=(h == 3))
                    kb.op("dve", lambda: nc.vector.tensor_copy(out=tok[:, 8:12, :], in_=B1[0:64, 0:4, :]), reads=[TB1], writes=[ttok])
                    xx, txx = xxr.next()
                    kb.op("dve", lambda: nc.vector.tensor_tensor(out=xx[:, 4:8, :], in0=B1[0:64, 4:8, :], in1=mskT[:], op=ALU.mult), reads=[TB1, tc], writes=[txx])
                    yield
                    B2, TB2 = nb()
                    for h in range(4):
                        ar, tar, bk, tbk, vh, tvh, gch_, tgch, a32, ta32 = H[h]
                        kb.op("pe", lambda: nc.tensor.matmul(B2[0:64, 2 * h:2 * h + 2, :], lhsT=bk[:, ch, 0, :], rhs=ar[:, ch, :, :], start=True, stop=True), reads=[tar, tbk], writes=[TB2], inc=(h == 3))
                    gbs, tgbs = gbr.next(); gks, tgks = gkr.next()
                    kb.op("dve", lambda: nc.vector.tensor_tensor(out=gbs[:], in0=B2[0:64].rearrange("p (h a) t -> p h (a t)", a=2), in1=msk[:], op=ALU.mult), reads=[TB2, tc], writes=[tgbs])
                    B3, TB3 = nb()
                    for h in range(4):
                        ar, tar, bk, tbk, vh, tvh, gch_, tgch, a32, ta32 = H[h]
                        kb.op("pe", lambda: nc.tensor.matmul(B3[0:64, 2 * h:2 * h + 2, :], lhsT=bk[:, ch, 1, :], rhs=ar[:, ch, :, :], start=True, stop=True), reads=[tar, tbk], writes=[TB3], inc=(h == 3))
                    kb.op("dve", lambda: nc.vector.tensor_tensor(out=gks[:], in0=B3[0:64].rearrange("p (h a) t -> p h (a t)", a=2), in1=msk[:], op=ALU.mult), reads=[TB3, tc], writes=[tgks])
                    kb.op("pool", lambda: nc.gpsimd.tensor_copy(out=xx[:, 0:4, :], in_=gbs[:, :, 0:64]), reads=[tgbs], writes=[txx])
                    pm, tpm = prr.next()
                    kb.op("pool", lambda: nc.gpsimd.tensor_tensor(out=pm[:], in0=xx[:, 0:4, :], in1=id4[:], op=ALU.add), reads=[txx, tc], writes=[tpm])
                    yield
                    def squares(lev, xx_, txx_):
                        Bq, TBq = nb()
                        if lev < 5:
                            for h in range(4):
                                kb.op("pe", lambda: nc.tensor.matmul(Bq[0:64, h, :], lhsT=xx_[:, 4 + h, :], rhs=xx_[:, h, :], start=True, stop=True), reads=[txx_], writes=[TBq], inc=False)
                        for h in range(4):
                            kb.op("pe", lambda: nc.tensor.matmul(Bq[0:64, 4 + h, :], lhsT=xx_[:, h, :], rhs=xx_[:, 4 + h, :], start=True, stop=True), reads=[txx_], writes=[TBq], inc=(h == 3))
                        return Bq, TBq

                    def evac_sq(lev, Bq, TBq):
                        xn, txn = xxr.next()
                        if lev < 5:
                            kb.op("act", lambda: nc.scalar.copy(out=xn[:, :, :], in_=Bq[0:64, :, :]), reads=[TBq], writes=[txn])
                        else:
                            kb.op("act", lambda: nc.scalar.copy(out=xn[:, 4:8, :], in_=Bq[0:64, 4:8, :]), reads=[TBq], writes=[txn])
                        return xn, txn
                    Bq, TBq = squares(1, xx, txx)
                    xx, txx = evac_sq(1, Bq, TBq)
                    yield
                    for lev in range(1, 6):
                        Bp, TBp = nb()
                        for h in range(4):
                            kb.op("pe", lambda: nc.tensor.matmul(Bp[0:64, h, :], lhsT=xx[:, 4 + h, :], rhs=pm[:, h, :], start=True, stop=True), reads=[txx, tpm], writes=[TBp], inc=(h == 3))
                        if lev < 5:
                            Bq, TBq = squares(lev + 1, xx, txx)
                        pn, tpn = prr.next() if lev < 5 else pfin.next()
                        kb.op("dve", lambda: nc.vector.tensor_tensor(out=pn[:], in0=Bp[0:64, 0:4, :], in1=pm[:], op=ALU.add), reads=[TBp, tpm], writes=[tpn])
                        pm, tpm = pn, tpn
                        if lev < 5:
                            xx, txx = evac_sq(lev + 1, Bq, TBq)
                        yield
                    out.update(tok=tok, ttok=ttok, gbs=gbs, tgbs=tgbs, gks=gks, tgks=tgks, pm=pm, tpm=tpm)

                def state_chain(ch, pr_):
                    gch = g0 + ch * CH
                    tok, ttok, gbs, tgbs, gks, tgks, pm, tpm = (pr_[k_] for k_ in ("tok", "ttok", "gbs", "tgbs", "gks", "tgks", "pm", "tpm"))
                    s0, ts0 = S0[scur_box[0]]
                    B0, TB0 = nb()
                    for h in range(4):
                        ar, tar, bk, tbk, vh, tvh, gch_, tgch, a32, ta32 = H[h]
                        kb.op("pe", lambda: nc.tensor.matmul(B0[0:64, h, :], lhsT=a32[:, ch, 0, :], rhs=s0[:, h, :], start=(h == 0), stop=False, skip_group_check=True), reads=[ta32, ts0], writes=[TB0], inc=False)
                        kb.op("pe", lambda: nc.tensor.matmul(B0[0:64, h, :], lhsT=gks[:, h, 0:64], rhs=tok[:, 8 + h, :], start=False, stop=True, skip_group_check=True), reads=[tgks, ttok], writes=[TB0], inc=(h == 3))
                    wsb, twsb = wr.next()
                    kb.op("act", lambda: nc.scalar.copy(out=wsb[:], in_=B0[0:64, 0:4, :]), reads=[TB0], writes=[twsb])
                    yield
                    B1, TB1 = nb()
                    for h in range(4):
                        kb.op("pe", lambda: nc.tensor.matmul(B1[0:64, h, :], lhsT=pm[:, h, :], rhs=wsb[:, h, :], start=True, stop=True), reads=[tpm, twsb], writes=[TB1], inc=(h == 3))
                    usb, tusb = ur.next()
                    kb.op("dve", lambda: nc.vector.tensor_copy(out=usb[:], in_=B1[0:64, 0:4, :]), reads=[TB1], writes=[tusb])
                    yield
                    B2, TB2 = nb()
                    for h in range(4):
                        kb.op("pe", lambda: nc.tensor.matmul(B2[0:64, h, :], lhsT=identf[0:64, 0:64], rhs=s0[:, h, :], start=(h == 0), stop=False, skip_group_check=True), reads=[ts0, tc], writes=[TB2], inc=False)
                        kb.op("pe", lambda: nc.tensor.matmul(B2[0:64, h, :], lhsT=tok[:, h, :], rhs=usb[:, h, :], start=False, stop=False, skip_group_check=True), reads=[ttok, tusb], writes=[TB2], inc=False)
                        kb.op("pe", lambda: nc.tensor.matmul(B2[0:64, h, :], lhsT=tok[:, 4 + h, :], rhs=tok[:, 8 + h, :], start=False, stop=True, skip_group_check=True), reads=[ttok], writes=[TB2], inc=(h == 3))
                    sn, tsn = S0[1 - scur_box[0]]
                    for h in range(4):
                        ar, tar, bk, tbk, vh, tvh, gch_, tgch, a32, ta32 = H[h]
                        kb.op("dve", lambda: nc.vector.tensor_scalar(out=sn[:, h, :], in0=B2[0:64, h, :], scalar1=gch_[:, ch:ch + 1], scalar2=None, op0=ALU.mult), reads=[TB2, tgch], writes=[tsn])
                    yield
                    B3, TB3 = nb()
                    for h in range(4):
                        ar, tar, bk, tbk, vh, tvh, gch_, tgch, a32, ta32 = H[h]
                        kb.op("pe", lambda: nc.tensor.matmul(B3[0:64, h, :], lhsT=a32[:, ch, 1, :], rhs=s0[:, h, :], start=(h == 0), stop=False, skip_group_check=True), reads=[ta32, ts0], writes=[TB3], inc=False)
                        kb.op("pe", lambda: nc.tensor.matmul(B3[0:64, h, :], lhsT=gbs[:, h, 64:128], rhs=usb[:, h, :], start=False, stop=False, skip_group_check=True), reads=[tgbs, tusb], writes=[TB3], inc=False)
                        kb.op("pe", lambda: nc.tensor.matmul(B3[0:64, h, :], lhsT=gks[:, h, 64:128], rhs=tok[:, 8 + h, :], start=False, stop=True, skip_group_check=True), reads=[tgks, ttok], writes=[TB3], inc=(h == 3))
                    scur_box[0] = 1 - scur_box[0]
                    ysb, tysb = yr.next()
                    kb.op("act", lambda: nc.scalar.copy(out=ysb[:], in_=B3[0:64, 0:4, :].rearrange("p h t -> p (h t)")), reads=[TB3], writes=[tysb])
                    B4, TB4 = nb()
                    for c in range(2):
                        kb.op("pe", lambda: nc.tensor.matmul(B4[0:64].rearrange("p h t -> p (h t)")[:, c * 128:(c + 1) * 128], lhsT=BON[c][par][0][:, ch * CH:(ch + 1) * CH], rhs=identf[:, :], start=True, stop=True),
                              reads=[BON[c][par][1], tc], writes=[TB4], inc=(c == 1))
                    kb.op("dve", lambda: nc.vector.tensor_tensor(out=ysb[:], in0=B4[0:64].rearrange("p h t -> p (h t)")[:, 0:256], in1=ysb[:], op=ALU.add), reads=[TB4, tysb], writes=[tysb])
                    kb.dma("pool", scr["YB"][d, gch:gch + CH, :], ysb[:], reads=[tysb])
                    yield


                prods = {}
                yield from prep_chain(chs[0], prods)
                for ci, ch in enumerate(chs):
                    gens_ = [state_chain(ch, prods)]
                    nxt = {}
                    if ci + 1 < len(chs):
                        gens_.append(prep_chain(chs[ci + 1], nxt))
                    alive_ = list(gens_)
                    while alive_:
                        for g_ in list(alive_):
                            try:
                                next(g_)
                            except StopIteration:
                                alive_.remove(g_)
                        yield
                    prods = nxt
    def p_rwkv_both(self, L):
        with self.phase():
            g0_ = self.rwkv_gen(L, 0)
            g1_ = self.rwkv_gen(L, 1)
            alive = [g0_]
            rounds = 0
            started1 = False
            while alive:
                for g in list(alive):
                    try:
                        next(g)
                    except StopIteration:
                        alive.remove(g)
                rounds += 1
                if not started1 and rounds >= 40:
                    alive.append(g1_)
                    started1 = True
            if not started1:
                for _ in g1_:
                    pass

    def p_rwkv_fin(self, L, with_ctx):
        nc, kb = self.nc, self.kb
        scr = self.scr
        with self.phase():
            tc = T()
            g2 = self.sb([128, 256]); lnx = self.sb([128, 2, 256]); mu8 = self.sb([128, 9]); identb = self.sb([128, 128], BF16)
            kb.dma("act", g2[:], self.inp["rk_g2"][L], writes=[tc])
            kb.dma("act", lnx[:], self.inp["rk_lnx128"][L], writes=[tc])
            kb.dma("act", mu8[:], self.inp["rk_mu"][L], writes=[tc])
            kb.dma("act", identb[:], self.inp["ident_bf"][:, :], writes=[tc])
            pgr = self.sbring(2, [128, 130]); zr_ = self.sbring(2, [128, 128]); t1r = self.sbring(2, [128, 128])
            y0r = self.sbring(2, [128, 256]); y1r = self.sbring(2, [128, 256]); str_ = self.sbring(2, [128, 40])
            fbr = self.sbring(2, [128, 256], BF16); obr = self.sbring(3, [128, 128], BF16)
            pgp = self.psring(2, [128, 256]); ptp = self.psring(2, [128, 128], BF16)
            for g0 in range(0 if with_ctx else CTX, NTOK, 128):
                s_lo, s_hi = (0, CTX) if g0 < CTX else (CTX, NTOK)
                lo = max(s_lo, g0 - 1); hi = min(s_hi, g0 + 129)
                pg, tpg = pgr.next()
                kb.op("pool", lambda: nc.gpsimd.memset(pg[:, 0:1], 0.0), writes=[tpg])
                kb.op("pool", lambda: nc.gpsimd.memset(pg[:, 129:130], 0.0), writes=[tpg])
                kb.dma("sp", pg[:, lo - (g0 - 1):hi - (g0 - 1)], scr["PBT"][1024:1152, lo:hi], writes=[tpg])
                t1, tt1 = t1r.next(); zg, tzg = zr_.next()
                kb.op("dve", lambda: nc.vector.tensor_tensor(out=t1[:], in0=pg[:, 0:128], in1=pg[:, 2:130], op=ALU.add), reads=[tpg], writes=[tt1])
                kb.op("dve", lambda: nc.vector.scalar_tensor_tensor(out=t1[:], in0=t1[:], scalar=0.5, in1=pg[:, 1:129], op0=ALU.mult, op1=ALU.subtract), reads=[tt1, tpg], writes=[tt1])
                kb.op("dve", lambda: nc.vector.scalar_tensor_tensor(out=zg[:], in0=t1[:], scalar=mu8[:, 8:9], in1=pg[:, 1:129], op0=ALU.mult, op1=ALU.add), reads=[tt1, tpg, tc], writes=[tzg])
                kb.op("act", lambda: nc.scalar.activation(out=zg[:], in_=zg[:], func=AF.Sigmoid), reads=[tzg], writes=[tzg])
                gp, tgp = pgp.next()
                kb.op("pe", lambda: nc.tensor.matmul(gp[:, :], lhsT=zg[:, :], rhs=g2[:, :], start=True, stop=True), reads=[tzg, tc], writes=[tgp])
                y0, ty0 = y0r.next(); y1, ty1 = y1r.next()
                kb.dma("sp", y0[:], scr["YB"][0, g0:g0 + 128, :], writes=[ty0])
                kb.dma("act", y1[:], scr["YB"][1, g0:g0 + 128, :], writes=[ty1])
                kb.op("pool", lambda: nc.gpsimd.tensor_tensor(out=y0[:], in0=y0[:], in1=y1[:], op=ALU.add), reads=[ty0, ty1], writes=[ty0])
                st, tst = str_.next()
                for h in range(4):
                    kb.op("dve", lambda: nc.vector.bn_stats(out=st[:, h * 6:(h + 1) * 6], in_=y0[:, h * 64:(h + 1) * 64]), reads=[ty0], writes=[tst])
                for h in range(4):
                    kb.op("dve", lambda: nc.vector.bn_aggr(out=st[:, 24 + 2 * h:26 + 2 * h], in_=st[:, h * 6:(h + 1) * 6]), reads=[tst], writes=[tst])
                sv = st[:, 24:32].rearrange("p (h a) -> p h a", a=2)
                kb.op("act", lambda: nc.scalar.activation(out=st[:, 32:36], in_=sv[:, :, 1], func=AF.Ln, bias=GN_EPS), reads=[tst], writes=[tst])
                kb.op("act", lambda: nc.scalar.activation(out=st[:, 32:36], in_=st[:, 32:36], func=AF.Exp, scale=-0.5), reads=[tst], writes=[tst])
                for h in range(4):
                    kb.op("dve", lambda: nc.vector.tensor_scalar(out=y0[:, h * 64:(h + 1) * 64], in0=y0[:, h * 64:(h + 1) * 64], scalar1=st[:, 24 + 2 * h:25 + 2 * h], scalar2=st[:, 32 + h:33 + h], op0=ALU.subtract, op1=ALU.mult),
                          reads=[ty0, tst], writes=[ty0])
                kb.op("pool", lambda: nc.gpsimd.tensor_tensor(out=y0[:], in0=y0[:], in1=lnx[:, 0, :], op=ALU.mult), reads=[ty0, tc], writes=[ty0])
                kb.op("pool", lambda: nc.gpsimd.tensor_tensor(out=y0[:], in0=y0[:], in1=lnx[:, 1, :], op=ALU.add), reads=[ty0, tc], writes=[ty0])
                fb, tfb = fbr.next()
                kb.op("dve", lambda: nc.vector.tensor_tensor(out=fb[:], in0=gp[:, :], in1=y0[:], op=ALU.mult), reads=[tgp, ty0], writes=[tfb])
                for c in range(2):
                    tp_, ttp = ptp.next()
                    kb.op("pe", lambda: nc.tensor.transpose(out=tp_[:, :], in_=fb[:, c * 128:(c + 1) * 128], identity=identb[:]), reads=[tfb, tc], writes=[ttp])
                    ob, tob = obr.next()
                    kb.op("act", lambda: nc.scalar.copy(out=ob[:], in_=tp_[:, :]), reads=[ttp], writes=[tob])
                    kb.dma("pool", scr["CATT"][512 + c * 128:512 + (c + 1) * 128, g0:g0 + 128], ob[:], reads=[tob])


def _rope_tables(dim):
    q = dim // 4
    inv = (10000.0 ** (-np.arange(q, dtype=np.float32) / q)).astype(np.float32)
    t = np.arange(SEQ)
    rows = (t // 64).astype(np.float32)
    cols = (t % 64).astype(np.float32)
    cos = np.zeros((dim, SEQ), np.float32)
    sin = np.zeros((dim, SEQ), np.float32)
    perm = np.zeros(dim, np.int64)
    for d in range(dim):
        half = d // (dim // 2)
        within = d % (dim // 2)
        part = within // q
        f = within % q
        ang = ((rows if half == 0 else cols) * inv[f]).astype(np.float32)
        cos[d] = np.cos(ang)
        sin[d] = -np.sin(ang) if part == 0 else np.sin(ang)
        perm[d] = d + q if part == 0 else d - q
    return cos, sin, perm


def prep_inputs(inp):
    f32 = np.float32
    cosA, sinA, permA = _rope_tables(64)
    cosC, sinC, permC = _rope_tables(32)
    shared = {}
    w_in = inp["w_in"]
    ext = np.zeros((DEPTH, D, NCOL), f32)
    pa = np.concatenate([m * 64 + permA for m in range(8)])
    ext[:, :, OQA:OQA + 512] = w_in[:, :, 0:512]
    ext[:, :, OQAP:OQAP + 512] = w_in[:, :, 0:512][:, :, pa]
    ext[:, :, OKA:OKA + 512] = w_in[:, :, 512:1024]
    ext[:, :, OKAP:OKAP + 512] = w_in[:, :, 512:1024][:, :, pa]
    ext[:, :, OVA:OVA + 512] = w_in[:, :, 1024:1536]
    ext[:, :, OB:OB + 1152] = w_in[:, :, 1536:2688]
    ext[:, :, OCQ:OCQ + 256] = w_in[:, :, 2688:2944]
    ext[:, :, OCKV:OCKV + 128] = w_in[:, :, 2944:3072]
    ext[:, :, OKPE:OKPE + 64] = w_in[:, :, 2944:3008]
    ext[:, :, OKPE + 64:OKPE + 96] = w_in[:, :, 3072:3104]
    ext[:, :, OKPEP:OKPEP + 64] = w_in[:, :, 2944:3008]
    ext[:, :, OKPEP + 64:OKPEP + 96] = w_in[:, :, 3072:3104][:, :, permC]
    shared["w_in_ext"] = ext
    shared["ada_w"] = inp["ada_w"]
    shared["ada_b2"] = np.ascontiguousarray(np.repeat(inp["ada_b"][:, None, :], 2, axis=1))
    shared["ident_bf"] = np.eye(128, dtype=f32).astype(ml_dtypes.bfloat16)
    shared["ident_f"] = np.eye(128, dtype=f32)
    shared["cosA"] = np.concatenate([cosA, cosA], 0)
    shared["sinA"] = np.concatenate([sinA, sinA], 0)
    cC = np.zeros((96, SEQ), f32); sC = np.zeros((96, SEQ), f32)
    cC[64:96] = cosC; sC[64:96] = sinC
    shared["cosC"] = cC; shared["sinC"] = sC
    wuq = inp["w_uq"]
    pc = np.concatenate([np.concatenate([h * 96 + np.arange(64), h * 96 + 64 + permC]) for h in range(4)])
    shared["wuq"] = wuq
    shared["wuqp"] = np.ascontiguousarray(wuq[:, :, pc])
    shared["wukv"] = inp["w_ukv"]
    vc = np.concatenate([h * 128 + 64 + np.arange(64) for h in range(4)])
    shared["wukv_v"] = np.ascontiguousarray(inp["w_ukv"][:, :, vc])
    shared["qng"] = np.ascontiguousarray(inp["q_norm_g"].reshape(DEPTH, 2, 128).transpose(0, 2, 1))
    shared["kvng"] = np.ascontiguousarray(inp["kv_norm_g"].reshape(DEPTH, 128, 1))
    lamv = np.stack([inp["lam_q1"], inp["lam_k1"], inp["lam_q2"], inp["lam_k2"]], 1)
    shared["lamv"] = np.ascontiguousarray(np.broadcast_to(lamv[:, None], (DEPTH, 128, 4, 64)))
    shared["dngc"] = np.ascontiguousarray(inp["diff_norm_g"].reshape(DEPTH, 128, 1))
    shared["w_out"] = inp["w_out"]
    fm2 = lambda a: np.ascontiguousarray(a.reshape(a.shape[:-1] + (2, 128)).swapaxes(-1, -2))
    shared["rk_mu"] = np.ascontiguousarray(inp["shift_mu"].reshape(DEPTH, 9, 128).transpose(0, 2, 1))
    shared["rk_w0"] = fm2(inp["w0"]); shared["rk_a0"] = fm2(inp["a0"])
    shared["rk_w2"] = np.ascontiguousarray(inp["w2"].reshape(DEPTH, 128, 256)); shared["rk_a2"] = np.ascontiguousarray(inp["a2"].reshape(DEPTH, 128, 256))
    shared["rk_g2"] = inp["g2"]
    shared["rk_kk"] = fm2(inp["k_k"]); shared["rk_ka"] = fm2(inp["k_a"]); shared["rk_rk"] = fm2(inp["r_k"].reshape(DEPTH, 256))
    lnx = np.stack([inp["lnx_g"], inp["lnx_b"]], 1)
    shared["rk_lnx128"] = np.ascontiguousarray(np.broadcast_to(lnx[:, None], (DEPTH, 128, 2, 256)))
    bo = np.zeros((128, 128), f32); bo[:64, :64] = 1; bo[64:, 64:] = 1
    shared["rk_bones"] = bo
    ii = np.arange(64)
    msk = np.zeros((2, 64, 4, 128), f32); mskT = np.zeros((2, 64, 4, 64), f32)
    msk[0, :, :, 0:64] = (ii[None, :] > ii[:, None])[:, None, :]; msk[0, :, :, 64:] = (ii[None, :] >= ii[:, None])[:, None, :]
    msk[1, :, :, 0:64] = (ii[None, :] < ii[:, None])[:, None, :]; msk[1, :, :, 64:] = (ii[None, :] <= ii[:, None])[:, None, :]
    mskT[0] = (ii[None, :] < ii[:, None])[:, None, :]; mskT[1] = (ii[None, :] > ii[:, None])[:, None, :]
    shared["rk_msk"] = msk; shared["rk_mskT"] = mskT
    shared["rk_id4"] = np.ascontiguousarray(np.broadcast_to(np.eye(64, dtype=f32)[:, None, :], (64, 4, 64)))
    sel8 = np.zeros((NEXP, NEXP, 128), f32)
    for e in range(NEXP):
        sel8[e, e, :] = 1.0
    shared["sel8"] = sel8
    lnp = np.stack([inp["ln1_g"], inp["ln1_b"], inp["ln2_g"], inp["ln2_b"]], 1)
    shared["lnp"] = np.ascontiguousarray(np.broadcast_to(lnp[:, :, None, :], (DEPTH, 4, 128, D)))
    shared["ff_w1"] = inp["ff_w1"][0]; shared["ff_w3"] = inp["ff_w3"][0]; shared["ff_w2"] = inp["ff_w2"][0]
    shared["router"] = inp["router"][0]
    shared["moe_w1"] = inp["moe_w1"][0]; shared["moe_w3"] = inp["moe_w3"][0]; shared["moe_w2"] = inp["moe_w2"][0]
    maps = []
    for b in range(8):
        m = dict(shared)
        m["x"] = inp["x"][b]
        m["ctx"] = inp["ctx"][b]
        cc = np.stack([inp["c"][b].reshape(8, 128).T, inp["c_ctx"].reshape(8, 128).T], -1)
        m["cc"] = np.ascontiguousarray(cc.astype(f32))
        maps.append(m)
    return maps


_CACHE = {}


def build_full():
    P = Prog()
    P.declare()
    P.p_adaln()
    for L in range(DEPTH):
        with_ctx = (L < DEPTH - 1)
        P.p_proj(L, with_ctx)
        P.p_attn_a(L, with_ctx)
        P.p_attn_c(L, with_ctx)
        P.p_rwkv_both(L)
        P.p_rwkv_fin(L, with_ctx)
        P.p_out_ln1(L, with_ctx)
        P.ffn(L, with_ctx, L == DEPTH - 1)
    return P


def kernel(**inputs):
    inp = {k: np.asarray(v) for k, v in inputs.items()}
    P = build_full()
    maps = prep_inputs(inp)
    maps = [{k: np.ascontiguousarray(v) for k, v in m.items() if k in P.inp} for m in maps]
    res = run_bass_kernel_spmd(P.nc, maps, core_ids=list(range(8)))
    out = np.stack([np.asarray(res.results[b]["y"], dtype=np.float32) for b in range(8)], 0)
    return out
```

```python
import math
import contextlib
import numpy as np
import ml_dtypes
import concourse.bass as bass
import concourse.mybir as mybir
from concourse.bass_utils import run_bass_kernel_spmd

F32 = mybir.dt.float32
BF16 = mybir.dt.bfloat16
AF = mybir.ActivationFunctionType
ALU = mybir.AluOpType
AX = mybir.AxisListType
NDS = 8
SEM_ROT = 20000

D = 1024
SEQ = 4096
CTX = 256
NTOK = SEQ + CTX
NKC = NTOK // 128
DEPTH = 2
DFF = 3584
NFC = DFF // 128
NEXP = 8
ALPHA = (2.0 * DEPTH) ** 0.25
LN_EPS = 1e-6
RMS_EPS = 1e-6
GN_EPS = 64e-5
OQA, OQAP, OKA, OKAP, OVA, OB, OCQ, OCKV, OKPE, OKPEP, NCOL = 0, 512, 1024, 1536, 2048, 2560, 3712, 3968, 4096, 4192, 4288


def lam_init(layer):
    return 0.8 - 0.6 * math.exp(-0.3 * layer)


class T:
    __slots__ = ("w", "r")

    def __init__(self):
        self.w = None
        self.r = {}


class KB:
    def __init__(self, nc, same_eng_wait=True):
        self.nc = nc
        self.same = same_eng_wait
        self.eng = dict(pe=nc.tensor, act=nc.scalar, dve=nc.vector, pool=nc.gpsimd, sp=nc.sync)
        self.sems = []
        self.csem = {}
        self.ccnt = {}
        self.waited = {e: {} for e in self.eng}
        self.pend = {e: [] for e in self.eng}
        for e in ("pe", "act", "dve", "pool"):
            self._new_csem(e)
        self.dq = {}
        for q in ("sp", "act", "pool"):
            ids = []
            for i in range(NDS):
                self.sems.append(nc.alloc_semaphore(f"dq_{q}{i}"))
                ids.append(len(self.sems) - 1)
            self.dq[q] = dict(ids=ids, use=[0] * NDS, rr=0)
        self.nins = 0
        self._uid = 0
        self.tot = {}
        self.plog = []

    def uid(self, p="t"):
        self._uid += 1
        return f"{p}{self._uid}"

    def _new_csem(self, e):
        self.sems.append(self.nc.alloc_semaphore(f"cs_{e}{len(self.sems)}"))
        self.csem[e] = len(self.sems) - 1
        self.ccnt[e] = 0

    def wait(self, e, ev):
        s, v = ev
        if self.waited[e].get(s, 0) >= v:
            return
        self.eng[e].wait_ge(self.sems[s], v)
        self.nins += 1
        self.waited[e][s] = v

    def _deps(self, reads, writes):
        deps = []
        for t in reads:
            if t.w is not None:
                deps.append(t.w)
        for t in writes:
            if t.w is not None:
                deps.append(t.w)
            deps.extend((s_, v_, e_) for s_, (v_, e_) in t.r.items())
        return deps

    def op(self, e, fn, reads=(), writes=(), inc=True):
        for ev in self._deps(reads, writes):
            if ev[2] == e and (e == "pe" or not self.same):
                continue
            self.wait(e, ev[:2])
        ins = fn()
        self.nins += 1
        self.pend[e].append((reads, writes))
        if inc:
            if self.ccnt[e] >= SEM_ROT:
                self._new_csem(e)
            self.ccnt[e] += 1
            self.tot[e] = self.tot.get(e, 0) + 1
            s = self.csem[e]
            ins.then_inc(self.sems[s], 1)
            ev = (s, self.ccnt[e], e)
            for (rs, ws) in self.pend[e]:
                for t in rs:
                    t.r[s] = (ev[1], e)
                for t in ws:
                    t.w = ev
                    t.r = {}
            self.pend[e] = []
        return ins

    def dma(self, q, out, in_, reads=(), writes=(), **kw):
        d = self.dq[q]
        k = d["rr"]
        d["rr"] = (k + 1) % NDS
        s = d["ids"][k]
        j = d["use"][k]
        if j > 0:
            self.wait(q, (s, 16 * j))
        for ev in self._deps(reads, writes):
            self.wait(q, ev[:2])
        ins = self.eng[q].dma_start(out=out, in_=in_, **kw)
        self.nins += 1
        ins.then_inc(self.sems[s], 16)
        d["use"][k] = j + 1
        ev = (s, 16 * (j + 1), "dma_" + q)
        for t in reads:
            t.r[s] = (ev[1], ev[2])
        for t in writes:
            t.w = ev
            t.r = {}
        return ins

    def barrier(self, engines=("pe", "act", "dve", "pool", "sp")):
        evs = []
        for e in ("pe", "act", "dve", "pool"):
            assert not self.pend[e], f"pending non-inc ops on {e}"
            if self.ccnt[e] > 0:
                evs.append((self.csem[e], self.ccnt[e]))
        for q, d in self.dq.items():
            for s, u in zip(d["ids"], d["use"]):
                if u > 0:
                    evs.append((s, 16 * u))
        for e in engines:
            for ev in evs:
                self.wait(e, ev)


class Ring:
    def __init__(self, items):
        self.items = [(it, T()) for it in items]
        self.i = 0

    def next(self):
        it = self.items[self.i]
        self.i = (self.i + 1) % len(self.items)
        return it


class Prog:
    def __init__(self, debug=()):
        self.debug = set(debug)
        nc = self.nc = bass.Bass("TRN2", target_bir_lowering=False)
        self.kb = KB(nc)
        self.inp = {}
        self.scr = {}
        self.tk = {}

    def din(self, name, shape, dt=F32):
        self.inp[name] = self.nc.dram_tensor(name, list(shape), dt, kind="ExternalInput").ap()
        self.tk[name] = T()
        return self.inp[name]

    def dscr(self, name, shape, dt=F32, out=False):
        kind = "ExternalOutput" if (out or name in self.debug) else "Internal"
        self.scr[name] = self.nc.dram_tensor(name, list(shape), dt, kind=kind).ap()
        self.tk[name] = T()
        return self.scr[name]

    @contextlib.contextmanager
    def phase(self, name=""):
        st = contextlib.ExitStack()
        self._st = st
        try:
            yield st
            self.kb.barrier()
            import sys as _sys
            self.kb.plog.append((_sys._getframe(2).f_code.co_name, dict(self.kb.tot)))
        finally:
            st.close()

    def sb(self, shape, dt=F32, name=None):
        return self._st.enter_context(self.nc.sbuf_tensor(self.kb.uid(name or "sb"), list(shape), dt))

    def ps(self, shape, dt=F32, name=None):
        return self._st.enter_context(self.nc.psum_tensor(self.kb.uid(name or "ps"), list(shape), dt))

    def sbring(self, n, shape, dt=F32, name=None):
        return Ring([self.sb(shape, dt, name) for _ in range(n)])

    def psring(self, n, shape=(128, 512), dt=F32, name=None):
        return Ring([self.ps(shape, dt, name) for _ in range(n)])

    def declare(self, big=True):
        din, dscr = self.din, self.dscr
        din("x", [SEQ, D]); din("ctx", [CTX, D]); din("cc", [128, 8, 2])
        din("ada_w", [DEPTH, D, 6 * D]); din("ada_b2", [DEPTH, 2, 6 * D])
        din("w_in_ext", [DEPTH, D, NCOL])
        din("ident_bf", [128, 128], BF16); din("ident_f", [128, 128])
        din("cosA", [128, SEQ]); din("sinA", [128, SEQ]); din("cosC", [96, SEQ]); din("sinC", [96, SEQ])
        din("wuq", [DEPTH, 256, 384]); din("wuqp", [DEPTH, 256, 384]); din("wukv", [DEPTH, 128, 512]); din("wukv_v", [DEPTH, 128, 256])
        din("qng", [DEPTH, 128, 2]); din("kvng", [DEPTH, 128, 1])
        din("lamv", [DEPTH, 128, 4, 64]); din("dngc", [DEPTH, 128, 1])
        din("w_out", [DEPTH, D, D])
        din("lnp", [DEPTH, 4, 128, D])
        if big:
            din("ff_w1", [D, DFF]); din("ff_w3", [D, DFF]); din("ff_w2", [DFF, D])
            din("router", [D, NEXP]); din("moe_w1", [NEXP, D, DFF]); din("moe_w3", [NEXP, D, DFF]); din("moe_w2", [NEXP, DFF, D])
        dscr("mod_d", [DEPTH, 2, 6 * D])
        dscr("QAT", [512, NTOK], BF16); dscr("KAT", [512, NTOK], BF16); dscr("VA", [NTOK, 512], BF16)
        dscr("PBT", [1152, NTOK])
        dscr("QCT", [4, 96, NTOK], BF16); dscr("KCT", [4, 96, NTOK], BF16); dscr("VC", [NTOK, 256], BF16)
        dscr("CATT", [D, NTOK], BF16)
        dscr("X1", [NTOK, D]); dscr("X2", [NTOK, D])
        dscr("UT", [DFF, NTOK], BF16)
        dscr("FACC", [NTOK, D])
        dscr("HT", [D, NTOK], BF16); dscr("GT", [NEXP, NTOK]); dscr("YB", [2, NTOK, 256])
        din("rk_mu", [DEPTH, 128, 9]); din("rk_w0", [DEPTH, 2, 128, 2]); din("rk_a0", [DEPTH, 2, 128, 2])
        din("rk_w2", [DEPTH, 128, 256]); din("rk_a2", [DEPTH, 128, 256]); din("rk_g2", [DEPTH, 128, 256])
        din("rk_kk", [DEPTH, 128, 2]); din("rk_ka", [DEPTH, 128, 2]); din("rk_rk", [DEPTH, 128, 2])
        din("rk_lnx128", [DEPTH, 128, 2, 256]); din("rk_bones", [128, 128])
        din("rk_msk", [2, 64, 4, 128]); din("rk_mskT", [2, 64, 4, 64]); din("rk_id4", [64, 4, 64])
        din("sel8", [NEXP, NEXP, 128])
        dscr("y", [SEQ, D], out=True)

    def src_rows(self, L, stage, g0, n):
        if stage == 1:
            return self.scr["X1"][g0:g0 + n, :]
        if L == 0:
            if g0 < CTX:
                return self.inp["ctx"][g0:g0 + n, :]
            return self.inp["x"][g0 - CTX:g0 - CTX + n, :]
        return self.scr["X2"][g0:g0 + n, :]

    def p_adaln(self):
        nc, kb = self.nc, self.kb
        with self.phase():
            cc = self.sb([128, 8, 2]); tcc = T()
            cond = self.sb([128, 8, 2]); tcond = T()
            kb.dma("sp", cc[:], self.inp["cc"][:, :, :], writes=[tcc])
            kb.op("act", lambda: nc.scalar.activation(out=cond[:], in_=cc[:], func=AF.Silu), reads=[tcc], writes=[tcond])
            wring = self.sbring(2, [128, 8, 512])
            pring = self.psring(2, [2, 512])
            for L in range(DEPTH):
                brow = self.sb([2, 6 * D]); tb = T()
                mrow = self.sb([2, 6 * D]); tm = T()
                kb.dma("act", brow[:], self.inp["ada_b2"][L, :, :], writes=[tb])
                wv = self.inp["ada_w"][L].rearrange("(k p) n -> p k n", p=128)
                for n in range(12):
                    wt, tw = wring.next()
                    kb.dma("sp", wt[:], wv[:, :, n * 512:(n + 1) * 512], writes=[tw])
                    pt, tp = pring.next()
                    for k in range(8):
                        kb.op("pe", lambda k=k: nc.tensor.matmul(pt[:, :], lhsT=cond[:, k, :], rhs=wt[:, k, :], start=(k == 0), stop=(k == 7)),
                              reads=[tcond, tw], writes=[tp], inc=(k == 7))
                    kb.op("dve", lambda: nc.vector.tensor_tensor(out=mrow[:, n * 512:(n + 1) * 512], in0=pt[:, :], in1=brow[:, n * 512:(n + 1) * 512], op=ALU.add),
                          reads=[tp, tb], writes=[tm])
                kb.dma("sp", self.scr["mod_d"][L, :, :], mrow[:], reads=[tm])

    def load_mod(self, L, stage):
        nc, kb = self.nc, self.kb
        md = self.scr["mod_d"]
        res = {}
        for s in range(2):
            fm = self.sb([128, 16]); tfm = T()
            gb = self.sb([128, D]); tgb = T()
            base = stage * 3 * D
            for j in range(2):
                v = md[L, s, base + j * D: base + (j + 1) * D].rearrange("(k p) -> p k", p=128)
                kb.dma("sp", fm[:, j * 8:(j + 1) * 8], v, writes=[tfm], allow_slow_non_contiguous=True)
            kb.op("dve", lambda fm=fm: nc.vector.tensor_scalar_add(out=fm[:, 8:16], in0=fm[:, 8:16], scalar1=1.0), reads=[tfm], writes=[tfm])
            kb.dma("act", gb[:], md[L, s, base + 2 * D: base + 3 * D].partition_broadcast(128), writes=[tgb])
            res[s] = dict(fm=fm, tfm=tfm, gb=gb, tgb=tgb)
        return res

    def ln_mod_tile(self, L, stage, g0, ntok, mod, R):
        xm, txm = R["xm"].next()
        for _ in self.ln_mod_gen(L, stage, g0, ntok, mod, R, xm, txm):
            pass
        return xm, txm

    def ln_mod_gen(self, L, stage, g0, ntok, mod, R, xm, txm):
        nc, kb = self.nc, self.kb
        fm, tfm = mod["fm"], mod["tfm"]
        for j in range(ntok // 128):
            xt, tx = R["xt"].next()
            kb.dma("sp", xt[:], self.src_rows(L, stage, g0 + j * 128, 128), writes=[tx])
            st, tst = R["st"].next()
            kb.op("dve", lambda: nc.vector.bn_stats(out=st[:, 0:6], in_=xt[:, 0:512]), reads=[tx], writes=[tst])
            kb.op("dve", lambda: nc.vector.bn_stats(out=st[:, 6:12], in_=xt[:, 512:1024]), reads=[tx], writes=[tst])
            kb.op("dve", lambda: nc.vector.bn_aggr(out=st[:, 12:14], in_=st[:, 0:12]), reads=[tst], writes=[tst])
            kb.op("act", lambda: nc.scalar.activation(out=st[:, 14:15], in_=st[:, 13:14], func=AF.Ln, bias=LN_EPS), reads=[tst], writes=[tst])
            kb.op("act", lambda: nc.scalar.activation(out=st[:, 15:16], in_=st[:, 14:15], func=AF.Exp, scale=-0.5), reads=[tst], writes=[tst])
            xn, txn = R["xn"].next()
            kb.op("dve", lambda: nc.vector.tensor_scalar(out=xn[:], in0=xt[:], scalar1=st[:, 12:13], scalar2=st[:, 15:16], op0=ALU.subtract, op1=ALU.mult),
                  reads=[tx, tst], writes=[txn])
            pT, tpT = R["pT"].next()
            for k in range(8):
                kb.op("pe", lambda: nc.tensor.transpose(out=pT[:, k * 128:(k + 1) * 128], in_=xn[:, k * 128:(k + 1) * 128], identity=R["identb"][:]),
                      reads=[txn, R["tconst"]], writes=[tpT], inc=(k == 7))
            for k in range(8):
                if k % 2 == 0:
                    kb.op("act", lambda: nc.scalar.activation(out=xm[:, k, j * 128:(j + 1) * 128], in_=pT[:, k * 128:(k + 1) * 128], func=AF.Identity,
                                                              scale=fm[:, 8 + k:9 + k], bias=fm[:, k:k + 1]), reads=[tpT, tfm], writes=[txm])
                else:
                    kb.op("dve", lambda: nc.vector.tensor_scalar(out=xm[:, k, j * 128:(j + 1) * 128], in0=pT[:, k * 128:(k + 1) * 128],
                                                                 scalar1=fm[:, 8 + k:9 + k], scalar2=fm[:, k:k + 1], op0=ALU.mult, op1=ALU.add),
                          reads=[tpT, tfm], writes=[txm])
            yield

    def ln_rings(self):
        nc, kb = self.nc, self.kb
        R = dict(xt=self.sbring(2, [128, D]), st=self.sbring(3, [128, 16]), xn=self.sbring(2, [128, D], BF16),
                 pT=self.psring(2, [128, D], BF16), xm=self.sbring(2, [128, 8, 512], BF16))
        R["identb"] = self.sb([128, 128], BF16)
        R["tconst"] = T()
        kb.dma("act", R["identb"][:], self.inp["ident_bf"][:, :], writes=[R["tconst"]])
        return R

    def rms_bc(self, pss, n, width, out_t, tout):
        nc, kb = self.nc, self.kb
        ps_t, tps = pss
        kb.op("act", lambda: nc.scalar.activation(out=out_t[:, 0:n], in_=ps_t[:, 0:n], func=AF.Ln, scale=1.0 / width, bias=RMS_EPS), reads=[tps], writes=[tout])
        kb.op("act", lambda: nc.scalar.activation(out=out_t[:, 0:n], in_=out_t[:, 0:n], func=AF.Exp, scale=-0.5), reads=[tout], writes=[tout])

    def p_proj(self, L, with_ctx_q):
        nc, kb = self.nc, self.kb
        scr = self.scr
        with self.phase():
            R = self.ln_rings()
            tc = R["tconst"]
            wext = self.sb([128, 8, NCOL], BF16); twk = [T() for _ in range(8)]
            wv = self.inp["w_in_ext"][L].rearrange("(k p) n -> p k n", p=128)
            for k in range(8):
                kb.dma("pool", wext[:, k, :], wv[:, k, :], writes=[twk[k]])
            wuq = self.sb([128, 2, 384], BF16); wuqp = self.sb([128, 2, 384], BF16)
            wukv = self.sb([128, 512], BF16); wukvv = self.sb([128, 256], BF16)
            kb.dma("pool", wuq[:], self.inp["wuq"][L].rearrange("(k p) n -> p k n", p=128), writes=[tc])
            kb.dma("pool", wuqp[:], self.inp["wuqp"][L].rearrange("(k p) n -> p k n", p=128), writes=[tc])
            kb.dma("pool", wukv[:], self.inp["wukv"][L], writes=[tc])
            kb.dma("pool", wukvv[:], self.inp["wukv_v"][L], writes=[tc])
            qng = self.sb([128, 2]); kvng = self.sb([128, 1])
            kb.dma("act", qng[:], self.inp["qng"][L], writes=[tc])
            kb.dma("act", kvng[:], self.inp["kvng"][L], writes=[tc])
            onesb = self.sb([128, 128], BF16)
            kb.op("pool", lambda: nc.gpsimd.memset(onesb[:], 1.0), writes=[tc])
            mods = self.load_mod(L, 0)
            pp = self.psring(4)
            sgb = self.sbring(6, [128, 512], BF16)
            sgf = self.sbring(6, [128, 512], F32)
            rcA = self.sbring(2, [128, 2, 512], F32)
            rcC = self.sbring(2, [96, 2, 512], F32)
            ded = [(self.sb([128, 512], BF16), T()) for _ in range(4)]
            tiles = [(0, CTX, 1)] + [(CTX + i * 512, 512, 0) for i in range(SEQ // 512)]
            evi = [0]

            def evac_copy(dst_ap, src_ap, reads, writes):
                evi[0] += 1
                if evi[0] % 2:
                    kb.op("act", lambda: nc.scalar.copy(out=dst_ap, in_=src_ap), reads=reads, writes=writes)
                else:
                    kb.op("dve", lambda: nc.vector.tensor_copy(out=dst_ap, in_=src_ap), reads=reads, writes=writes)

            lnq = {}

            def ln_start(ti_):
                g0_, n_, s_ = tiles[ti_]
                xm_, txm_ = R["xm"].next()
                lnq[ti_] = (xm_, txm_, self.ln_mod_gen(L, 0, g0_, n_, mods[s_], R, xm_, txm_))

            def ln_step(ti_):
                if ti_ in lnq:
                    try:
                        next(lnq[ti_][2])
                    except StopIteration:
                        pass
            ln_start(0)
            for ti, (g0, n, s) in enumerate(tiles):
                lat = (s == 0)
                t0 = g0 - CTX
                xm, txm, gcur = lnq.pop(ti)
                for _ in gcur:
                    pass
                if ti + 1 < len(tiles):
                    ln_start(ti + 1)

                def proj_fm(off, m):
                    pt, tp = pp.next()
                    for k in range(8):
                        kb.op("pe", lambda: nc.tensor.matmul(pt[0:m, 0:n], lhsT=wext[:, k, off:off + m], rhs=xm[:, k, 0:n], start=(k == 0), stop=(k == 7)),
                              reads=[twk[k], txm], writes=[tp], inc=(k == 7))
                    return pt, tp

                def rope_comb(dst, tdst, p1, tp1, p2, tp2, tab, ttab, lo, hi):
                    f1, tf1 = sgf.next()
                    f2, tf2 = sgf.next()
                    kb.op("dve", lambda: nc.vector.tensor_tensor(out=f1[lo:hi, 0:n], in0=p1[lo:hi, 0:n], in1=tab[lo:hi, 0, 0:n], op=ALU.mult), reads=[tp1, ttab], writes=[tf1])
                    kb.op("dve", lambda: nc.vector.tensor_tensor(out=f2[lo:hi, 0:n], in0=p2[lo:hi, 0:n], in1=tab[lo:hi, 1, 0:n], op=ALU.mult), reads=[tp2, ttab], writes=[tf2])
                    kb.op("pool", lambda: nc.gpsimd.tensor_tensor(out=dst[lo:hi, 0:n], in0=f1[lo:hi, 0:n], in1=f2[lo:hi, 0:n], op=ALU.add), reads=[tf1, tf2], writes=[tdst])

                if lat:
                    tabA, ttA = rcA.next()
                    kb.dma("act", tabA[:, 0, 0:n], self.inp["cosA"][:, t0:t0 + n], writes=[ttA])
                    kb.dma("act", tabA[:, 1, 0:n], self.inp["sinA"][:, t0:t0 + n], writes=[ttA])
                    tabC, ttC = rcC.next()
                    kb.dma("act", tabC[:, 0, 0:n], self.inp["cosC"][:, t0:t0 + n], writes=[ttC])
                    kb.dma("act", tabC[:, 1, 0:n], self.inp["sinC"][:, t0:t0 + n], writes=[ttC])
                for (off, offp, dst) in ((OQA, OQAP, "QAT"), (OKA, OKAP, "KAT")):
                    for c in range(4):
                        p1, tp1 = proj_fm(off + c * 128, 128)
                        sg, tsg = sgb.next()
                        if lat:
                            p2, tp2 = proj_fm(offp + c * 128, 128)
                            rope_comb(sg, tsg, p1, tp1, p2, tp2, tabA, ttA, 0, 128)
                        else:
                            evac_copy(sg[:, 0:n], p1[:, 0:n], [tp1], [tsg])
                        kb.dma("pool", scr[dst][c * 128:(c + 1) * 128, g0:g0 + n], sg[:, 0:n], reads=[tsg])
                ln_step(ti + 1)
                for j in range(n // 128):
                    pt, tp = pp.next()
                    for k in range(8):
                        kb.op("pe", lambda: nc.tensor.matmul(pt[:, :], lhsT=xm[:, k, j * 128:(j + 1) * 128], rhs=wext[:, k, OVA:OVA + 512], start=(k == 0), stop=(k == 7)),
                              reads=[twk[k], txm], writes=[tp], inc=(k == 7))
                    sg, tsg = sgb.next()
                    evac_copy(sg[:, :], pt[:, :], [tp], [tsg])
                    kb.dma("pool", scr["VA"][g0 + j * 128:g0 + (j + 1) * 128, :], sg[:, :], reads=[tsg])
                ln_step(ti + 1)
                for c in range(9):
                    p1, tp1 = proj_fm(OB + c * 128, 128)
                    sg, tsg = sgf.next()
                    evac_copy(sg[:, 0:n], p1[:, 0:n], [tp1], [tsg])
                    kb.dma("pool", scr["PBT"][c * 128:(c + 1) * 128, g0:g0 + n], sg[:, 0:n], reads=[tsg])
                ln_step(ti + 1)
                cqn = []
                cqr = []
                ssp = pp.next()
                for c in range(2):
                    p1, tp1 = proj_fm(OCQ + c * 128, 128)
                    f, tf = sgf.next()
                    kb.op("act", lambda: nc.scalar.copy(out=f[:, 0:n], in_=p1[:, 0:n]), reads=[tp1], writes=[tf])
                    sq, tsq = sgb.next()
                    kb.op("act", lambda: nc.scalar.activation(out=sq[:, 0:n], in_=p1[:, 0:n], func=AF.Square), reads=[tp1], writes=[tsq])
                    kb.op("pe", lambda: nc.tensor.matmul(ssp[0][:, 0:n], lhsT=onesb[:, :], rhs=sq[:, 0:n], start=(c == 0), stop=(c == 1)),
                          reads=[tc, tsq], writes=[ssp[1]], inc=(c == 1))
                    cqr.append((f, tf))
                rsb, trsb = sgf.next()
                self.rms_bc(ssp, n, 256.0, rsb, trsb)
                for c in range(2):
                    f, tf = cqr[c]
                    o, to = ded[c]
                    kb.op("dve", lambda: nc.vector.scalar_tensor_tensor(out=o[:, 0:n], in0=f[:, 0:n], scalar=qng[:, c:c + 1], in1=rsb[:, 0:n], op0=ALU.mult, op1=ALU.mult),
                          reads=[tf, trsb, tc], writes=[to])
                    cqn.append((o, to))
                if lat or with_ctx_q:
                    for h in range(4):
                        pq, tpq = pp.next()
                        for rc in range(2):
                            kb.op("pe", lambda: nc.tensor.matmul(pq[0:96, 0:n], lhsT=wuq[:, rc, h * 96:(h + 1) * 96], rhs=cqn[rc][0][:, 0:n], start=(rc == 0), stop=(rc == 1)),
                                  reads=[tc, cqn[rc][1]], writes=[tpq], inc=(rc == 1))
                        sg, tsg = sgb.next()
                        if lat:
                            pq2, tpq2 = pp.next()
                            for rc in range(2):
                                kb.op("pe", lambda: nc.tensor.matmul(pq2[0:96, 0:n], lhsT=wuqp[:, rc, h * 96:(h + 1) * 96], rhs=cqn[rc][0][:, 0:n], start=(rc == 0), stop=(rc == 1)),
                                      reads=[tc, cqn[rc][1]], writes=[tpq2], inc=(rc == 1))
                            evac_copy(sg[0:64, 0:n], pq[0:64, 0:n], [tpq], [tsg])
                            rope_comb(sg, tsg, pq, tpq, pq2, tpq2, tabC, ttC, 64, 96)
                        else:
                            evac_copy(sg[0:96, 0:n], pq[0:96, 0:n], [tpq], [tsg])
                        kb.dma("pool", scr["QCT"][h, :, g0:g0 + n], sg[0:96, 0:n], reads=[tsg])
                ln_step(ti + 1)
                p1, tp1 = proj_fm(OCKV, 128)
                f, tf = sgf.next()
                kb.op("act", lambda: nc.scalar.copy(out=f[:, 0:n], in_=p1[:, 0:n]), reads=[tp1], writes=[tf])
                sq, tsq = sgb.next()
                kb.op("act", lambda: nc.scalar.activation(out=sq[:, 0:n], in_=p1[:, 0:n], func=AF.Square), reads=[tp1], writes=[tsq])
                ssp = pp.next()
                kb.op("pe", lambda: nc.tensor.matmul(ssp[0][:, 0:n], lhsT=onesb[:, :], rhs=sq[:, 0:n], start=True, stop=True), reads=[tc, tsq], writes=[ssp[1]])
                rsb, trsb = sgf.next()
                self.rms_bc(ssp, n, 128.0, rsb, trsb)
                ckvn, tckvn = ded[2]
                kb.op("dve", lambda: nc.vector.scalar_tensor_tensor(out=ckvn[:, 0:n], in0=f[:, 0:n], scalar=kvng[:, 0:1], in1=rsb[:, 0:n], op0=ALU.mult, op1=ALU.mult),
                      reads=[tf, trsb, tc], writes=[tckvn])
                pk, tpk = proj_fm(OKPE, 96)
                kpe, tkpe = ded[3]
                if lat:
                    pk2, tpk2 = proj_fm(OKPEP, 96)
                    rope_comb(kpe, tkpe, pk, tpk, pk2, tpk2, tabC, ttC, 64, 96)
                else:
                    evac_copy(kpe[64:96, 0:n], pk[64:96, 0:n], [tpk], [tkpe])
                for h in range(4):
                    pkn, tpkn = pp.next()
                    kb.op("pe", lambda: nc.tensor.matmul(pkn[0:64, 0:n], lhsT=wukv[:, h * 128:h * 128 + 64], rhs=ckvn[:, 0:n], start=True, stop=True),
                          reads=[tc, tckvn], writes=[tpkn])
                    sg, tsg = sgb.next()
                    evac_copy(sg[0:64, 0:n], pkn[0:64, 0:n], [tpkn], [tsg])
                    kb.dma("pool", scr["KCT"][h, 0:64, g0:g0 + n], sg[0:64, 0:n], reads=[tsg])
                    kb.dma("pool", scr["KCT"][h, 64:96, g0:g0 + n], kpe[64:96, 0:n], reads=[tkpe])
                for j in range(n // 128):
                    pv, tpv = pp.next()
                    kb.op("pe", lambda: nc.tensor.matmul(pv[:, 0:256], lhsT=ckvn[:, j * 128:(j + 1) * 128], rhs=wukvv[:, :], start=True, stop=True),
                          reads=[tc, tckvn], writes=[tpv])
                    sg, tsg = sgb.next()
                    evac_copy(sg[:, 0:256], pv[:, 0:256], [tpv], [tsg])
                    kb.dma("pool", scr["VC"][g0 + j * 128:g0 + (j + 1) * 128, :], sg[:, 0:256], reads=[tsg])


    def p_attn_a(self, L, with_ctx):
        nc, kb = self.nc, self.kb
        scr = self.scr
        sc = 64 ** -0.5
        li = lam_init(L)
        with self.phase():
            tc = T()
            KT = self.sb([128, 4, NTOK], BF16)
            V = self.sb([128, NKC, 512], BF16)
            tkh = [T() for _ in range(4)]; tvk = [T() for _ in range((NKC + 5) // 6)]
            for c in range(4):
                kb.dma("sp", KT[:, c, :], scr["KAT"][c * 128:(c + 1) * 128, :], writes=[tkh[c]])
            vv = scr["VA"].rearrange("(k p) n -> p k n", p=128)
            for k0 in range(0, NKC, 6):
                k1 = min(NKC, k0 + 6)
                kb.dma("act", V[:, k0:k1, :], vv[:, k0:k1, :], writes=[tvk[k0 // 6]])
            onesb = self.sb([128, 128], BF16); onesf = self.sb([128, 128])
            kb.op("pool", lambda: nc.gpsimd.memset(onesb[:], 1.0), writes=[tc])
            kb.op("pool", lambda: nc.gpsimd.memset(onesf[:], 1.0), writes=[tc])
            dng = self.sb([128, 1])
            kb.dma("act", dng[:], self.inp["dngc"][L], writes=[tc])
            kb.op("dve", lambda: nc.vector.tensor_scalar(out=dng[:], in0=dng[:], scalar1=(1.0 - li), scalar2=None, op0=ALU.mult), reads=[tc], writes=[tc])
            lv = self.sb([128, 4, 64]); lt = self.sb([128, 2, 64]); ls = self.sb([128, 4]); tl = T()
            kb.dma("act", lv[:], self.inp["lamv"][L], writes=[tl])
            kb.op("dve", lambda: nc.vector.tensor_tensor(out=lt[:, 0, :], in0=lv[:, 0, :], in1=lv[:, 1, :], op=ALU.mult), reads=[tl], writes=[tl])
            kb.op("dve", lambda: nc.vector.tensor_tensor(out=lt[:, 1, :], in0=lv[:, 2, :], in1=lv[:, 3, :], op=ALU.mult), reads=[tl], writes=[tl])
            kb.op("dve", lambda: nc.vector.tensor_reduce(out=ls[:, 0:2], in_=lt[:, :, :], axis=AX.X, op=ALU.add), reads=[tl], writes=[tl])
            kb.op("act", lambda: nc.scalar.activation(out=ls[:, 0:2], in_=ls[:, 0:2], func=AF.Exp), reads=[tl], writes=[tl])
            kb.op("dve", lambda: nc.vector.tensor_tensor(out=ls[:, 2:3], in0=ls[:, 1:2], in1=ls[:, 0:1], op=ALU.subtract), reads=[tl], writes=[tl])
            kb.op("dve", lambda: nc.vector.tensor_scalar_add(out=ls[:, 2:3], in0=ls[:, 2:3], scalar1=-li), reads=[tl], writes=[tl])
            qz = [[(self.sb([128, 512], BF16), T()) for _m in range(2)] for _ in range(3)]
            for sl_ in qz:
                for (qt_, tq_) in sl_:
                    kb.op("pool", lambda: nc.gpsimd.memset(qt_[:], 0.0), writes=[tq_])
            spr = self.psring(3)
            pr = self.sbring(5, [128, 512], BF16)
            O = [(self.ps([128, 512]), T()) for _ in range(2)]
            S = [(self.ps([128, 512]), T()) for _ in range(2)]
            SS = (self.ps([128, 512]), T())
            pacc = [[(self.sb([128, 512]), T()) for _ in range(2)] for _ in range(2)]
            rr = self.sbring(2, [128, 2, 512]); orr = self.sbring(2, [128, 512]); tr2 = self.sbring(2, [128, 512])
            sqr = self.sbring(2, [128, 512], BF16); rsr = self.sbring(2, [128, 512]); obr = self.sbring(3, [128, 512], BF16)
            tiles = [(CTX + i * 512, 512, 0, NKC) for i in range(SEQ // 512)]
            if with_ctx:
                tiles = [(0, CTX, 0, CTX // 128)] + tiles
            hi = 0
            for (g0, n, kc0, kc1) in tiles:
                for h in range(4):
                    par = hi % 2
                    hi += 1
                    qsl = qz[(hi - 1) % 3]
                    for m_ in range(2):
                        kb.dma("sp", qsl[m_][0][64 * m_:64 * m_ + 64, 0:n], scr["QAT"][h * 128 + 64 * m_:h * 128 + 64 * m_ + 64, g0:g0 + n], writes=[qsl[m_][1]])
                    units = [(kc, m) for kc in range(kc0, kc1) for m in range(2)]

                    def emit_score(kc, m):
                        st_, tst = spr.next()
                        kb.op("pe", lambda: nc.tensor.matmul(st_[:, 0:n], lhsT=KT[:, h, kc * 128:(kc + 1) * 128], rhs=qsl[m][0][:, 0:n], start=True, stop=True),
                              reads=[tkh[h], qsl[m][1]], writes=[tst])
                        pt, tp = pr.next()
                        kb.op("act", lambda: nc.scalar.activation(out=pt[:, 0:n], in_=st_[:, 0:n], func=AF.Exp, scale=sc), reads=[tst], writes=[tp])
                        return pt, tp

                    def emit_pv(kc, m, pt, tp):
                        kb.op("pe", lambda: nc.tensor.matmul(O[m][0][:, 0:n], lhsT=V[:, kc, h * 128:(h + 1) * 128], rhs=pt[:, 0:n], start=(kc == kc0), stop=(kc == kc1 - 1)),
                              reads=[tp, tvk[kc // 6]], writes=[O[m][1]])
                        if m == 0:
                            kb.op("pe", lambda: nc.tensor.matmul(S[0][0][:, 0:n], lhsT=onesb[:, :], rhs=pt[:, 0:n], start=(kc == kc0), stop=(kc == kc1 - 1)),
                                  reads=[tp, tc], writes=[S[0][1]])
                        else:
                            pa, tpa = pacc[par][1]
                            if kc == kc0:
                                kb.op("dve", lambda: nc.vector.tensor_copy(out=pa[:, 0:n], in_=pt[:, 0:n]), reads=[tp], writes=[tpa])
                            else:
                                kb.op("dve", lambda: nc.vector.tensor_tensor(out=pa[:, 0:n], in0=pa[:, 0:n], in1=pt[:, 0:n], op=ALU.add), reads=[tp, tpa], writes=[tpa])
                    pend = []
                    for (kc, m) in units:
                        pend.append((kc, m) + emit_score(kc, m))
                        if len(pend) > 2:
                            emit_pv(*pend.pop(0))
                    while pend:
                        emit_pv(*pend.pop(0))
                    for m in range(1, 2):
                        pa, tpa = pacc[par][m]
                        kb.op("pe", lambda: nc.tensor.matmul(S[m][0][:, 0:n], lhsT=onesf[:, :], rhs=pa[:, 0:n], start=True, stop=True), reads=[tpa, tc], writes=[S[m][1]])
                    r_, tr_ = rr.next()
                    kb.op("dve", lambda: nc.vector.reciprocal(out=r_[:, 0, 0:n], in_=S[0][0][:, 0:n]), reads=[S[0][1]], writes=[tr_])
                    kb.op("dve", lambda: nc.vector.reciprocal(out=r_[:, 1, 0:n], in_=S[1][0][:, 0:n]), reads=[S[1][1]], writes=[tr_])
                    o, to = orr.next(); t2, tt2 = tr2.next()
                    kb.op("dve", lambda: nc.vector.tensor_tensor(out=o[:, 0:n], in0=O[0][0][:, 0:n], in1=r_[:, 0, 0:n], op=ALU.mult), reads=[O[0][1], tr_], writes=[to])
                    kb.op("dve", lambda: nc.vector.tensor_tensor(out=t2[:, 0:n], in0=O[1][0][:, 0:n], in1=r_[:, 1, 0:n], op=ALU.mult), reads=[O[1][1], tr_], writes=[tt2])
                    kb.op("dve", lambda: nc.vector.scalar_tensor_tensor(out=o[:, 0:n], in0=t2[:, 0:n], scalar=ls[:, 2:3], in1=o[:, 0:n], op0=ALU.mult, op1=ALU.add), reads=[tt2, to, tl], writes=[to])
                    sq, tsq = sqr.next()
                    kb.op("act", lambda: nc.scalar.activation(out=sq[:, 0:n], in_=o[:, 0:n], func=AF.Square), reads=[to], writes=[tsq])
                    kb.op("pe", lambda: nc.tensor.matmul(SS[0][:, 0:n], lhsT=onesb[:, :], rhs=sq[:, 0:n], start=True, stop=True), reads=[tsq, tc], writes=[SS[1]])
                    rs, trs = rsr.next()
                    kb.op("act", lambda: nc.scalar.activation(out=rs[:, 0:n], in_=SS[0][:, 0:n], func=AF.Ln, scale=1.0 / 128, bias=RMS_EPS), reads=[SS[1]], writes=[trs])
                    kb.op("act", lambda: nc.scalar.activation(out=rs[:, 0:n], in_=rs[:, 0:n], func=AF.Exp, scale=-0.5), reads=[trs], writes=[trs])
                    ob, tob = obr.next()
                    kb.op("dve", lambda: nc.vector.scalar_tensor_tensor(out=ob[:, 0:n], in0=o[:, 0:n], scalar=dng[:, 0:1], in1=rs[:, 0:n], op0=ALU.mult, op1=ALU.mult), reads=[to, trs, tc], writes=[tob])
                    kb.dma("pool", scr["CATT"][h * 128:(h + 1) * 128, g0:g0 + n], ob[:, 0:n], reads=[tob])

    def p_attn_c(self, L, with_ctx):
        nc, kb = self.nc, self.kb
        scr = self.scr
        sc = 96 ** -0.5
        with self.phase():
            tc = T()
            KT = self.sb([96, 4, NTOK], BF16)
            V = self.sb([128, NKC, 4, 65], BF16)
            tkh = [T() for _ in range(4)]
            for h in range(4):
                kb.dma("sp", KT[:, h, :], scr["KCT"][h, :, :], writes=[tkh[h]])
            kb.op("pool", lambda: nc.gpsimd.memset(V[:], 1.0), writes=[tc])
            vv = scr["VC"].rearrange("(k p) (h d) -> p k h d", p=128, h=4)
            for k0 in range(0, NKC, 6):
                k1 = min(NKC, k0 + 6)
                for h in range(4):
                    kb.dma("act", V[:, k0:k1, h, 0:64], vv[:, k0:k1, h, :], writes=[tc])
            identb = self.sb([128, 128], BF16)
            kb.dma("act", identb[:], self.inp["ident_bf"][:, :], writes=[tc])
            qr = self.sbring(3, [96, 512], BF16)
            spr = self.psring(3)
            pr = self.sbring(4, [128, 512], BF16)
            O = (self.ps([128, 4, 65]), T())
            tpr = self.psring(2, [128, 128], BF16)
            stage = self.sbring(2, [128, 4, 256], BF16)
            eo = self.sbring(3, [128, 128], BF16)
            sm = self.sbring(2, [128, 4], F32)
            tiles = [(CTX + i * 512, 512, 0, NKC) for i in range(SEQ // 512)]
            if with_ctx:
                tiles = [(0, CTX, 0, CTX // 128)] + tiles
            for (g0, n, kc0, kc1) in tiles:
                nj = n // 128
                sg, tsg = stage.next()
                for h in range(4):
                    qt, tq = qr.next()
                    kb.dma("sp", qt[:, 0:n], scr["QCT"][h, :, g0:g0 + n], writes=[tq])
                    first = [True]

                    def emit_score(kc):
                        st_, tst = spr.next()
                        kb.op("pe", lambda: nc.tensor.matmul(st_[:, 0:n], lhsT=KT[:, h, kc * 128:(kc + 1) * 128], rhs=qt[:, 0:n], start=True, stop=True),
                              reads=[tkh[h], tq], writes=[tst])
                        pt, tp = pr.next()
                        kb.op("act", lambda: nc.scalar.activation(out=pt[:, 0:n], in_=st_[:, 0:n], func=AF.Exp, scale=sc), reads=[tst], writes=[tp])
                        return pt, tp

                    def emit_pv(kc, pt, tp):
                        last = (kc == kc1 - 1)
                        for j in range(nj):
                            kb.op("pe", lambda: nc.tensor.matmul(O[0][:, j, :], lhsT=pt[:, j * 128:(j + 1) * 128], rhs=V[:, kc, h, :],
                                                                 start=first[0], stop=last, skip_group_check=True), reads=[tp, tc], writes=[O[1]], inc=(j == nj - 1))
                            first[0] = False
                    pend = []
                    for kc in range(kc0, kc1):
                        pend.append((kc,) + emit_score(kc))
                        if len(pend) > 2:
                            emit_pv(*pend.pop(0))
                    while pend:
                        emit_pv(*pend.pop(0))
                    s_, ts = sm.next()
                    kb.op("dve", lambda: nc.vector.reciprocal(out=s_[:, 0:nj], in_=O[0][:, 0:nj, 64]), reads=[O[1]], writes=[ts])
                    for j in range(nj):
                        kb.op("dve", lambda: nc.vector.tensor_scalar(out=sg[:, j, h * 64:(h + 1) * 64], in0=O[0][:, j, 0:64], scalar1=s_[:, j:j + 1], scalar2=None, op0=ALU.mult),
                              reads=[O[1], ts], writes=[tsg])
                for j in range(nj):
                    for c in range(2):
                        tp_, ttp = tpr.next()
                        kb.op("pe", lambda: nc.tensor.transpose(out=tp_[:, :], in_=sg[:, j, c * 128:(c + 1) * 128], identity=identb[:]), reads=[tsg, tc], writes=[ttp])
                        oo, too = eo.next()
                        kb.op("act", lambda: nc.scalar.copy(out=oo[:], in_=tp_[:, :]), reads=[ttp], writes=[too])
                        kb.dma("pool", scr["CATT"][768 + c * 128:768 + (c + 1) * 128, g0 + j * 128:g0 + (j + 1) * 128], oo[:], reads=[too])


    def resid_ln(self, xt, tx, o_parts, gate, tgate, lng, lnb, tln, RR, dst_ap):
        nc, kb = self.nc, self.kb
        y, ty = RR["y"].next()
        for hh in range(2):
            sl = slice(hh * 512, (hh + 1) * 512)
            kb.op("dve", lambda: nc.vector.tensor_tensor(out=y[:, sl], in0=o_parts[hh][0], in1=gate[:, sl], op=ALU.mult), reads=[o_parts[hh][1], tgate], writes=[ty])
        kb.op("dve", lambda: nc.vector.scalar_tensor_tensor(out=y[:], in0=xt[:], scalar=ALPHA, in1=y[:], op0=ALU.mult, op1=ALU.add), reads=[tx, ty], writes=[ty])
        st, tst = RR["st"].next()
        kb.op("dve", lambda: nc.vector.bn_stats(out=st[:, 0:6], in_=y[:, 0:512]), reads=[ty], writes=[tst])
        kb.op("dve", lambda: nc.vector.bn_stats(out=st[:, 6:12], in_=y[:, 512:1024]), reads=[ty], writes=[tst])
        kb.op("dve", lambda: nc.vector.bn_aggr(out=st[:, 12:14], in_=st[:, 0:12]), reads=[tst], writes=[tst])
        kb.op("act", lambda: nc.scalar.activation(out=st[:, 14:15], in_=st[:, 13:14], func=AF.Ln, bias=LN_EPS), reads=[tst], writes=[tst])
        kb.op("act", lambda: nc.scalar.activation(out=st[:, 15:16], in_=st[:, 14:15], func=AF.Exp, scale=-0.5), reads=[tst], writes=[tst])
        z, tz = RR["z"].next()
        kb.op("dve", lambda: nc.vector.tensor_scalar(out=z[:], in0=y[:], scalar1=st[:, 12:13], scalar2=st[:, 15:16], op0=ALU.subtract, op1=ALU.mult), reads=[ty, tst], writes=[tz])
        kb.op("pool", lambda: nc.gpsimd.tensor_tensor(out=z[:], in0=z[:], in1=lng[:], op=ALU.mult), reads=[tz, tln], writes=[tz])
        kb.op("pool", lambda: nc.gpsimd.tensor_tensor(out=z[:], in0=z[:], in1=lnb[:], op=ALU.add), reads=[tz, tln], writes=[tz])
        kb.dma("pool", dst_ap, z[:], reads=[tz])

    def resid_rings(self, L, which):
        kb = self.kb
        RR = dict(y=self.sbring(2, [128, D]), z=self.sbring(2, [128, D]), st=self.sbring(3, [128, 16]), xt=self.sbring(2, [128, D]))
        lng = self.sb([128, D]); lnb = self.sb([128, D]); tln = T()
        kb.dma("act", lng[:], self.inp["lnp"][L, 2 * which], writes=[tln])
        kb.dma("act", lnb[:], self.inp["lnp"][L, 2 * which + 1], writes=[tln])
        RR.update(lng=lng, lnb=lnb, tln=tln)
        return RR

    def p_out_ln1(self, L, with_ctx):
        nc, kb = self.nc, self.kb
        scr = self.scr
        with self.phase():
            tc = T()
            wo = self.sb([128, 8, D], BF16)
            wv = self.inp["w_out"][L].rearrange("(k p) n -> p k n", p=128)
            two = [T() for _ in range(8)]
            for k in range(8):
                kb.dma("pool", wo[:, k, :], wv[:, k, :], writes=[two[k]])
            mods = self.load_mod(L, 0)
            RR = self.resid_rings(L, 0)
            ctr = self.sbring(3, [128, 8, 128], BF16)
            pp = self.psring(4)
            cv = scr["CATT"].rearrange("(k p) t -> p k t", p=128)
            for g0 in range(0 if with_ctx else CTX, NTOK, 128):
                s = 1 if g0 < CTX else 0
                ct, tct = ctr.next()
                kb.dma("sp", ct[:], cv[:, :, g0:g0 + 128], writes=[tct])
                xt, tx = RR["xt"].next()
                kb.dma("sp", xt[:], self.src_rows(L, 0, g0, 128), writes=[tx])
                parts = []
                for hh in range(2):
                    pt, tp = pp.next()
                    for k in range(8):
                        kb.op("pe", lambda: nc.tensor.matmul(pt[:, :], lhsT=ct[:, k, :], rhs=wo[:, k, hh * 512:(hh + 1) * 512], start=(k == 0), stop=(k == 7)),
                              reads=[tct, two[k]], writes=[tp], inc=(k == 7))
                    parts.append((pt[:, :], tp))
                self.resid_ln(xt, tx, parts, mods[s]["gb"], mods[s]["tgb"], RR["lng"], RR["lnb"], RR["tln"], RR, scr["X1"][g0:g0 + 128, :])

    def p_ffn_prep(self, L, with_ctx, moe):
        nc, kb = self.nc, self.kb
        scr = self.scr
        with self.phase():
            R = self.ln_rings()
            mods = self.load_mod(L, 1)
            tiles = ([(0, CTX, 1)] if with_ctx else []) + [(CTX + i * 512, 512, 0) for i in range(SEQ // 512)]
            if moe:
                identf = self.sb([128, 128]); tc = T()
                kb.dma("act", identf[:], self.inp["ident_f"][:, :], writes=[tc])
                rw = self.sb([128, 8, NEXP])
                kb.dma("act", rw[:], self.inp["router"].rearrange("(k p) e -> p k e", p=128), writes=[tc])
                xfr = self.sbring(2, [128, D]); hfr = self.sbring(2, [128, 8, 128])
                ptr = self.psring(1, [128, D]); plr = self.psring(2, [128, NEXP])
                gr = self.sbring(2, [128, 40]); gtr = self.sbring(2, [NEXP, 128])
                ptg = self.psring(1, [NEXP, 128])
                fmb = self.sb([128, 2, D]); tfmb = T()
                md = scr["mod_d"]
                kb.dma("act", fmb[:, 0, :], md[L, 0, 3 * D:4 * D].partition_broadcast(128), writes=[tfmb])
                kb.dma("act", fmb[:, 1, :], md[L, 0, 4 * D:5 * D].partition_broadcast(128), writes=[tfmb])
                kb.op("pool", lambda: nc.gpsimd.tensor_scalar(out=fmb[:, 1, :], in0=fmb[:, 1, :], scalar1=1.0, scalar2=None, op0=ALU.add), reads=[tfmb], writes=[tfmb])
            for (g0, n, s) in tiles:
                xm, txm = self.ln_mod_tile(L, 1, g0, n, mods[s], R)
                for k in range(8):
                    kb.dma("pool", scr["HT"][k * 128:(k + 1) * 128, g0:g0 + n], xm[:, k, 0:n], reads=[txm])
                if not moe:
                    continue
                for j in range(n // 128):
                    gg = g0 + j * 128
                    xt, tx = xfr.next()
                    kb.dma("sp", xt[:], scr["X1"][gg:gg + 128, :], writes=[tx])
                    st, tst = R["st"].next()
                    kb.op("dve", lambda: nc.vector.bn_stats(out=st[:, 0:6], in_=xt[:, 0:512]), reads=[tx], writes=[tst])
                    kb.op("dve", lambda: nc.vector.bn_stats(out=st[:, 6:12], in_=xt[:, 512:1024]), reads=[tx], writes=[tst])
                    kb.op("dve", lambda: nc.vector.bn_aggr(out=st[:, 12:14], in_=st[:, 0:12]), reads=[tst], writes=[tst])
                    kb.op("act", lambda: nc.scalar.activation(out=st[:, 14:15], in_=st[:, 13:14], func=AF.Ln, bias=LN_EPS), reads=[tst], writes=[tst])
                    kb.op("act", lambda: nc.scalar.activation(out=st[:, 15:16], in_=st[:, 14:15], func=AF.Exp, scale=-0.5), reads=[tst], writes=[tst])
                    kb.op("dve", lambda: nc.vector.tensor_scalar(out=xt[:], in0=xt[:], scalar1=st[:, 12:13], scalar2=st[:, 15:16], op0=ALU.subtract, op1=ALU.mult), reads=[tx, tst], writes=[tx])
                    kb.op("pool", lambda: nc.gpsimd.tensor_tensor(out=xt[:], in0=xt[:], in1=fmb[:, 1, :], op=ALU.mult), reads=[tx, tfmb], writes=[tx])
                    kb.op("pool", lambda: nc.gpsimd.tensor_tensor(out=xt[:], in0=xt[:], in1=fmb[:, 0, :], op=ALU.add), reads=[tx, tfmb], writes=[tx])
                    pt, tp = ptr.next()
                    for k in range(8):
                        kb.op("pe", lambda: nc.tensor.matmul(pt[:, k * 128:(k + 1) * 128], lhsT=xt[:, k * 128:(k + 1) * 128], rhs=identf[:], start=True, stop=True), reads=[tx, tc], writes=[tp], inc=(k == 7))
                    hf, thf = hfr.next()
                    kb.op("act", lambda: nc.scalar.copy(out=hf[:, 0:4, :], in_=pt[:, 0:512]), reads=[tp], writes=[thf])
                    kb.op("dve", lambda: nc.vector.tensor_copy(out=hf[:, 4:8, :], in_=pt[:, 512:1024]), reads=[tp], writes=[thf])
                    pl, tpl = plr.next()
                    for k in range(8):
                        kb.op("pe", lambda: nc.tensor.matmul(pl[:, :], lhsT=hf[:, k, :], rhs=rw[:, k, :], start=(k == 0), stop=(k == 7)), reads=[thf, tc], writes=[tpl], inc=(k == 7))
                    g, tg = gr.next()
                    kb.op("dve", lambda: nc.vector.tensor_copy(out=g[:, 0:8], in_=pl[:, :]), reads=[tpl], writes=[tg])
                    kb.op("dve", lambda: nc.vector.tensor_reduce(out=g[:, 8:9], in_=g[:, 0:8], axis=AX.X, op=ALU.max), reads=[tg], writes=[tg])
                    kb.op("dve", lambda: nc.vector.tensor_scalar(out=g[:, 10:18], in0=g[:, 0:8], scalar1=g[:, 8:9], scalar2=None, op0=ALU.is_equal), reads=[tg], writes=[tg])
                    kb.op("dve", lambda: nc.vector.scalar_tensor_tensor(out=g[:, 18:26], in0=g[:, 10:18], scalar=-1e30, in1=g[:, 0:8], op0=ALU.mult, op1=ALU.add), reads=[tg], writes=[tg])
                    kb.op("dve", lambda: nc.vector.tensor_reduce(out=g[:, 9:10], in_=g[:, 18:26], axis=AX.X, op=ALU.max), reads=[tg], writes=[tg])
                    kb.op("dve", lambda: nc.vector.tensor_scalar(out=g[:, 18:26], in0=g[:, 18:26], scalar1=g[:, 9:10], scalar2=None, op0=ALU.is_equal), reads=[tg], writes=[tg])
                    kb.op("dve", lambda: nc.vector.tensor_tensor(out=g[:, 26:27], in0=g[:, 9:10], in1=g[:, 8:9], op=ALU.subtract), reads=[tg], writes=[tg])
                    kb.op("act", lambda: nc.scalar.activation(out=g[:, 26:27], in_=g[:, 26:27], func=AF.Exp), reads=[tg], writes=[tg])
                    kb.op("dve", lambda: nc.vector.tensor_scalar_add(out=g[:, 26:27], in0=g[:, 26:27], scalar1=1.0), reads=[tg], writes=[tg])
                    kb.op("dve", lambda: nc.vector.reciprocal(out=g[:, 26:27], in_=g[:, 26:27]), reads=[tg], writes=[tg])
                    kb.op("dve", lambda: nc.vector.tensor_scalar(out=g[:, 27:28], in0=g[:, 26:27], scalar1=-1.0, scalar2=1.0, op0=ALU.mult, op1=ALU.add), reads=[tg], writes=[tg])
                    kb.op("dve", lambda: nc.vector.tensor_scalar(out=g[:, 28:36], in0=g[:, 10:18], scalar1=g[:, 26:27], scalar2=None, op0=ALU.mult), reads=[tg], writes=[tg])
                    kb.op("dve", lambda: nc.vector.scalar_tensor_tensor(out=g[:, 28:36], in0=g[:, 18:26], scalar=g[:, 27:28], in1=g[:, 28:36], op0=ALU.mult, op1=ALU.add), reads=[tg], writes=[tg])
                    pg, tpg = ptg.next()
                    kb.op("pe", lambda: nc.tensor.matmul(pg[:, :], lhsT=g[:, 28:36], rhs=identf[:], start=True, stop=True), reads=[tg, tc], writes=[tpg])
                    gt, tgt = gtr.next()
                    kb.op("act", lambda: nc.scalar.copy(out=gt[:, :], in_=pg[:, :]), reads=[tpg], writes=[tgt])
                    kb.dma("sp", scr["GT"][:, gg:gg + 128], gt[:, :], reads=[tgt])

    def p_ffn_up(self, w1_ap, w3_ap, e, tok0, moe):
        nc, kb = self.nc, self.kb
        scr = self.scr
        ntok = NTOK - tok0
        with self.phase():
            tc = T()
            hT = self.sb([128, 8, ntok], BF16)
            thT = [T() for _ in range(8)]
            for k in range(8):
                kb.dma("sp" if k % 2 else "act", hT[:, k, :], scr["HT"][k * 128:(k + 1) * 128, tok0:NTOK], writes=[thT[k]])
            if moe:
                sel = self.sb([NEXP, 128]); gT = self.sb([NEXP, ntok]); gbc = self.sb([128, ntok]); tg = T()
                kb.dma("act", gT[:], scr["GT"][:, tok0:NTOK], writes=[tg])
                kb.dma("act", sel[:], self.inp["sel8"][e], writes=[tg])
            w1r = self.sbring(2, [128, 8, 256], BF16); w3r = self.sbring(2, [128, 8, 256], BF16)
            pp = self.psring(6)
            sr = self.sbring(3, [128, 512], F32); tr_ = self.sbring(3, [128, 512], F32); ur = self.sbring(3, [128, 512], BF16)
            tiles = ([(0, CTX)] if tok0 == 0 else []) + [(CTX + i * 512, 512) for i in range(SEQ // 512)]
            if moe:
                for (g0, n) in tiles:
                    pt, tp = pp.next()
                    kb.op("pe", lambda: nc.tensor.matmul(pt[:, 0:n], lhsT=sel[:, :], rhs=gT[:, g0 - tok0:g0 - tok0 + n], start=True, stop=True), reads=[tg], writes=[tp])
                    kb.op("act", lambda: nc.scalar.copy(out=gbc[:, g0 - tok0:g0 - tok0 + n], in_=pt[:, 0:n]), reads=[tp], writes=[tg])
            w1v = w1_ap.rearrange("(k p) n -> p k n", p=128)
            w3v = w3_ap.rearrange("(k p) n -> p k n", p=128)
            wl = {}

            def wload(fp_):
                w1_, tw1_ = w1r.next(); w3_, tw3_ = w3r.next()
                kb.dma("pool", w1_[:], w1v[:, :, fp_ * 256:(fp_ + 1) * 256], writes=[tw1_])
                kb.dma("pool", w3_[:], w3v[:, :, fp_ * 256:(fp_ + 1) * 256], writes=[tw3_])
                wl[fp_] = (w1_, tw1_, w3_, tw3_)
            wload(0)
            for fp in range(NFC // 2):
                if fp + 1 < NFC // 2:
                    wload(fp + 1)
                w1, tw1, w3, tw3 = wl.pop(fp)
                for fi in range(2):
                    f = fp * 2 + fi
                    for (g0, n) in tiles:
                        c0 = g0 - tok0
                        p1, tp1 = pp.next(); p3, tp3 = pp.next()
                        for k in range(8):
                            kb.op("pe", lambda: nc.tensor.matmul(p1[:, 0:n], lhsT=w1[:, k, fi * 128:(fi + 1) * 128], rhs=hT[:, k, c0:c0 + n], start=(k == 0), stop=(k == 7)),
                                  reads=[tw1, thT[k]], writes=[tp1], inc=(k == 7))
                        for k in range(8):
                            kb.op("pe", lambda: nc.tensor.matmul(p3[:, 0:n], lhsT=w3[:, k, fi * 128:(fi + 1) * 128], rhs=hT[:, k, c0:c0 + n], start=(k == 0), stop=(k == 7)),
                                  reads=[tw3, thT[k]], writes=[tp3], inc=(k == 7))
                        s_, ts = sr.next()
                        kb.op("act", lambda: nc.scalar.activation(out=s_[:, 0:n], in_=p1[:, 0:n], func=AF.Silu), reads=[tp1], writes=[ts])
                        u, tu = ur.next()
                        if moe:
                            t_, tt = tr_.next()
                            kb.op("dve", lambda: nc.vector.tensor_tensor(out=t_[:, 0:n], in0=p3[:, 0:n], in1=gbc[:, c0:c0 + n], op=ALU.mult), reads=[tp3, tg], writes=[tt])
                            kb.op("dve", lambda: nc.vector.tensor_tensor(out=u[:, 0:n], in0=s_[:, 0:n], in1=t_[:, 0:n], op=ALU.mult), reads=[ts, tt], writes=[tu])
                        else:
                            kb.op("dve", lambda: nc.vector.tensor_tensor(out=u[:, 0:n], in0=p3[:, 0:n], in1=s_[:, 0:n], op=ALU.mult), reads=[tp3, ts], writes=[tu])
                        kb.dma("sp", scr["UT"][f * 128:(f + 1) * 128, g0:g0 + n], u[:, 0:n], reads=[tu])

    def p_ffn_down(self, L, w2_ap, tok0, first, last, final):
        nc, kb = self.nc, self.kb
        scr = self.scr
        with self.phase():
            tc = T()
            w2 = self.sb([128, NFC, D], BF16)
            w2v = w2_ap.rearrange("(f p) n -> p f n", p=128)
            tw2 = [T() for _ in range(NFC // 4)]
            for f0 in range(0, NFC, 4):
                kb.dma("pool", w2[:, f0:f0 + 4, :], w2v[:, f0:f0 + 4, :], writes=[tw2[f0 // 4]])
            utr = self.sbring(2, [128, NFC, 512], BF16)
            pp = self.psring(4)
            uv = scr["UT"].rearrange("(f p) t -> p f t", p=128)
            if last:
                mods = self.load_mod(L, 1)
                RR = self.resid_rings(L, 1)
            accr = self.sbring(2, [128, D])
            ut = tut = None
            ubase = None
            for g0 in range(tok0, NTOK, 128):
                s = 1 if g0 < CTX else 0
                if ubase is None or g0 >= ubase + uw:
                    ubase = g0
                    uw = CTX - g0 if g0 < CTX else 512
                    ut, tut = utr.next()
                    for qi, f0 in enumerate(range(0, NFC, 7)):
                        kb.dma("act" if qi % 2 == 0 else "sp", ut[:, f0:f0 + 7, 0:uw], uv[:, f0:f0 + 7, ubase:ubase + uw], writes=[tut])
                uo = g0 - ubase
                parts = []
                for hh in range(2):
                    pt, tp = pp.next()
                    for f in range(NFC):
                        kb.op("pe", lambda: nc.tensor.matmul(pt[:, :], lhsT=ut[:, f, uo:uo + 128], rhs=w2[:, f, hh * 512:(hh + 1) * 512], start=(f == 0), stop=(f == NFC - 1)),
                              reads=[tut, tw2[f // 4]], writes=[tp], inc=(f == NFC - 1))
                    parts.append((pt[:, :], tp))
                if not first:
                    acc, ta = accr.next()
                    kb.dma("sp", acc[:], scr["FACC"][g0:g0 + 128, :], writes=[ta])
                    for hh in range(2):
                        sl = slice(hh * 512, (hh + 1) * 512)
                        kb.op("dve", lambda: nc.vector.tensor_tensor(out=acc[:, sl], in0=parts[hh][0], in1=acc[:, sl], op=ALU.add), reads=[parts[hh][1], ta], writes=[ta])
                    parts = [(acc[:, 0:512], ta), (acc[:, 512:1024], ta)]
                if last:
                    xt, tx = RR["xt"].next()
                    kb.dma("sp", xt[:], scr["X1"][g0:g0 + 128, :], writes=[tx])
                    dst = scr["y"][g0 - CTX:g0 - CTX + 128, :] if final else scr["X2"][g0:g0 + 128, :]
                    self.resid_ln(xt, tx, parts, mods[s]["gb"], mods[s]["tgb"], RR["lng"], RR["lnb"], RR["tln"], RR, dst)
                else:
                    if first:
                        acc, ta = accr.next()
                        kb.op("act", lambda: nc.scalar.copy(out=acc[:, 0:512], in_=parts[0][0]), reads=[parts[0][1]], writes=[ta])
                        kb.op("dve", lambda: nc.vector.tensor_copy(out=acc[:, 512:1024], in_=parts[1][0]), reads=[parts[1][1]], writes=[ta])
                    kb.dma("pool", scr["FACC"][g0:g0 + 128, :], acc[:], reads=[ta])

    def ffn(self, L, with_ctx, final):
        moe = (L % 2 == 1)
        tok0 = 0 if with_ctx else CTX
        self.p_ffn_prep(L, with_ctx, moe)
        if not moe:
            self.p_ffn_up(self.inp["ff_w1"], self.inp["ff_w3"], 0, tok0, False)
            self.p_ffn_down(L, self.inp["ff_w2"], tok0, True, True, final)
        else:
            for e in range(NEXP):
                self.p_ffn_up(self.inp["moe_w1"][e], self.inp["moe_w3"][e], e, tok0, True)
                self.p_ffn_down(L, self.inp["moe_w2"][e], tok0, e == 0, e == NEXP - 1, final)


    def rwkv_gen(self, L, d):
        nc, kb = self.nc, self.kb
        scr = self.scr
        rev = (d == 1)
        CH = 64
        with contextlib.nullcontext():
            tc = T()
            def cload(name, shape, src, q="act"):
                t_ = self.sb(shape)
                kb.dma(q, t_[:], src, writes=[tc])
                return t_
            mu = cload("mu", [128, 9], self.inp["rk_mu"][L])
            w0 = cload("w0", [128, 2], self.inp["rk_w0"][L, d]); a0 = cload("a0", [128, 2], self.inp["rk_a0"][L, d])
            w2 = cload("w2", [128, 256], self.inp["rk_w2"][L]); a2 = cload("a2", [128, 256], self.inp["rk_a2"][L])
            kkv = cload("kkv", [128, 2], self.inp["rk_kk"][L]); kav = cload("kav", [128, 2], self.inp["rk_ka"][L]); rkv = cload("rkv", [128, 2], self.inp["rk_rk"][L])
            bones = cload("bones", [128, 128], self.inp["rk_bones"])
            msk = cload("msk", [64, 4, 128], self.inp["rk_msk"][d]); mskT = cload("mskT", [64, 4, 64], self.inp["rk_mskT"][d])
            id4 = cload("id4", [64, 4, 64], self.inp["rk_id4"])
            identf = cload("identf", [128, 128], self.inp["ident_f"][:, :])
            identb = self.sb([128, 128], BF16)
            kb.dma("act", identb[:], self.inp["ident_bf"][:, :], writes=[tc])
            TW = 256
            pbr = self.sbring(1, [128, 9, TW + 2])
            tmp = self.sbring(10, [128, TW])
            tmp1 = self.sbring(2, [128, TW])
            zded = [(self.sb([128, TW]), T()) for _ in range(3)]
            pps = self.psring(1)
            NB = 3
            bkr = Ring([0, 1, 2])
            bank = [(self.ps([128, 8, 64]), T(), T()) for _ in range(NB)]
            def half(i):
                b_, t0_, t1_ = bank[i // 2]
                return (b_[0:64, 0:4, :], t0_) if i % 2 == 0 else (b_[0:64, 4:8, :], t1_)
            def halfF(i):
                b_, t0_, t1_ = bank[i // 2]
                return (b_[:, 0:4, :], t0_) if i % 2 == 0 else (b_[:, 4:8, :], t1_)
            def mk(shape):
                return [(self.sb(shape), T()) for _ in range(1)]
            AR = [mk([128, 4, 2, 64]) for _ in range(2)]; BK = [mk([128, 4, 2, 64]) for _ in range(2)]
            VV = [mk([128, 4, 64]) for _ in range(2)]; GC = [mk([128, 4]) for _ in range(2)]
            ARo = [mk([64, 4, 2, 64]) for _ in range(2)]; BKo = [mk([64, 4, 2, 64]) for _ in range(2)]
            VVo = [mk([64, 4, 64]) for _ in range(2)]; GCo = [mk([64, 4]) for _ in range(2)]
            BON = [mk([128, TW]) for _ in range(2)]
            def mkb(shape):
                return [(self.sb(shape, BF16), T()) for _ in range(1)]
            ARb = [mkb([128, 4, 2, 64]) for _ in range(2)]; BKb = [mkb([128, 4, 2, 64]) for _ in range(2)]; VVb = [mkb([128, 4, 64]) for _ in range(2)]
            ARbo = [mkb([64, 4, 2, 64]) for _ in range(2)]; BKbo = [mkb([64, 4, 2, 64]) for _ in range(2)]; VVbo = [mkb([64, 4, 64]) for _ in range(2)]
            S0 = [(self.sb([64, 4, 64]), T()) for _ in range(2)]
            kb.op("pool", lambda: nc.gpsimd.memset(S0[0][0][:], 0.0), writes=[S0[0][1]])
            scur_box = [0]
            pfin = self.sbring(2, [64, 4, 64], BF16)
            tokr = self.sbring(2, [64, 12, 64], BF16); gbr = self.sbring(2, [64, 4, 128], BF16); gkr = self.sbring(2, [64, 4, 128], BF16)
            xxr = self.sbring(3, [64, 8, 64], BF16); prr = self.sbring(3, [64, 4, 64], BF16)
            wr = self.sbring(2, [64, 4, 64], BF16); ur = self.sbring(2, [64, 4, 64], BF16); yr = self.sbring(3, [64, 256])
            tiles = [(0, CTX, 0, CTX)] + [(CTX + i * TW, TW, CTX, NTOK) for i in range(SEQ // TW)]
            if rev:
                tiles = [tiles[0]] + tiles[:0:-1]
            for ti, (g0, n, s_lo, s_hi) in enumerate(tiles):
                par = 0
                nch = n // CH
                pb, tpb = pbr.next()
                lo = max(s_lo, g0 - 1); hi = min(s_hi, g0 + n + 1)
                kb.op("pool", lambda: nc.gpsimd.memset(pb[:, :, 0:1], 0.0), writes=[tpb])
                kb.op("pool", lambda: nc.gpsimd.memset(pb[:, :, n + 1:n + 2], 0.0), writes=[tpb])
                pv = scr["PBT"].rearrange("(c p) t -> p c t", p=128)
                for c0 in range(0, 9, 3):
                    kb.dma("sp", pb[:, c0:c0 + 3, lo - (g0 - 1):hi - (g0 - 1)], pv[:, c0:c0 + 3, lo:hi], writes=[tpb])

                def zshift(c, dst, tdst):
                    t1, tt1 = tmp1.next()
                    kb.op("dve", lambda: nc.vector.tensor_tensor(out=t1[:, 0:n], in0=pb[:, c, 0:n], in1=pb[:, c, 2:n + 2], op=ALU.add), reads=[tpb], writes=[tt1])
                    kb.op("dve", lambda: nc.vector.scalar_tensor_tensor(out=t1[:, 0:n], in0=t1[:, 0:n], scalar=0.5, in1=pb[:, c, 1:n + 1], op0=ALU.mult, op1=ALU.subtract), reads=[tt1, tpb], writes=[tt1])
                    kb.op("dve", lambda: nc.vector.scalar_tensor_tensor(out=dst, in0=t1[:, 0:n], scalar=mu[:, c:c + 1], in1=pb[:, c, 1:n + 1], op0=ALU.mult, op1=ALU.add), reads=[tt1, tpb, tc], writes=[tdst])

                def v3(ap_):
                    return ap_.rearrange("p (c t) -> p c t", t=CH)

                zw, tzw = zded[0]; zshift(6, zw[:, 0:n], tzw)
                kb.op("act", lambda: nc.scalar.activation(out=zw[:, 0:n], in_=zw[:, 0:n], func=AF.Tanh), reads=[tzw], writes=[tzw])
                za, tza = zded[1]; zshift(7, za[:, 0:n], tza)
                yield
                for c in range(2):
                    ar, tar = AR[c][par]; bk, tbk = BK[c][par]; vv_, tvv = VV[c][par]; gc, tgc = GC[c][par]; bon, tbon = BON[c][par]
                    p1, tp1 = pps.next()
                    kb.op("pe", lambda: nc.tensor.matmul(p1[:, 0:n], lhsT=w2[64 * d:64 * d + 64, c * 128:(c + 1) * 128], rhs=zw[64 * d:64 * d + 64, 0:n], start=True, stop=True), reads=[tzw, tc], writes=[tp1])
                    ld, tld = tmp.next()
                    kb.op("act", lambda: nc.scalar.activation(out=ld[:, 0:n], in_=p1[:, 0:n], func=AF.Sigmoid, bias=w0[:, c:c + 1]), reads=[tp1, tc], writes=[tld])
                    kb.op("pool", lambda: nc.gpsimd.tensor_scalar(out=ld[:, 0:n], in0=ld[:, 0:n], scalar1=-math.exp(-0.5), scalar2=None, op0=ALU.mult), reads=[tld], writes=[tld])
                    yield
                    p2, tp2 = pps.next()
                    kb.op("pe", lambda: nc.tensor.matmul(p2[:, 0:n], lhsT=a2[64 * d:64 * d + 64, c * 128:(c + 1) * 128], rhs=za[64 * d:64 * d + 64, 0:n], start=True, stop=True), reads=[tza, tc], writes=[tp2])
                    ac, tac = tmp.next()
                    kb.op("act", lambda: nc.scalar.activation(out=ac[:, 0:n], in_=p2[:, 0:n], func=AF.Sigmoid, bias=a0[:, c:c + 1]), reads=[tp2, tc], writes=[tac])
                    yield
                    bufs = [tmp.next(), tmp.next()]
                    src, tsrc = ld, tld
                    bi = 0
                    sh = 1
                    while sh < CH:
                        dst_, tdst_ = bufs[bi]
                        kb.op("pool", lambda: nc.gpsimd.tensor_copy(out=dst_[:, 0:n], in_=src[:, 0:n]), reads=[tsrc], writes=[tdst_])
                        if not rev:
                            kb.op("dve", lambda: nc.vector.tensor_tensor(out=v3(dst_[:, 0:n])[:, :, sh:CH], in0=v3(src[:, 0:n])[:, :, sh:CH], in1=v3(src[:, 0:n])[:, :, 0:CH - sh], op=ALU.add), reads=[tsrc, tdst_], writes=[tdst_])
                        else:
                            kb.op("dve", lambda: nc.vector.tensor_tensor(out=v3(dst_[:, 0:n])[:, :, 0:CH - sh], in0=v3(src[:, 0:n])[:, :, 0:CH - sh], in1=v3(src[:, 0:n])[:, :, sh:CH], op=ALU.add), reads=[tsrc, tdst_], writes=[tdst_])
                        src, tsrc = dst_, tdst_
                        bi = 1 - bi
                        sh *= 2
                        if sh in (4, 16):
                            yield
                    cs, tcs = src, tsrc
                    egx, tegx = bufs[bi]
                    kb.op("dve", lambda: nc.vector.tensor_tensor(out=egx[:, 0:n], in0=cs[:, 0:n], in1=ld[:, 0:n], op=ALU.subtract), reads=[tcs, tld], writes=[tegx])
                    kb.op("act", lambda: nc.scalar.activation(out=egx[:, 0:n], in_=egx[:, 0:n], func=AF.Exp), reads=[tegx], writes=[tegx])
                    egi, tegi = ld, tld
                    kb.op("act", lambda: nc.scalar.activation(out=egi[:, 0:n], in_=cs[:, 0:n], func=AF.Exp, scale=-1.0), reads=[tcs, tegx], writes=[tegi])
                    kb.op("act", lambda: nc.scalar.activation(out=cs[:, 0:n], in_=cs[:, 0:n], func=AF.Exp), reads=[tcs, tegi], writes=[tcs])
                    eg, teg = cs, tcs
                    yield
                    gsel = (CH - 1) if not rev else 0
                    kb.op("pool", lambda: nc.gpsimd.tensor_copy(out=gc[:, 0:nch], in_=v3(eg[:, 0:n])[:, :, gsel]), reads=[teg], writes=[tgc])
                    yield
                    zr, tzr = tmp.next(); zshift(0 + c, zr[:, 0:n], tzr)
                    zk, tzk = tmp.next(); zshift(2 + c, zk[:, 0:n], tzk)
                    zshift(4 + c, vv_[:, 0:nch, :].rearrange("p c t -> p (c t)"), tvv)
                    yield
                    kx, tkx = tmp.next()
                    kb.op("dve", lambda: nc.vector.tensor_scalar(out=kx[:, 0:n], in0=zk[:, 0:n], scalar1=kkv[:, c:c + 1], scalar2=None, op0=ALU.mult), reads=[tzk, tc], writes=[tkx])
                    sq, tsq = tmp.next()
                    kb.op("pool", lambda: nc.gpsimd.tensor_tensor(out=sq[:, 0:n], in0=kx[:, 0:n], in1=kx[:, 0:n], op=ALU.mult), reads=[tkx], writes=[tsq])
                    p3, tp3 = pps.next()
                    kb.op("pe", lambda: nc.tensor.matmul(p3[:, 0:n], lhsT=bones[:, :], rhs=sq[:, 0:n], start=True, stop=True), reads=[tsq, tc], writes=[tp3])
                    kb.op("act", lambda: nc.scalar.activation(out=sq[:, 0:n], in_=p3[:, 0:n], func=AF.Ln, bias=1e-24), reads=[tp3], writes=[tsq])
                    kb.op("act", lambda: nc.scalar.activation(out=sq[:, 0:n], in_=sq[:, 0:n], func=AF.Exp, scale=-0.5), reads=[tsq], writes=[tsq])
                    kb.op("dve", lambda: nc.vector.tensor_tensor(out=kx[:, 0:n], in0=kx[:, 0:n], in1=sq[:, 0:n], op=ALU.mult), reads=[tkx, tsq], writes=[tkx])
                    yield
                    kb.op("dve", lambda: nc.vector.scalar_tensor_tensor(out=ar[:, 0:nch, 0, :], in0=v3(kx[:, 0:n]), scalar=-1.0, in1=v3(egx[:, 0:n]), op0=ALU.mult, op1=ALU.mult), reads=[tkx, tegx], writes=[tar])
                    kb.op("pool", lambda: nc.gpsimd.tensor_tensor(out=sq[:, 0:n], in0=kx[:, 0:n], in1=ac[:, 0:n], op=ALU.mult), reads=[tkx, tac], writes=[tsq])
                    kb.op("dve", lambda: nc.vector.tensor_tensor(out=bk[:, 0:nch, 0, :], in0=v3(sq[:, 0:n]), in1=v3(egi[:, 0:n]), op=ALU.mult), reads=[tsq, tegi], writes=[tbk])
                    kb.op("dve", lambda: nc.vector.tensor_tensor(out=ar[:, 0:nch, 1, :], in0=v3(zr[:, 0:n]), in1=v3(eg[:, 0:n]), op=ALU.mult), reads=[tzr, teg], writes=[tar])
                    yield
                    kb.op("dve", lambda: nc.vector.tensor_scalar(out=ac[:, 0:n], in0=ac[:, 0:n], scalar1=-1.0, scalar2=kav[:, c:c + 1], op0=ALU.add, op1=ALU.mult), reads=[tac, tsq, tc], writes=[tac])
                    kb.op("dve", lambda: nc.vector.scalar_tensor_tensor(out=zk[:, 0:n], in0=ac[:, 0:n], scalar=1.0, in1=zk[:, 0:n], op0=ALU.add, op1=ALU.mult), reads=[tac, tzk, tkx], writes=[tzk])
                    kb.op("dve", lambda: nc.vector.tensor_tensor(out=bk[:, 0:nch, 1, :], in0=v3(zk[:, 0:n]), in1=v3(egi[:, 0:n]), op=ALU.mult), reads=[tzk, tegi], writes=[tbk])
                    yield
                    kb.op("dve", lambda: nc.vector.scalar_tensor_tensor(out=zr[:, 0:n], in0=zr[:, 0:n], scalar=rkv[:, c:c + 1], in1=zk[:, 0:n], op0=ALU.mult, op1=ALU.mult), reads=[tzr, tzk, tar, tc], writes=[tzr])
                    p4, tp4 = pps.next()
                    kb.op("pe", lambda: nc.tensor.matmul(p4[:, 0:n], lhsT=bones[:, :], rhs=zr[:, 0:n], start=True, stop=True), reads=[tzr, tc], writes=[tp4])
                    kb.op("dve", lambda: nc.vector.tensor_tensor(out=bon[:, 0:n], in0=p4[:, 0:n], in1=vv_[:, 0:nch, :].rearrange("p c t -> p (c t)"), op=ALU.mult), reads=[tp4, tvv], writes=[tbon])
                    yield
                    arb, tarb = ARb[c][par]; bkb, tbkb = BKb[c][par]; vvb, tvvb = VVb[c][par]
                    kb.op("pool", lambda: nc.gpsimd.tensor_copy(out=arb[:, 0:nch], in_=ar[:, 0:nch]), reads=[tar], writes=[tarb])
                    kb.op("pool", lambda: nc.gpsimd.tensor_copy(out=bkb[:, 0:nch], in_=bk[:, 0:nch]), reads=[tbk], writes=[tbkb])
                    kb.op("pool", lambda: nc.gpsimd.tensor_copy(out=vvb[:, 0:nch], in_=vv_[:, 0:nch]), reads=[tvv], writes=[tvvb])
                    kb.dma("sp", ARo[c][par][0][:, 0:nch], ar[64:128, 0:nch], reads=[tar], writes=[ARo[c][par][1]])
                    kb.dma("sp", ARbo[c][par][0][:, 0:nch], arb[64:128, 0:nch], reads=[tarb], writes=[ARbo[c][par][1]])
                    kb.dma("sp", BKbo[c][par][0][:, 0:nch], bkb[64:128, 0:nch], reads=[tbkb], writes=[BKbo[c][par][1]])
                    kb.dma("sp", VVbo[c][par][0][:, 0:nch], vvb[64:128, 0:nch], reads=[tvvb], writes=[VVbo[c][par][1]])
                    kb.dma("sp", GCo[c][par][0][:, 0:nch], gc[64:128, 0:nch], reads=[tgc], writes=[GCo[c][par][1]])

                def hd(h):
                    c = h // 2
                    if h % 2 == 0:
                        return (ARb[c][par][0][0:64], ARb[c][par][1], BKb[c][par][0][0:64], BKb[c][par][1], VVb[c][par][0][0:64], VVb[c][par][1], GC[c][par][0][0:64], GC[c][par][1], AR[c][par][0][0:64], AR[c][par][1])
                    return (ARbo[c][par][0], ARbo[c][par][1], BKbo[c][par][0], BKbo[c][par][1], VVbo[c][par][0], VVbo[c][par][1], GCo[c][par][0], GCo[c][par][1], ARo[c][par][0], ARo[c][par][1])
                H = [hd(h) for h in range(4)]
                chs = list(range(nch))
                if rev:
                    chs = chs[::-1]
                def nb():
                    i_, _t = bkr.next()
                    return bank[i_][0], bank[i_][1]

                def prep_chain(ch, out):
                    gch = g0 + ch * CH
                    tok, ttok = tokr.next()
                    B0, TB0 = nb()
                    for a_ in range(2):
                        for h in range(4):
                            ar, tar, bk, tbk, vh, tvh, gch_, tgch, a32, ta32 = H[h]
                            kb.op("pe", lambda: nc.tensor.matmul(B0[0:64, a_ * 4 + h, :], lhsT=bk[:, ch, a_, :], rhs=identb[0:64, 0:64], start=True, stop=True), reads=[tbk, tc], writes=[TB0], inc=(a_ == 1 and h == 3))
                    kb.op("act", lambda: nc.scalar.copy(out=tok[:, 0:8, :], in_=B0[0:64, :, :]), reads=[TB0], writes=[ttok])
                    B1, TB1 = nb()
                    for h in range(4):
                        ar, tar, bk, tbk, vh, tvh, gch_, tgch, a32, ta32 = H[h]
                        kb.op("pe", lambda: nc.tensor.matmul(B1[0:64, h, :], lhsT=vh[:, ch, :], rhs=identb[0:64, 0:64], start=True, stop=True), reads=[tvh, tc], writes=[TB1], inc=False)
                    for h in range(4):
                        ar, tar, bk, tbk, vh, tvh, gch_, tgch, a32, ta32 = H[h]
                        kb.op("pe", lambda: nc.tensor.matmul(B1[0:64, 4 + h, :], lhsT=ar[:, ch, 0, :], rhs=bk[:, ch, 0, :], start=True, stop=True), reads=[tar, tbk], writes=[TB1], inc=(h == 3))
                    kb.op("dve", lambda: nc.vector.tensor_copy(out=tok[:, 8:12, :], in_=B1[0:64, 0:4, :]), reads=[TB1], writes=[ttok])
                    xx, txx = xxr.next()
                    kb.op("dve", lambda: nc.vector.tensor_tensor(out=xx[:, 4:8, :], in0=B1[0:64, 4:8, :], in1=mskT[:], op=ALU.mult), reads=[TB1, tc], writes=[txx])
                    yield
                    B2, TB2 = nb()
                    for h in range(4):
                        ar, tar, bk, tbk, vh, tvh, gch_, tgch, a32, ta32 = H[h]
                        kb.op("pe", lambda: nc.tensor.matmul(B2[0:64, 2 * h:2 * h + 2, :], lhsT=bk[:, ch, 0, :], rhs=ar[:, ch, :, :], start=True, stop=True), reads=[tar, tbk], writes=[TB2], inc=(h == 3))
                    gbs, tgbs = gbr.next(); gks, tgks = gkr.next()
                    kb.op("dve", lambda: nc.vector.tensor_tensor(out=gbs[:], in0=B2[0:64].rearrange("p (h a) t -> p h (a t)", a=2), in1=msk[:], op=ALU.mult), reads=[TB2, tc], writes=[tgbs])
                    B3, TB3 = nb()
                    for h in range(4):
                        ar, tar, bk, tbk, vh, tvh, gch_, tgch, a32, ta32 = H[h]
                        kb.op("pe", lambda: nc.tensor.matmul(B3[0:64, 2 * h:2 * h + 2, :], lhsT=bk[:, ch, 1, :], rhs=ar[:, ch, :, :], start=True, stop=True), reads=[tar, tbk], writes=[TB3], inc=(h == 3))
                    kb.op("dve", lambda: nc.vector.tensor_tensor(out=gks[:], in0=B3[0:64].rearrange("p (h a) t -> p h (a t)", a=2), in1=msk[:], op=ALU.mult), reads=[TB3, tc], writes=[tgks])
                    kb.op("pool", lambda: nc.gpsimd.tensor_copy(out=xx[:, 0:4, :], in_=gbs[:, :, 0:64]), reads=[tgbs], writes=[txx])
                    pm, tpm = prr.next()
                    kb.op("pool", lambda: nc.gpsimd.tensor_tensor(out=pm[:], in0=xx[:, 0:4, :], in1=id4[:], op=ALU.add), reads=[txx, tc], writes=[tpm])
                    yield
                    def squares(lev, xx_, txx_):
                        Bq, TBq = nb()
                        if lev < 5:
                            for h in range(4):
                                kb.op("pe", lambda: nc.tensor.matmul(Bq[0:64, h, :], lhsT=xx_[:, 4 + h, :], rhs=xx_[:, h, :], start=True, stop=True), reads=[txx_], writes=[TBq], inc=False)
                        for h in range(4):
                            kb.op("pe", lambda: nc.tensor.matmul(Bq[0:64, 4 + h, :], lhsT=xx_[:, h, :], rhs=xx_[:, 4 + h, :], start=True, stop=True), reads=[txx_], writes=[TBq], inc=(h == 3))
                        return Bq, TBq

                    def evac_sq(lev, Bq, TBq):
                        xn, txn = xxr.next()
                        if lev < 5:
                            kb.op("act", lambda: nc.scalar.copy(out=xn[:, :, :], in_=Bq[0:64, :, :]), reads=[TBq], writes=[txn])
                        else:
                            kb.op("act", lambda: nc.scalar.copy(out=xn[:, 4:8, :], in_=Bq[0:64, 4:8, :]), reads=[TBq], writes=[txn])
                        return xn, txn
                    Bq, TBq = squares(1, xx, txx)
                    xx, txx = evac_sq(1, Bq, TBq)
                    yield
                    for lev in range(1, 6):
                        Bp, TBp = nb()
                        for h in range(4):
                            kb.op("pe", lambda: nc.tensor.matmul(Bp[0:64, h, :], lhsT=xx[:, 4 + h, :], rhs=pm[:, h, :], start=True, stop=True), reads=[txx, tpm], writes=[TBp], inc=(h == 3))
                        if lev < 5:
                            Bq, TBq = squares(lev + 1, xx, txx)
                        pn, tpn = prr.next() if lev < 5 else pfin.next()
                        kb.op("dve", lambda: nc.vector.tensor_tensor(out=pn[:], in0=Bp[0:64, 0:4, :], in1=pm[:], op=ALU.add), reads=[TBp, tpm], writes=[tpn])
                        pm, tpm = pn, tpn
                        if lev < 5:
                            xx, txx = evac_sq(lev + 1, Bq, TBq)
                        yield
                    out.update(tok=tok, ttok=ttok, gbs=gbs, tgbs=tgbs, gks=gks, tgks=tgks, pm=pm, tpm=tpm)

                def state_chain(ch, pr_):
                    gch = g0 + ch * CH
                    tok, ttok, gbs, tgbs, gks, tgks, pm, tpm = (pr_[k_] for k_ in ("tok", "ttok", "gbs", "tgbs", "gks", "tgks", "pm", "tpm"))
                    s0, ts0 = S0[scur_box[0]]
                    B0, TB0 = nb()
                    for h in range(4):
                        ar, tar, bk, tbk, vh, tvh, gch_, tgch, a32, ta32 = H[h]
                        kb.op("pe", lambda: nc.tensor.matmul(B0[0:64, h, :], lhsT=a32[:, ch, 0, :], rhs=s0[:, h, :], start=(h == 0), stop=False, skip_group_check=True), reads=[ta32, ts0], writes=[TB0], inc=False)
                        kb.op("pe", lambda: nc.tensor.matmul(B0[0:64, h, :], lhsT=gks[:, h, 0:64], rhs=tok[:, 8 + h, :], start=False, stop=True, skip_group_check=True), reads=[tgks, ttok], writes=[TB0], inc=(h == 3))
                    wsb, twsb = wr.next()
                    kb.op("act", lambda: nc.scalar.copy(out=wsb[:], in_=B0[0:64, 0:4, :]), reads=[TB0], writes=[twsb])
                    yield
                    B1, TB1 = nb()
                    for h in range(4):
                        kb.op("pe", lambda: nc.tensor.matmul(B1[0:64, h, :], lhsT=pm[:, h, :], rhs=wsb[:, h, :], start=True, stop=True), reads=[tpm, twsb], writes=[TB1], inc=(h == 3))
                    usb, tusb = ur.next()
                    kb.op("dve", lambda: nc.vector.tensor_copy(out=usb[:], in_=B1[0:64, 0:4, :]), reads=[TB1], writes=[tusb])
                    yield
                    B2, TB2 = nb()
                    for h in range(4):
                        kb.op("pe", lambda: nc.tensor.matmul(B2[0:64, h, :], lhsT=identf[0:64, 0:64], rhs=s0[:, h, :], start=(h == 0), stop=False, skip_group_check=True), reads=[ts0, tc], writes=[TB2], inc=False)
                        kb.op("pe", lambda: nc.tensor.matmul(B2[0:64, h, :], lhsT=tok[:, h, :], rhs=usb[:, h, :], start=False, stop=False, skip_group_check=True), reads=[ttok, tusb], writes=[TB2], inc=False)
                        kb.op("pe", lambda: nc.tensor.matmul(B2[0:64, h, :], lhsT=tok[:, 4 + h, :], rhs=tok[:, 8 + h, :], start=False, stop=True, skip_group_check=True), reads=[ttok], writes=[TB2], inc=(h == 3))
                    sn, tsn = S0[1 - scur_box[0]]
                    for h in range(4):
                        ar, tar, bk, tbk, vh, tvh, gch_, tgch, a32, ta32 = H[h]
                        kb.op("dve", lambda: nc.vector.tensor_scalar(out=sn[:, h, :], in0=B2[0:64, h, :], scalar1=gch_[:, ch:ch + 1], scalar2=None, op0=ALU.mult), reads=[TB2, tgch], writes=[tsn])
                    yield
                    B3, TB3 = nb()
                    for h in range(4):
                        ar, tar, bk, tbk, vh, tvh, gch_, tgch, a32, ta32 = H[h]
                        kb.op("pe", lambda: nc.tensor.matmul(B3[0:64, h, :], lhsT=a32[:, ch, 1, :], rhs=s0[:, h, :], start=(h == 0), stop=False, skip_group_check=True), reads=[ta32, ts0], writes=[TB3], inc=False)
                        kb.op("pe", lambda: nc.tensor.matmul(B3[0:64, h, :], lhsT=gbs[:, h, 64:128], rhs=usb[:, h, :], start=False, stop=False, skip_group_check=True), reads=[tgbs, tusb], writes=[TB3], inc=False)
                        kb.op("pe", lambda: nc.tensor.matmul(B3[0:64, h, :], lhsT=gks[:, h, 64:128], rhs=tok[:, 8 + h, :], start=False, stop=True, skip_group_check=True), reads=[tgks, ttok], writes=[TB3], inc=(h == 3))
                    scur_box[0] = 1 - scur_box[0]
                    ysb, tysb = yr.next()
                    kb.op("act", lambda: nc.scalar.copy(out=ysb[:], in_=B3[0:64, 0:4, :].rearrange("p h t -> p (h t)")), reads=[TB3], writes=[tysb])
                    B4, TB4 = nb()
                    for c in range(2):
                        kb.op("pe", lambda: nc.tensor.matmul(B4[0:64].rearrange("p h t -> p (h t)")[:, c * 128:(c + 1) * 128], lhsT=BON[c][par][0][:, ch * CH:(ch + 1) * CH], rhs=identf[:, :], start=True, stop=True),
                              reads=[BON[c][par][1], tc], writes=[TB4], inc=(c == 1))
                    kb.op("dve", lambda: nc.vector.tensor_tensor(out=ysb[:], in0=B4[0:64].rearrange("p h t -> p (h t)")[:, 0:256], in1=ysb[:], op=ALU.add), reads=[TB4, tysb], writes=[tysb])
                    kb.dma("pool", scr["YB"][d, gch:gch + CH, :], ysb[:], reads=[tysb])
                    yield


                prods = {}
                yield from prep_chain(chs[0], prods)
                for ci, ch in enumerate(chs):
                    gens_ = [state_chain(ch, prods)]
                    nxt = {}
                    if ci + 1 < len(chs):
                        gens_.append(prep_chain(chs[ci + 1], nxt))
                    alive_ = list(gens_)
                    while alive_:
                        for g_ in list(alive_):
                            try:
                                next(g_)
                            except StopIteration:
                                alive_.remove(g_)
                        yield
                    prods = nxt
    def p_rwkv_both(self, L):
        with self.phase():
            g0_ = self.rwkv_gen(L, 0)
            g1_ = self.rwkv_gen(L, 1)
            alive = [g0_]
            rounds = 0
            started1 = False
            while alive:
                for g in list(alive):
                    try:
                        next(g)
                    except StopIteration:
                        alive.remove(g)
                rounds += 1
                if not started1 and rounds >= 40:
                    alive.append(g1_)
                    started1 = True
            if not started1:
                for _ in g1_:
                    pass

    def p_rwkv_fin(self, L, with_ctx):
        nc, kb = self.nc, self.kb
        scr = self.scr
        with self.phase():
            tc = T()
            g2 = self.sb([128, 256]); lnx = self.sb([128, 2, 256]); mu8 = self.sb([128, 9]); identb = self.sb([128, 128], BF16)
            kb.dma("act", g2[:], self.inp["rk_g2"][L], writes=[tc])
            kb.dma("act", lnx[:], self.inp["rk_lnx128"][L], writes=[tc])
            kb.dma("act", mu8[:], self.inp["rk_mu"][L], writes=[tc])
            kb.dma("act", identb[:], self.inp["ident_bf"][:, :], writes=[tc])
            pgr = self.sbring(2, [128, 130]); zr_ = self.sbring(2, [128, 128]); t1r = self.sbring(2, [128, 128])
            y0r = self.sbring(2, [128, 256]); y1r = self.sbring(2, [128, 256]); str_ = self.sbring(2, [128, 40])
            fbr = self.sbring(2, [128, 256], BF16); obr = self.sbring(3, [128, 128], BF16)
            pgp = self.psring(2, [128, 256]); ptp = self.psring(2, [128, 128], BF16)
            for g0 in range(0 if with_ctx else CTX, NTOK, 128):
                s_lo, s_hi = (0, CTX) if g0 < CTX else (CTX, NTOK)
                lo = max(s_lo, g0 - 1); hi = min(s_hi, g0 + 129)
                pg, tpg = pgr.next()
                kb.op("pool", lambda: nc.gpsimd.memset(pg[:, 0:1], 0.0), writes=[tpg])
                kb.op("pool", lambda: nc.gpsimd.memset(pg[:, 129:130], 0.0), writes=[tpg])
                kb.dma("sp", pg[:, lo - (g0 - 1):hi - (g0 - 1)], scr["PBT"][1024:1152, lo:hi], writes=[tpg])
                t1, tt1 = t1r.next(); zg, tzg = zr_.next()
                kb.op("dve", lambda: nc.vector.tensor_tensor(out=t1[:], in0=pg[:, 0:128], in1=pg[:, 2:130], op=ALU.add), reads=[tpg], writes=[tt1])
                kb.op("dve", lambda: nc.vector.scalar_tensor_tensor(out=t1[:], in0=t1[:], scalar=0.5, in1=pg[:, 1:129], op0=ALU.mult, op1=ALU.subtract), reads=[tt1, tpg], writes=[tt1])
                kb.op("dve", lambda: nc.vector.scalar_tensor_tensor(out=zg[:], in0=t1[:], scalar=mu8[:, 8:9], in1=pg[:, 1:129], op0=ALU.mult, op1=ALU.add), reads=[tt1, tpg, tc], writes=[tzg])
                kb.op("act", lambda: nc.scalar.activation(out=zg[:], in_=zg[:], func=AF.Sigmoid), reads=[tzg], writes=[tzg])
                gp, tgp = pgp.next()
                kb.op("pe", lambda: nc.tensor.matmul(gp[:, :], lhsT=zg[:, :], rhs=g2[:, :], start=True, stop=True), reads=[tzg, tc], writes=[tgp])
                y0, ty0 = y0r.next(); y1, ty1 = y1r.next()
                kb.dma("sp", y0[:], scr["YB"][0, g0:g0 + 128, :], writes=[ty0])
                kb.dma("act", y1[:], scr["YB"][1, g0:g0 + 128, :], writes=[ty1])
                kb.op("pool", lambda: nc.gpsimd.tensor_tensor(out=y0[:], in0=y0[:], in1=y1[:], op=ALU.add), reads=[ty0, ty1], writes=[ty0])
                st, tst = str_.next()
                for h in range(4):
                    kb.op("dve", lambda: nc.vector.bn_stats(out=st[:, h * 6:(h + 1) * 6], in_=y0[:, h * 64:(h + 1) * 64]), reads=[ty0], writes=[tst])
                for h in range(4):
                    kb.op("dve", lambda: nc.vector.bn_aggr(out=st[:, 24 + 2 * h:26 + 2 * h], in_=st[:, h * 6:(h + 1) * 6]), reads=[tst], writes=[tst])
                sv = st[:, 24:32].rearrange("p (h a) -> p h a", a=2)
                kb.op("act", lambda: nc.scalar.activation(out=st[:, 32:36], in_=sv[:, :, 1], func=AF.Ln, bias=GN_EPS), reads=[tst], writes=[tst])
                kb.op("act", lambda: nc.scalar.activation(out=st[:, 32:36], in_=st[:, 32:36], func=AF.Exp, scale=-0.5), reads=[tst], writes=[tst])
                for h in range(4):
                    kb.op("dve", lambda: nc.vector.tensor_scalar(out=y0[:, h * 64:(h + 1) * 64], in0=y0[:, h * 64:(h + 1) * 64], scalar1=st[:, 24 + 2 * h:25 + 2 * h], scalar2=st[:, 32 + h:33 + h], op0=ALU.subtract, op1=ALU.mult),
                          reads=[ty0, tst], writes=[ty0])
                kb.op("pool", lambda: nc.gpsimd.tensor_tensor(out=y0[:], in0=y0[:], in1=lnx[:, 0, :], op=ALU.mult), reads=[ty0, tc], writes=[ty0])
                kb.op("pool", lambda: nc.gpsimd.tensor_tensor(out=y0[:], in0=y0[:], in1=lnx[:, 1, :], op=ALU.add), reads=[ty0, tc], writes=[ty0])
                fb, tfb = fbr.next()
                kb.op("dve", lambda: nc.vector.tensor_tensor(out=fb[:], in0=gp[:, :], in1=y0[:], op=ALU.mult), reads=[tgp, ty0], writes=[tfb])
                for c in range(2):
                    tp_, ttp = ptp.next()
                    kb.op("pe", lambda: nc.tensor.transpose(out=tp_[:, :], in_=fb[:, c * 128:(c + 1) * 128], identity=identb[:]), reads=[tfb, tc], writes=[ttp])
                    ob, tob = obr.next()
                    kb.op("act", lambda: nc.scalar.copy(out=ob[:], in_=tp_[:, :]), reads=[ttp], writes=[tob])
                    kb.dma("pool", scr["CATT"][512 + c * 128:512 + (c + 1) * 128, g0:g0 + 128], ob[:], reads=[tob])


def _rope_tables(dim):
    q = dim // 4
    inv = (10000.0 ** (-np.arange(q, dtype=np.float32) / q)).astype(np.float32)
    t = np.arange(SEQ)
    rows = (t // 64).astype(np.float32)
    cols = (t % 64).astype(np.float32)
    cos = np.zeros((dim, SEQ), np.float32)
    sin = np.zeros((dim, SEQ), np.float32)
    perm = np.zeros(dim, np.int64)
    for d in range(dim):
        half = d // (dim // 2)
        within = d % (dim // 2)
        part = within // q
        f = within % q
        ang = ((rows if half == 0 else cols) * inv[f]).astype(np.float32)
        cos[d] = np.cos(ang)
        sin[d] = -np.sin(ang) if part == 0 else np.sin(ang)
        perm[d] = d + q if part == 0 else d - q
    return cos, sin, perm


def prep_inputs(inp):
    f32 = np.float32
    cosA, sinA, permA = _rope_tables(64)
    cosC, sinC, permC = _rope_tables(32)
    shared = {}
    w_in = inp["w_in"]
    ext = np.zeros((DEPTH, D, NCOL), f32)
    pa = np.concatenate([m * 64 + permA for m in range(8)])
    ext[:, :, OQA:OQA + 512] = w_in[:, :, 0:512]
    ext[:, :, OQAP:OQAP + 512] = w_in[:, :, 0:512][:, :, pa]
    ext[:, :, OKA:OKA + 512] = w_in[:, :, 512:1024]
    ext[:, :, OKAP:OKAP + 512] = w_in[:, :, 512:1024][:, :, pa]
    ext[:, :, OVA:OVA + 512] = w_in[:, :, 1024:1536]
    ext[:, :, OB:OB + 1152] = w_in[:, :, 1536:2688]
    ext[:, :, OCQ:OCQ + 256] = w_in[:, :, 2688:2944]
    ext[:, :, OCKV:OCKV + 128] = w_in[:, :, 2944:3072]
    ext[:, :, OKPE:OKPE + 64] = w_in[:, :, 2944:3008]
    ext[:, :, OKPE + 64:OKPE + 96] = w_in[:, :, 3072:3104]
    ext[:, :, OKPEP:OKPEP + 64] = w_in[:, :, 2944:3008]
    ext[:, :, OKPEP + 64:OKPEP + 96] = w_in[:, :, 3072:3104][:, :, permC]
    shared["w_in_ext"] = ext
    shared["ada_w"] = inp["ada_w"]
    shared["ada_b2"] = np.ascontiguousarray(np.repeat(inp["ada_b"][:, None, :], 2, axis=1))
    shared["ident_bf"] = np.eye(128, dtype=f32).astype(ml_dtypes.bfloat16)
    shared["ident_f"] = np.eye(128, dtype=f32)
    shared["cosA"] = np.concatenate([cosA, cosA], 0)
    shared["sinA"] = np.concatenate([sinA, sinA], 0)
    cC = np.zeros((96, SEQ), f32); sC = np.zeros((96, SEQ), f32)
    cC[64:96] = cosC; sC[64:96] = sinC
    shared["cosC"] = cC; shared["sinC"] = sC
    wuq = inp["w_uq"]
    pc = np.concatenate([np.concatenate([h * 96 + np.arange(64), h * 96 + 64 + permC]) for h in range(4)])
    shared["wuq"] = wuq
    shared["wuqp"] = np.ascontiguousarray(wuq[:, :, pc])
    shared["wukv"] = inp["w_ukv"]
    vc = np.concatenate([h * 128 + 64 + np.arange(64) for h in range(4)])
    shared["wukv_v"] = np.ascontiguousarray(inp["w_ukv"][:, :, vc])
    shared["qng"] = np.ascontiguousarray(inp["q_norm_g"].reshape(DEPTH, 2, 128).transpose(0, 2, 1))
    shared["kvng"] = np.ascontiguousarray(inp["kv_norm_g"].reshape(DEPTH, 128, 1))
    lamv = np.stack([inp["lam_q1"], inp["lam_k1"], inp["lam_q2"], inp["lam_k2"]], 1)
    shared["lamv"] = np.ascontiguousarray(np.broadcast_to(lamv[:, None], (DEPTH, 128, 4, 64)))
    shared["dngc"] = np.ascontiguousarray(inp["diff_norm_g"].reshape(DEPTH, 128, 1))
    shared["w_out"] = inp["w_out"]
    fm2 = lambda a: np.ascontiguousarray(a.reshape(a.shape[:-1] + (2, 128)).swapaxes(-1, -2))
    shared["rk_mu"] = np.ascontiguousarray(inp["shift_mu"].reshape(DEPTH, 9, 128).transpose(0, 2, 1))
    shared["rk_w0"] = fm2(inp["w0"]); shared["rk_a0"] = fm2(inp["a0"])
    shared["rk_w2"] = np.ascontiguousarray(inp["w2"].reshape(DEPTH, 128, 256)); shared["rk_a2"] = np.ascontiguousarray(inp["a2"].reshape(DEPTH, 128, 256))
    shared["rk_g2"] = inp["g2"]
    shared["rk_kk"] = fm2(inp["k_k"]); shared["rk_ka"] = fm2(inp["k_a"]); shared["rk_rk"] = fm2(inp["r_k"].reshape(DEPTH, 256))
    lnx = np.stack([inp["lnx_g"], inp["lnx_b"]], 1)
    shared["rk_lnx128"] = np.ascontiguousarray(np.broadcast_to(lnx[:, None], (DEPTH, 128, 2, 256)))
    bo = np.zeros((128, 128), f32); bo[:64, :64] = 1; bo[64:, 64:] = 1
    shared["rk_bones"] = bo
    ii = np.arange(64)
    msk = np.zeros((2, 64, 4, 128), f32); mskT = np.zeros((2, 64, 4, 64), f32)
    msk[0, :, :, 0:64] = (ii[None, :] > ii[:, None])[:, None, :]; msk[0, :, :, 64:] = (ii[None, :] >= ii[:, None])[:, None, :]
    msk[1, :, :, 0:64] = (ii[None, :] < ii[:, None])[:, None, :]; msk[1, :, :, 64:] = (ii[None, :] <= ii[:, None])[:, None, :]
    mskT[0] = (ii[None, :] < ii[:, None])[:, None, :]; mskT[1] = (ii[None, :] > ii[:, None])[:, None, :]
    shared["rk_msk"] = msk; shared["rk_mskT"] = mskT
    shared["rk_id4"] = np.ascontiguousarray(np.broadcast_to(np.eye(64, dtype=f32)[:, None, :], (64, 4, 64)))
    sel8 = np.zeros((NEXP, NEXP, 128), f32)
    for e in range(NEXP):
        sel8[e, e, :] = 1.0
    shared["sel8"] = sel8
    lnp = np.stack([inp["ln1_g"], inp["ln1_b"], inp["ln2_g"], inp["ln2_b"]], 1)
    shared["lnp"] = np.ascontiguousarray(np.broadcast_to(lnp[:, :, None, :], (DEPTH, 4, 128, D)))
    shared["ff_w1"] = inp["ff_w1"][0]; shared["ff_w3"] = inp["ff_w3"][0]; shared["ff_w2"] = inp["ff_w2"][0]
    shared["router"] = inp["router"][0]
    shared["moe_w1"] = inp["moe_w1"][0]; shared["moe_w3"] = inp["moe_w3"][0]; shared["moe_w2"] = inp["moe_w2"][0]
    maps = []
    for b in range(8):
        m = dict(shared)
        m["x"] = inp["x"][b]
        m["ctx"] = inp["ctx"][b]
        cc = np.stack([inp["c"][b].reshape(8, 128).T, inp["c_ctx"].reshape(8, 128).T], -1)
        m["cc"] = np.ascontiguousarray(cc.astype(f32))
        maps.append(m)
    return maps


_CACHE = {}


def build_full():
    P = Prog()
    P.declare()
    P.p_adaln()
    for L in range(DEPTH):
        with_ctx = (L < DEPTH - 1)
        P.p_proj(L, with_ctx)
        P.p_attn_a(L, with_ctx)
        P.p_attn_c(L, with_ctx)
        P.p_rwkv_both(L)
        P.p_rwkv_fin(L, with_ctx)
        P.p_out_ln1(L, with_ctx)
        P.ffn(L, with_ctx, L == DEPTH - 1)
    return P


def kernel(**inputs):
    inp = {k: np.asarray(v) for k, v in inputs.items()}
    P = build_full()
    maps = prep_inputs(inp)
    maps = [{k: np.ascontiguousarray(v) for k, v in m.items() if k in P.inp} for m in maps]
    res = run_bass_kernel_spmd(P.nc, maps, core_ids=list(range(8)))
    out = np.stack([np.asarray(res.results[b]["y"], dtype=np.float32) for b in range(8)], 0)
    return out
```
